# Optimizing a Trainium2 kernel written in Bass

```python
import math
import jax
import jax.numpy as jnp
from jax import lax
import numpy as np

D_MODEL = 1024
BATCH = 8
SEQ = 4096
DEPTH = 2

MIX_W = D_MODEL // 4
N_BRANCH = 4
S5_W = MIX_W
S5_GROUP = 16
S5_GROUPS = S5_W // S5_GROUP
S5_STATE = 64
HEAD_DIM = 64
NSA_HEADS = MIX_W // HEAD_DIM
L_CMP = 32
CMP_STRIDE = 16
CMP_HIDDEN = 2 * HEAD_DIM
L_SEL = 64
TOP_N = 16
WINDOW = 512
Q_BLOCK = 128
ROPE_THETA = 10000.0
FORCED_SCORE = 1.0e4
NEG_INF = -1.0e30
LRU_W = MIX_W
LRU_BLOCKS = 8
LRU_BLOCK_W = LRU_W // LRU_BLOCKS
LRU_CONV = 4
LRU_C = 8.0
SC_W = MIX_W
SC_CONV = 3
FFN_DIM = 2816
N_EXPERTS = 8
TOP_K = 2
EXPERT_DIM = FFN_DIM // 2
RMS_EPS = 1e-6
IN_SPLITS = (S5_W, NSA_HEADS * HEAD_DIM, HEAD_DIM, HEAD_DIM, HEAD_DIM, HEAD_DIM, HEAD_DIM, HEAD_DIM, NSA_HEADS * 3, LRU_W, LRU_W, SC_W, SC_W, SC_W, N_BRANCH * D_MODEL)
IN_COLS = sum(IN_SPLITS)

kernel_name = 'hybrid_s5_nsa_rglru_shortconv_moe_adaln'


def rms_norm(x, g):
    xf = x.astype(jnp.float32)
    y = xf * lax.rsqrt(jnp.mean(xf * xf, axis=-1, keepdims=True) + RMS_EPS)
    return (y * g.astype(jnp.float32)).astype(x.dtype)


def causal_depthwise_conv(x, w):
    width, ch = w.shape
    return lax.conv_general_dilated(x, w[:, None, :].astype(x.dtype), window_strides=(1,), padding=[(width - 1, 0)], dimension_numbers=('NWC', 'WIO', 'NWC'), feature_group_count=ch)


def linear_combine(left, right):
    a_l, b_l = left
    a_r, b_r = right
    return a_l * a_r, a_r * b_l + b_r


def masked_softmax(s, mask):
    p = jax.nn.softmax(jnp.where(mask, s, NEG_INF), axis=-1)
    return jnp.where(mask, p, 0.0)


def rope_tables(seq):
    inv = ROPE_THETA ** (-jnp.arange(0, HEAD_DIM, 2, dtype=jnp.float32) / HEAD_DIM)
    ang = jnp.arange(seq, dtype=jnp.float32)[:, None] * inv[None, :]
    return jnp.cos(ang), jnp.sin(ang)


def apply_rope(x, cos, sin):
    half = HEAD_DIM // 2
    x1, x2 = x[..., :half], x[..., half:]
    return jnp.concatenate([x1 * cos - x2 * sin, x2 * cos + x1 * sin], axis=-1)


def swiglu(h, wg, wu, wd):
    return (jax.nn.silu(h @ wg) * (h @ wu)) @ wd


def moe_swiglu(h, router_w, router_b, wg, wu, wd):
    bsz, seq, d = h.shape
    tok = h.reshape(bsz * seq, d)
    logits = (tok @ router_w + router_b).astype(jnp.float32)
    top_val, top_idx = lax.top_k(logits, TOP_K)
    top_w = jax.nn.softmax(top_val, axis=-1)
    combine = jnp.sum(jax.nn.one_hot(top_idx, N_EXPERTS, dtype=jnp.float32) * top_w[..., None], axis=1)
    out = jnp.zeros((bsz * seq, d), jnp.float32)
    for e in range(N_EXPERTS):
        out = out + combine[:, e:e + 1] * swiglu(tok, wg[e], wu[e], wd[e]).astype(jnp.float32)
    return out.reshape(bsz, seq, d).astype(h.dtype)


def s5_mixer(u, lam_re, lam_im, log_step, b_re, b_im, c_re, c_im, d_skip, w_glu):
    bsz, seq, _ = u.shape
    f32 = jnp.float32
    uf = u.astype(f32).reshape(bsz, seq, S5_GROUPS, S5_GROUP)
    lam = lax.complex(lam_re.astype(f32), lam_im.astype(f32))
    step = jnp.exp(log_step.astype(f32))[:, None]
    a_bar = jnp.exp(lam * step)
    b_bar = ((a_bar - 1.0) / lam)[..., None] * lax.complex(b_re.astype(f32), b_im.astype(f32))
    bu = lax.complex(jnp.einsum('bsgc,gnc->bsgn', uf, jnp.real(b_bar)), jnp.einsum('bsgc,gnc->bsgn', uf, jnp.imag(b_bar)))
    a = jnp.broadcast_to(a_bar, bu.shape)
    _, hs = lax.associative_scan(linear_combine, (a, bu), axis=1)
    y = jnp.einsum('bsgn,gcn->bsgc', jnp.real(hs), c_re.astype(f32)) - jnp.einsum('bsgn,gcn->bsgc', jnp.imag(hs), c_im.astype(f32))
    y = (y + d_skip.astype(f32).reshape(S5_GROUPS, S5_GROUP) * uf).reshape(bsz, seq, S5_W)
    z = jax.nn.gelu(y)
    return (z * jax.nn.sigmoid(z @ w_glu.astype(f32))).astype(u.dtype)


def nsa_mixer(q, kc_tok, vc_tok, ks_tok, vs_tok, kw_tok, vw_tok, gate_logits, pe_k, pe_v, wk1, wk2, wv1, wv2, cos, sin):
    bsz, seq, _ = q.shape
    f32 = jnp.float32
    scale = HEAD_DIM ** -0.5
    qf = apply_rope(q.astype(f32).reshape(bsz, seq, NSA_HEADS, HEAD_DIM), cos[:, None, :], sin[:, None, :])
    kcf = apply_rope(kc_tok.astype(f32), cos, sin)
    ksf = apply_rope(ks_tok.astype(f32), cos, sin)
    kwf = apply_rope(kw_tok.astype(f32), cos, sin)
    vcf = vc_tok.astype(f32)
    vsf = vs_tok.astype(f32)
    vwf = vw_tok.astype(f32)
    t = np.arange(seq)

    n_cmp = (seq - L_CMP) // CMP_STRIDE + 1
    cmp_start = np.arange(n_cmp) * CMP_STRIDE
    cmp_idx = cmp_start[:, None] + np.arange(L_CMP)[None, :]

    def compress(tok, pe, w1, w2):
        blk = (tok[:, cmp_idx] + pe.astype(f32)).reshape(bsz, n_cmp, L_CMP * HEAD_DIM)
        return jax.nn.gelu(blk @ w1.astype(f32)) @ w2.astype(f32)

    k_cmp = compress(kcf, pe_k, wk1, wk2)
    v_cmp = compress(vcf, pe_v, wv1, wv2)
    cmp_mask = (cmp_start + L_CMP - 1)[None, :] <= t[:, None]
    p_cmp = masked_softmax(jnp.einsum('bshd,bnd->bhsn', qf, k_cmp) * scale, cmp_mask)
    o_cmp = jnp.einsum('bhsn,bnd->bshd', p_cmp, v_cmp)

    n_sel = seq // L_SEL
    sel_start = np.arange(n_sel) * L_SEL
    overlap = np.clip(np.minimum(cmp_start[:, None] + L_CMP, sel_start[None, :] + L_SEL) - np.maximum(cmp_start[:, None], sel_start[None, :]), 0, None) / L_CMP
    imp = jnp.einsum('bhsn,nj->bsj', p_cmp, jnp.asarray(overlap, f32))
    cur = t // L_SEL
    blk_id = np.arange(n_sel)[None, :]
    forced = (blk_id == 0) | (blk_id == cur[:, None]) | (blk_id == cur[:, None] - 1)
    future = sel_start[None, :] > t[:, None]
    imp = jnp.where(future, -1.0, jnp.where(forced, FORCED_SCORE, imp))
    n_top = min(TOP_N, n_sel)
    _, sel_idx = lax.top_k(imp, n_top)
    n_key = n_top * L_SEL
    kpos = (sel_idx[..., None] * L_SEL + jnp.arange(L_SEL, dtype=jnp.int32)).reshape(bsz, seq, n_key)
    n_qb = seq // Q_BLOCK
    q_blocks = qf.reshape(bsz, n_qb, Q_BLOCK, NSA_HEADS, HEAD_DIM)
    gather_rows = jax.vmap(lambda tok, ii: tok[ii])

    def sel_block(args):
        qq, kp, tt = args
        kg = gather_rows(ksf, kp)
        vg = gather_rows(vsf, kp)
        s = jnp.einsum('bqhd,bqkd->bhqk', qq, kg) * scale
        p = masked_softmax(s, (kp <= tt[None, :, None])[:, None])
        return jnp.einsum('bhqk,bqkd->bqhd', p, vg)

    o_sel = lax.map(sel_block, (q_blocks.transpose(1, 0, 2, 3, 4), kpos.reshape(bsz, n_qb, Q_BLOCK, n_key).transpose(1, 0, 2, 3), jnp.asarray(t.reshape(n_qb, Q_BLOCK), dtype=jnp.int32)))
    o_sel = o_sel.transpose(1, 0, 2, 3, 4).reshape(bsz, seq, NSA_HEADS, HEAD_DIM)

    n_wb = WINDOW // Q_BLOCK + 1

    def band(tok):
        padded = jnp.pad(tok, ((0, 0), (WINDOW, 0), (0, 0))).reshape(bsz, n_qb + n_wb - 1, Q_BLOCK, HEAD_DIM)
        return jnp.concatenate([padded[:, j:j + n_qb] for j in range(n_wb)], axis=2)

    k_band = band(kwf)
    v_band = band(vwf)
    key_pos = (np.arange(n_qb) * Q_BLOCK - WINDOW)[:, None] + np.arange(n_wb * Q_BLOCK)[None, :]
    dist = t.reshape(n_qb, Q_BLOCK)[:, :, None] - key_pos[:, None, :]
    win_mask = (dist >= 0) & (dist < WINDOW) & (key_pos[:, None, :] >= 0)
    p_win = masked_softmax(jnp.einsum('bnqhd,bnkd->bhnqk', q_blocks, k_band) * scale, win_mask)
    o_win = jnp.einsum('bhnqk,bnkd->bnqhd', p_win, v_band).reshape(bsz, seq, NSA_HEADS, HEAD_DIM)

    g = jax.nn.sigmoid(gate_logits.astype(f32)).reshape(bsz, seq, NSA_HEADS, 3)
    o = g[..., 0:1] * o_cmp + g[..., 1:2] * o_sel + g[..., 2:3] * o_win
    return o.reshape(bsz, seq, NSA_HEADS * HEAD_DIM).astype(q.dtype)


def rglru_mixer(x_in, gate_in, conv_w, conv_b, w_a, b_a, w_x, b_x, lam):
    bsz, seq, _ = x_in.shape
    f32 = jnp.float32
    xc = (causal_depthwise_conv(x_in, conv_w) + conv_b).astype(f32)
    xb = xc.reshape(bsz, seq, LRU_BLOCKS, LRU_BLOCK_W)
    r = jax.nn.sigmoid(jnp.einsum('bsnc,ncd->bsnd', xb, w_a.astype(f32)).reshape(bsz, seq, LRU_W) + b_a.astype(f32))
    i = jax.nn.sigmoid(jnp.einsum('bsnc,ncd->bsnd', xb, w_x.astype(f32)).reshape(bsz, seq, LRU_W) + b_x.astype(f32))
    log_a = -LRU_C * r * jax.nn.softplus(-lam.astype(f32))
    a = jnp.exp(log_a)
    b = jnp.sqrt(-jnp.expm1(2.0 * log_a)) * (i * xc)
    _, hs = lax.associative_scan(linear_combine, (a, b), axis=1)
    return (hs * jax.nn.gelu(gate_in.astype(f32))).astype(x_in.dtype)


def hybrid_mixer(h, w_in, s5_lambda_re, s5_lambda_im, s5_log_step, s5_b_re, s5_b_im, s5_c_re, s5_c_im, s5_d, s5_w_glu, nsa_pe_k, nsa_pe_v, nsa_cmp_k_w1, nsa_cmp_k_w2, nsa_cmp_v_w1, nsa_cmp_v_w2, lru_conv_w, lru_conv_b, lru_w_a, lru_b_a, lru_w_x, lru_b_x, lru_lambda, sc_conv_w, w_branch, w_out, cos, sin):
    z = h @ w_in
    points = np.cumsum(np.array(IN_SPLITS))[:-1].tolist()
    (u_s5, q, kc, vc, ks, vs, kw, vw, g_nsa, x_lru, g_lru, b_sc, c_sc, x_sc, g_merge) = jnp.split(z, points, axis=-1)
    y_a = s5_mixer(u_s5, s5_lambda_re, s5_lambda_im, s5_log_step, s5_b_re, s5_b_im, s5_c_re, s5_c_im, s5_d, s5_w_glu)
    y_b = nsa_mixer(q, kc, vc, ks, vs, kw, vw, g_nsa, nsa_pe_k, nsa_pe_v, nsa_cmp_k_w1, nsa_cmp_k_w2, nsa_cmp_v_w1, nsa_cmp_v_w2, cos, sin)
    y_c = rglru_mixer(x_lru, g_lru, lru_conv_w, lru_conv_b, lru_w_a, lru_b_a, lru_w_x, lru_b_x, lru_lambda)
    y_d = b_sc * causal_depthwise_conv(c_sc * x_sc, sc_conv_w)
    merged = None
    for m, y in enumerate((y_a, y_b, y_c, y_d)):
        term = jax.nn.sigmoid(g_merge[..., m * D_MODEL:(m + 1) * D_MODEL]) * (y @ w_branch[m])
        merged = term if merged is None else merged + term
    return merged @ w_out


def setup_inputs(seed: int = 0) -> dict:
    key = jax.random.key(seed)
    ks = iter(jax.random.split(key, 48))
    f32 = jnp.float32

    def nrm(shape, scale):
        return jax.random.normal(next(ks), shape, f32) * scale

    L = DEPTH
    D = D_MODEL
    n_dense = (DEPTH + 1) // 2
    n_moe = DEPTH // 2
    x = nrm((BATCH, SEQ, D), 1.0)
    c = nrm((BATCH, D), 1.0)
    mod_w = nrm((L, D, 6 * D), 0.5 * D ** -0.5)
    mod_b = nrm((L, 6 * D), 0.02)
    norm_mix_g = 1.0 + nrm((L, D), 0.05)
    norm_ffn_g = 1.0 + nrm((L, D), 0.05)
    w_in = nrm((L, D, IN_COLS), D ** -0.5)
    s5_lambda_re = -0.5 + nrm((L, S5_GROUPS, S5_STATE), 0.01)
    s5_lambda_im = math.pi * jnp.arange(S5_STATE, dtype=f32) + nrm((L, S5_GROUPS, S5_STATE), 0.01)
    s5_log_step = jax.random.uniform(next(ks), (L, S5_GROUPS), f32, math.log(1e-3), math.log(1e-1))
    s5_b_re = nrm((L, S5_GROUPS, S5_STATE, S5_GROUP), (2 * S5_GROUP) ** -0.5)
    s5_b_im = nrm((L, S5_GROUPS, S5_STATE, S5_GROUP), (2 * S5_GROUP) ** -0.5)
    s5_c_re = nrm((L, S5_GROUPS, S5_GROUP, S5_STATE), S5_STATE ** -0.5)
    s5_c_im = nrm((L, S5_GROUPS, S5_GROUP, S5_STATE), S5_STATE ** -0.5)
    s5_d = nrm((L, S5_W), 1.0)
    s5_w_glu = nrm((L, S5_W, S5_W), S5_W ** -0.5)
    nsa_pe_k = nrm((L, L_CMP, HEAD_DIM), 0.02)
    nsa_pe_v = nrm((L, L_CMP, HEAD_DIM), 0.02)
    nsa_cmp_k_w1 = nrm((L, L_CMP * HEAD_DIM, CMP_HIDDEN), (L_CMP * HEAD_DIM) ** -0.5)
    nsa_cmp_k_w2 = nrm((L, CMP_HIDDEN, HEAD_DIM), CMP_HIDDEN ** -0.5)
    nsa_cmp_v_w1 = nrm((L, L_CMP * HEAD_DIM, CMP_HIDDEN), (L_CMP * HEAD_DIM) ** -0.5)
    nsa_cmp_v_w2 = nrm((L, CMP_HIDDEN, HEAD_DIM), CMP_HIDDEN ** -0.5)
    lru_conv_w = nrm((L, LRU_CONV, LRU_W), LRU_CONV ** -0.5)
    lru_conv_b = nrm((L, LRU_W), 0.02)
    lru_w_a = nrm((L, LRU_BLOCKS, LRU_BLOCK_W, LRU_BLOCK_W), LRU_BLOCK_W ** -0.5)
    lru_b_a = nrm((L, LRU_W), 0.02)
    lru_w_x = nrm((L, LRU_BLOCKS, LRU_BLOCK_W, LRU_BLOCK_W), LRU_BLOCK_W ** -0.5)
    lru_b_x = nrm((L, LRU_W), 0.02)
    a_pow = jax.random.uniform(next(ks), (L, LRU_W), f32, 0.9, 0.999)
    a_base = a_pow ** (1.0 / LRU_C)
    lru_lambda = jnp.log(a_base) - jnp.log1p(-a_base)
    sc_conv_w = nrm((L, SC_CONV, SC_W), SC_CONV ** -0.5)
    w_branch = nrm((L, N_BRANCH, MIX_W, D), MIX_W ** -0.5)
    w_out = nrm((L, D, D), D ** -0.5)
    ffn_w_gate = nrm((n_dense, D, FFN_DIM), D ** -0.5)
    ffn_w_up = nrm((n_dense, D, FFN_DIM), D ** -0.5)
    ffn_w_down = nrm((n_dense, FFN_DIM, D), FFN_DIM ** -0.5)
    moe_router_w = nrm((n_moe, D, N_EXPERTS), D ** -0.5)
    moe_router_b = nrm((n_moe, N_EXPERTS), 0.01)
    moe_w_gate = nrm((n_moe, N_EXPERTS, D, EXPERT_DIM), D ** -0.5)
    moe_w_up = nrm((n_moe, N_EXPERTS, D, EXPERT_DIM), D ** -0.5)
    moe_w_down = nrm((n_moe, N_EXPERTS, EXPERT_DIM, D), EXPERT_DIM ** -0.5)
    final_norm_g = 1.0 + nrm((D,), 0.05)
    return {'x': x, 'c': c, 'mod_w': mod_w, 'mod_b': mod_b, 'norm_mix_g': norm_mix_g, 'norm_ffn_g': norm_ffn_g, 'w_in': w_in, 's5_lambda_re': s5_lambda_re, 's5_lambda_im': s5_lambda_im, 's5_log_step': s5_log_step, 's5_b_re': s5_b_re, 's5_b_im': s5_b_im, 's5_c_re': s5_c_re, 's5_c_im': s5_c_im, 's5_d': s5_d, 's5_w_glu': s5_w_glu, 'nsa_pe_k': nsa_pe_k, 'nsa_pe_v': nsa_pe_v, 'nsa_cmp_k_w1': nsa_cmp_k_w1, 'nsa_cmp_k_w2': nsa_cmp_k_w2, 'nsa_cmp_v_w1': nsa_cmp_v_w1, 'nsa_cmp_v_w2': nsa_cmp_v_w2, 'lru_conv_w': lru_conv_w, 'lru_conv_b': lru_conv_b, 'lru_w_a': lru_w_a, 'lru_b_a': lru_b_a, 'lru_w_x': lru_w_x, 'lru_b_x': lru_b_x, 'lru_lambda': lru_lambda, 'sc_conv_w': sc_conv_w, 'w_branch': w_branch, 'w_out': w_out, 'ffn_w_gate': ffn_w_gate, 'ffn_w_up': ffn_w_up, 'ffn_w_down': ffn_w_down, 'moe_router_w': moe_router_w, 'moe_router_b': moe_router_b, 'moe_w_gate': moe_w_gate, 'moe_w_up': moe_w_up, 'moe_w_down': moe_w_down, 'final_norm_g': final_norm_g}


def reference(x, c, mod_w, mod_b, norm_mix_g, norm_ffn_g, w_in, s5_lambda_re, s5_lambda_im, s5_log_step, s5_b_re, s5_b_im, s5_c_re, s5_c_im, s5_d, s5_w_glu, nsa_pe_k, nsa_pe_v, nsa_cmp_k_w1, nsa_cmp_k_w2, nsa_cmp_v_w1, nsa_cmp_v_w2, lru_conv_w, lru_conv_b, lru_w_a, lru_b_a, lru_w_x, lru_b_x, lru_lambda, sc_conv_w, w_branch, w_out, ffn_w_gate, ffn_w_up, ffn_w_down, moe_router_w, moe_router_b, moe_w_gate, moe_w_up, moe_w_down, final_norm_g):
    cos, sin = rope_tables(x.shape[1])
    c_act = jax.nn.silu(c)
    for i in range(DEPTH):
        mod = (c_act @ mod_w[i] + mod_b[i])[:, None, :]
        shift1, scale1, gate1, shift2, scale2, gate2 = jnp.split(mod, 6, axis=-1)
        h = rms_norm(x, norm_mix_g[i]) * (1.0 + scale1) + shift1
        mix = hybrid_mixer(h, w_in[i], s5_lambda_re[i], s5_lambda_im[i], s5_log_step[i], s5_b_re[i], s5_b_im[i], s5_c_re[i], s5_c_im[i], s5_d[i], s5_w_glu[i], nsa_pe_k[i], nsa_pe_v[i], nsa_cmp_k_w1[i], nsa_cmp_k_w2[i], nsa_cmp_v_w1[i], nsa_cmp_v_w2[i], lru_conv_w[i], lru_conv_b[i], lru_w_a[i], lru_b_a[i], lru_w_x[i], lru_b_x[i], lru_lambda[i], sc_conv_w[i], w_branch[i], w_out[i], cos, sin)
        x = (x + gate1 * mix).astype(x.dtype)
        h = rms_norm(x, norm_ffn_g[i]) * (1.0 + scale2) + shift2
        j = i // 2
        if i % 2 == 0:
            f = swiglu(h, ffn_w_gate[j], ffn_w_up[j], ffn_w_down[j])
        else:
            f = moe_swiglu(h, moe_router_w[j], moe_router_b[j], moe_w_gate[j], moe_w_up[j], moe_w_down[j])
        x = (x + gate2 * f).astype(x.dtype)
    return rms_norm(x, final_norm_g)
```

```python
import numpy as np
import concourse.bass as bass
import concourse.mybir as mybir
from concourse.bass_utils import run_bass_kernel_spmd

F32 = mybir.dt.float32
BF16 = mybir.dt.bfloat16
U32 = mybir.dt.uint32
AF = mybir.ActivationFunctionType
ALU = mybir.AluOpType
AX = mybir.AxisListType

EPOCH = 30000
NSLOT = 12


class K:
    def __init__(self, nc):
        self.nc = nc
        self.eng = {'pe': nc.tensor, 'act': nc.scalar, 'dve': nc.vector,
                    'pool': nc.gpsimd, 'sp': nc.sync}
        self.sem = {}
        self.cnt = {}
        self.known = {e: {} for e in self.eng}
        self.last_w = {}
        self.readers = {}
        self.vcs = {}
        self.epoch = {e: 0 for e in ('pe', 'act', 'dve', 'pool')}
        self.dma_i = {e: 0 for e in ('sp', 'pool', 'act')}
        self.nsem = 0
        self.ninst = 0
        self.out_events = []

    def _clock(self, name):
        if name not in self.sem:
            self.sem[name] = self.nc.alloc_semaphore("s_%s_%s" % (name[0], name[1]))
            self.cnt[name] = 0
            self.nsem += 1
        return name

    def _eclock(self, e):
        c = (e, self.epoch[e])
        self._clock(c)
        if self.cnt[c] >= EPOCH:
            self.epoch[e] += 1
            c = (e, self.epoch[e])
            self._clock(c)
        return c

    def _wait(self, e, deps):
        need = {}
        for (c, v) in deps:
            if v > need.get(c, 0):
                need[c] = v
        kn = self.known[e]
        for c, v in need.items():
            if e == 'pe' and c[0] == 'pe':
                continue
            if kn.get(c, 0) >= v:
                continue
            self.eng[e].wait_ge(self.sem[c], v)
            self.ninst += 1
            vc = self.vcs.get((c, v))
            if vc is not None:
                for c2, v2 in vc.items():
                    if kn.get(c2, 0) < v2:
                        kn[c2] = v2
            if kn.get(c, 0) < v:
                kn[c] = v

    def _deps(self, reads, writes):
        deps = set()
        for k in reads:
            w = self.last_w.get(k)
            if w is not None:
                deps.add(w)
        for k in writes:
            w = self.last_w.get(k)
            if w is not None:
                deps.add(w)
            r = self.readers.get(k)
            if r:
                for c, v in r.items():
                    deps.add((c, v))
        return deps

    def _record(self, ev, e, reads, writes):
        vc = dict(self.known[e])
        vc[ev[0]] = ev[1]
        self.vcs[ev] = vc
        for k in writes:
            self.last_w[k] = ev
            self.readers[k] = {}
        for k in reads:
            r = self.readers.setdefault(k, {})
            if r.get(ev[0], 0) < ev[1]:
                r[ev[0]] = ev[1]

    def op(self, e, fn, reads=(), writes=()):
        pr = [x for x in reads if isinstance(x, tuple) and x[0] == 'ps']
        if pr:
            writes = list(writes) + pr
        self._wait(e, self._deps(reads, writes))
        c = self._eclock(e)
        ins = fn()
        ins.then_inc(self.sem[c], 1)
        self.cnt[c] += 1
        self.ninst += 1
        self._record((c, self.cnt[c]), e, reads, writes)

    def dma(self, out, in_, reads=(), writes=(), q='sp', is_output=False, **kw):
        i = self.dma_i[q]
        self.dma_i[q] = i + 1
        slot = self._clock(('d' + q, i % NSLOT))
        deps = self._deps(reads, writes)
        if self.cnt[slot] > 0:
            deps.add((slot, self.cnt[slot]))
        self._wait(q, deps)
        ins = self.eng[q].dma_start(out=out, in_=in_, **kw)
        ins.then_inc(self.sem[slot], 16)
        self.cnt[slot] += 16
        self.ninst += 1
        ev = (slot, self.cnt[slot])
        self._record(ev, q, reads, writes)
        if is_output:
            self.out_events.append(ev)

    def finish(self):
        deps = set(self.out_events)
        for k, w in self.last_w.items():
            deps.add(w)
        self._wait('sp', deps)

    def barrier(self):
        allc = [(c, v) for c, v in self.cnt.items() if v > 0]
        for e in self.eng:
            self._wait(e, allc)


S = 4096
D = 1024
NT = 32
NEG = -30000.0
import ml_dtypes
from contextlib import ExitStack

WZ_U, WZ_Q, WZ_QS, WZ_K, WZ_KS, WZ_VC, WZ_LRU, WZ_SC, WZ_GM, WZ_END = 0, 256, 512, 768, 960, 1152, 1216, 1728, 2496, 6592


def _col(v, nt):
    return np.ascontiguousarray(np.asarray(v, np.float32).reshape(nt, 128).T)


def host_consts():
    c = {}
    c["ident"] = np.eye(128, dtype=np.float32)
    c["identb"] = np.eye(128, dtype=np.float32).astype(ml_dtypes.bfloat16)
    inv = (10000.0 ** (-np.arange(0, 64, 2, dtype=np.float32) / np.float32(64))).astype(np.float32)
    ang = (np.arange(S, dtype=np.float32)[:, None] * inv[None, :]).astype(np.float32)
    cs, sn = np.cos(ang).astype(np.float32).T, np.sin(ang).astype(np.float32).T
    c["ropeC"] = np.ascontiguousarray(np.concatenate([cs, cs], 0))
    c["ropeS"] = np.ascontiguousarray(np.concatenate([-sn, sn], 0))
    p = np.arange(128)[:, None, None]
    r = np.arange(4)[None, :, None]
    f = np.arange(512)[None, None, :]
    c["caus"] = np.where(128 * r + p <= f, 0.0, NEG).astype(ml_dtypes.bfloat16)
    c["wlow"] = np.where(128 * r + p > f, 0.0, NEG).astype(ml_dtypes.bfloat16)
    n = 128 * np.arange(2)[None, :, None] + p
    t = np.arange(S)[None, None, :]
    c["cmpb"] = np.where((n <= 254) & (16 * n + 31 <= t), 0.0, NEG).astype(ml_dtypes.bfloat16)
    j = np.arange(64)[:, None, None]
    kt = np.arange(32)[None, :, None]
    s_ = np.arange(128)[None, None, :]
    c["xpf"] = np.ascontiguousarray((j == 2 * kt + s_ // 64).astype(np.float32).astype(ml_dtypes.bfloat16).reshape(64, S))
    nn = np.arange(256)
    jj = np.arange(64)
    ov = np.clip(np.minimum(nn[:, None] * 16 + 32, jj[None, :] * 64 + 64) - np.maximum(nn[:, None] * 16, jj[None, :] * 64), 0, None) / 32.0
    ov[255] = 0
    c["ovl"] = np.ascontiguousarray(ov.reshape(2, 128, 64).transpose(1, 0, 2)).astype(np.float32)
    tt = np.arange(S)
    cur = tt // 64
    blk = np.arange(64)[None, :]
    forced = (blk == 0) | (blk == cur[:, None]) | (blk == cur[:, None] - 1)
    future = (blk * 64) > tt[:, None]
    A = (~forced & ~future).astype(np.float32)
    Bc = np.where(future, -1.0, np.where(forced, 1.0e4, 0.0)).astype(np.float32)
    c["impA"] = np.ascontiguousarray(A.reshape(32, 128, 64).transpose(1, 0, 2))
    c["impB"] = np.ascontiguousarray(Bc.reshape(32, 128, 64).transpose(1, 0, 2))
    c["iota"] = np.ascontiguousarray(np.broadcast_to(np.arange(513, dtype=np.float32), (128, 513)))
    return c


def host_layer(P, l):
    o = {}
    f32 = np.float32
    o["modw"] = P["mod_w"][l]
    o["modbc"] = _col(P["mod_b"][l], 48)
    o["modbr"] = np.ascontiguousarray(P["mod_b"][l].reshape(1, 6144))
    o["nmg"] = _col(P["norm_mix_g"][l], 8)
    o["nfg"] = _col(P["norm_ffn_g"][l], 8)
    w = P["w_in"][l]
    pts = np.cumsum((256, 256, 64, 64, 64, 64, 64, 64, 12, 256, 256, 256, 256, 256, 4096))[:-1]
    u, q, kc, vc, ks, vs, kw, vw, gn, xl, gl, bs, cs_, xs, gm = np.split(w, pts, axis=1)

    def sw(m):
        m4 = m.reshape(1024, -1, 2, 32)
        return m4[:, :, ::-1, :].reshape(1024, -1)
    k3 = np.concatenate([kc, ks, kw], 1)
    o["wz"] = np.ascontiguousarray(np.concatenate([u, q, sw(q), k3, sw(k3), vc, xl, gl, bs, cs_, xs, gm], 1))
    o["wzt"] = np.ascontiguousarray(np.concatenate([vc, vs, vw, gn], 1))
    def stcol(a):
        return np.ascontiguousarray(a.reshape(8, 128).T)
    o["s5lr"] = stcol(P["s5_lambda_re"][l])
    o["s5li"] = stcol(P["s5_lambda_im"][l])
    o["s5ls"] = stcol(np.repeat(P["s5_log_step"][l][:, None], 64, 1))
    Bp = np.zeros((2, 8, 128, 128), f32)
    Cp = np.zeros((2, 8, 128, 128), f32)
    for g in range(16):
        j, gl_ = g // 2, g % 2
        c0 = 32 * (j % 4) + gl_ * 16
        for ri, (bb, cc) in enumerate(((P["s5_b_re"][l], P["s5_c_re"][l]), (P["s5_b_im"][l], P["s5_c_im"][l]))):
            Bp[ri, j, c0:c0 + 16, gl_ * 64:gl_ * 64 + 64] = bb[g].T
            Cp[ri, j, gl_ * 64:gl_ * 64 + 64, c0:c0 + 16] = cc[g].T
    o["s5B"] = Bp
    o["s5C"] = Cp
    o["s5d"] = _col(P["s5_d"][l], 2)
    o["s5glu"] = P["s5_w_glu"][l]
    o["pekT"] = np.ascontiguousarray(P["nsa_pe_k"][l].T)
    o["pevT"] = np.ascontiguousarray(P["nsa_pe_v"][l].T)
    o["ckw1"] = P["nsa_cmp_k_w1"][l]
    o["ckw2"] = P["nsa_cmp_k_w2"][l]
    o["cvw1"] = P["nsa_cmp_v_w1"][l]
    o["cvw2"] = P["nsa_cmp_v_w2"][l]
    o["lrucw"] = np.ascontiguousarray(P["lru_conv_w"][l].reshape(4, 2, 128).transpose(2, 1, 0))
    o["lrucb"] = _col(P["lru_conv_b"][l], 2)
    for nm, src in (("lruwa", P["lru_w_a"][l]), ("lruwx", P["lru_w_x"][l])):
        m = np.zeros((2, 128, 128), f32)
        for nb in range(8):
            ct, bl = nb // 4, nb % 4
            m[ct, bl * 32:bl * 32 + 32, bl * 32:bl * 32 + 32] = src[nb]
        o[nm] = m
    o["lruba"] = _col(P["lru_b_a"][l], 2)
    o["lrubx"] = _col(P["lru_b_x"][l], 2)
    o["lrulam"] = _col(P["lru_lambda"][l], 2)
    o["sccw"] = np.ascontiguousarray(P["sc_conv_w"][l].reshape(3, 2, 128).transpose(2, 1, 0))
    o["wbr"] = P["w_branch"][l]
    o["wout"] = P["w_out"][l]
    return o


_UID = [0]


def _sbt(nc, name, shape, dtype):
    _UID[0] += 1
    return nc.sbuf_tensor("%s_u%d" % (name, _UID[0]), shape, dtype)


class Ring:
    def __init__(self, nc, es, name, shape, dtype, n):
        self.tiles = [es.enter_context(_sbt(nc, "sb_%s%d" % (name, i), shape, dtype)) for i in range(n)]
        self.keys = [(name, i) for i in range(n)]
        self.i = 0

    def next(self):
        j = self.i % len(self.tiles)
        self.i += 1
        return self.tiles[j], self.keys[j]


class Prog:
    def __init__(self, nc, dbg=()):
        self.nc = nc
        self.k = K(nc)
        self.dbg = set(dbg)
        self.din = {}
        self.dram = {}
        self.ps = [nc.alloc_psum_tensor("psb%d" % i, [128, 512], F32) for i in range(8)]
        self.psk = [('ps', i) for i in range(8)]
        self.alt = 0

    def inp(self, name, arr):
        shape = list(arr.shape)
        dt = BF16 if arr.dtype == ml_dtypes.bfloat16 else F32
        self.din[name] = nc_ap = self.nc.dram_tensor(name, shape, dt, kind="ExternalInput").ap()
        return nc_ap

    def scratch(self, name, shape, dt):
        kind = "ExternalOutput" if name in self.dbg else "Internal"
        t = self.nc.dram_tensor(name, shape, dt, kind=kind).ap()
        self.dram[name] = t
        return t

    def evac_engine(self):
        self.alt += 1
        return 'dve' if self.alt % 2 else 'act'

    def copy(self, e, out, in_, reads, writes):
        nc = self.nc
        if e == 'act':
            self.k.op('act', lambda: nc.scalar.copy(out=out, in_=in_), reads, writes)
        elif e == 'dve':
            self.k.op('dve', lambda: nc.vector.tensor_copy(out=out, in_=in_), reads, writes)
        else:
            self.k.op('pool', lambda: nc.gpsimd.tensor_copy(out=out, in_=in_), reads, writes)


def stage_mod(pg, es, L, ccol_d, lay):
    nc, k = pg.nc, pg.k
    vcol = es.enter_context(_sbt(nc, "sb_vcol", [128, L, 4, 8], F32))
    grow = es.enter_context(_sbt(nc, "sb_grow", [128, L, 2, 1024], F32))
    with ExitStack() as st:
        cc = st.enter_context(_sbt(nc, "sb_m_cc", [128, 8], F32))
        sc = st.enter_context(_sbt(nc, "sb_m_sc", [128, 8], F32))
        cb = st.enter_context(_sbt(nc, "sb_m_cb", [128, 8, 128], F32))
        mw = Ring(nc, st, "m_mw", [128, 8, 1024], F32, 2)
        mbc = st.enter_context(_sbt(nc, "sb_m_mbc", [128, 48], F32))
        mbr = st.enter_context(_sbt(nc, "sb_m_mbr", [128, 1024], F32))
        ng = st.enter_context(_sbt(nc, "sb_m_ng", [128, 8], F32))
        raw = st.enter_context(_sbt(nc, "sb_m_raw", [128, 6, 8], F32))
        rowt = st.enter_context(_sbt(nc, "sb_m_rowt", [128, 1024], F32))
        k.dma(cc[:], ccol_d, writes=['m_cc'])
        k.op('act', lambda: nc.scalar.activation(out=sc[:], in_=cc[:], func=AF.Silu), ['m_cc'], ['m_sc'])
        for kc in range(8):
            k.op('dve', lambda kc=kc: nc.vector.tensor_copy(out=cb[:, kc, :], in_=sc[:, kc:kc + 1].to_broadcast([128, 128])), ['m_sc'], [('m_cb', kc)])
        for l in range(L):
            d = lay[l]
            k.dma(mbc[:], d["modbc"], writes=['m_mbc'])
            for v in range(6):
                wt, wk = mw.next()
                k.dma(wt[:], d["modw"].rearrange("(kc p) n -> p kc n", p=128)[:, :, v * 1024:(v + 1) * 1024], writes=[wk])
                if v in (2, 5):
                    gi = 0 if v == 2 else 1
                    k.dma(mbr[:], d["modbr"][:, v * 1024:(v + 1) * 1024].to_broadcast([128, 1024]), writes=['m_mbr'])
                    for hf in range(2):
                        ps, pk = pg.ps[hf], pg.psk[hf]
                        for kc in range(8):
                            k.op('pe', lambda kc=kc, ps=ps, hf=hf, wt=wt: nc.tensor.matmul(ps[:, :], lhsT=cb[:, kc, :], rhs=wt[:, kc, hf * 512:(hf + 1) * 512], start=(kc == 0), stop=(kc == 7)),
                                 [wk, ('m_cb', kc)], [pk])
                        k.op('dve', lambda ps=ps, hf=hf, gi=gi, l=l: nc.vector.tensor_tensor(out=grow[:, l, gi, hf * 512:(hf + 1) * 512], in0=ps[:, :], in1=mbr[:, hf * 512:(hf + 1) * 512], op=ALU.add),
                             [pk, 'm_mbr'], [('grow', l, gi, hf)])
                else:
                    for hf in range(2):
                        ps, pk = pg.ps[2 + hf], pg.psk[2 + hf]
                        for kc in range(8):
                            k.op('pe', lambda kc=kc, ps=ps, hf=hf, wt=wt: nc.tensor.matmul(ps[:, :], lhsT=cb[:, kc, :], rhs=wt[:, kc, hf * 512:(hf + 1) * 512], start=(kc == 0), stop=(kc == 7)),
                                 [wk, ('m_cb', kc)], [pk])
                        if hf == 0:
                            k.op('dve', lambda ps=ps: nc.vector.tensor_copy(out=rowt[:, 0:512], in_=ps[:, :]), [pk], [('m_rowt', 0)])
                        else:
                            k.op('act', lambda ps=ps: nc.scalar.copy(out=rowt[:, 512:1024], in_=ps[:, :]), [pk], [('m_rowt', 1)])
                    for b_ in range(2):
                        ps, pk = pg.ps[6 + b_], pg.psk[6 + b_]
                        for j in range(4):
                            kc = 4 * b_ + j
                            k.op('pe', lambda kc=kc, j=j, ps=ps: nc.tensor.transpose(out=ps[:, j * 128:(j + 1) * 128], in_=rowt[:, kc * 128:(kc + 1) * 128], identity=pg.ident[:]),
                                 [('m_rowt', b_), 'ident'], [pk])
                        k.op('dve', lambda ps=ps, v=v, b_=b_: nc.vector.tensor_tensor(out=raw[:, v, 4 * b_:4 * b_ + 4], in0=ps[:, 0:512:128], in1=mbc[:, v * 8 + 4 * b_:v * 8 + 4 * b_ + 4], op=ALU.add),
                             [pk, 'm_mbc'], [('m_raw', v, b_)])
            for half, (vs, vsh, gname) in enumerate(((1, 0, "nmg"), (4, 3, "nfg"))):
                k.dma(ng[:], d[gname], writes=['m_ng'])
                rk_s = [('m_raw', vs, 0), ('m_raw', vs, 1)]
                rk_h = [('m_raw', vsh, 0), ('m_raw', vsh, 1)]
                k.op('dve', lambda vs=vs: nc.vector.tensor_scalar(out=raw[:, vs, :], in0=raw[:, vs, :], scalar1=1.0, scalar2=None, op0=ALU.add), rk_s, rk_s)
                k.op('dve', lambda vs=vs, half=half, l=l: nc.vector.tensor_tensor(out=vcol[:, l, 2 * half, :], in0=raw[:, vs, :], in1=ng[:], op=ALU.mult), rk_s + ['m_ng'], [('vcol', l, 2 * half)])
                k.op('dve', lambda vsh=vsh, half=half, l=l: nc.vector.tensor_copy(out=vcol[:, l, 2 * half + 1, :], in_=raw[:, vsh, :]), rk_h, [('vcol', l, 2 * half + 1)])
        k.barrier()
    return vcol, grow


def norm_tiles(pg, rings, xsrc, tts, hT, hcol0, gm, sh, gmk, shk, hkeyf, router=None):
    for _ in norm_tiles_gen(pg, rings, xsrc, tts, hT, hcol0, gm, sh, gmk, shk, hkeyf, router=router):
        pass


def norm_tiles_gen(pg, rings, xsrc, tts, hT, hcol0, gm, sh, gmk, shk, hkeyf, router=None, pbase=0):
    nc, k = pg.nc, pg.k
    xr, sqr, xnr, str_ = rings["x"], rings["sq"], rings["xn"], rings["st"]
    tts = list(tts)
    n = len(tts)
    stt = {}

    def part1(i):
        tt = tts[i]
        xt, xk = xr.next()
        k.dma(xt[:], xsrc[tt * 128:(tt + 1) * 128, :], writes=[xk])
        sq, sqk = sqr.next()
        st, stk = str_.next()
        k.op('act', lambda: nc.scalar.activation(out=sq[:], in_=xt[:], func=AF.Square, accum_out=st[:, 0:1]), [xk], [sqk, (stk, 0)])
        k.op('dve', lambda: nc.vector.tensor_scalar(out=st[:, 1:2], in0=st[:, 0:1], scalar1=1.0 / D, scalar2=1e-6, op0=ALU.mult, op1=ALU.add), [(stk, 0)], [(stk, 1)])
        k.op('act', lambda: nc.scalar.activation(out=st[:, 2:3], in_=st[:, 1:2], func=AF.Sqrt), [(stk, 1)], [(stk, 2)])
        k.op('dve', lambda: nc.vector.reciprocal(out=st[:, 3:4], in_=st[:, 2:3]), [(stk, 2)], [(stk, 3)])
        xn, xnk = xnr.next()
        k.op('dve', lambda: nc.vector.tensor_scalar(out=xn[:], in0=xt[:], scalar1=st[:, 3:4], scalar2=None, op0=ALU.mult), [xk, (stk, 3)], [xnk])
        stt[i] = dict(xn=(xn, xnk))

    def part2(i):
        xn, xnk = stt[i]["xn"]
        c0 = hcol0 + i * 128
        h32 = None
        if router is not None:
            h32, h32k = router["h32"].next()
            stt[i]["h32"] = (h32, h32k)
        for half in range(2):
            pb = pbase + (2 * i + half) % 4
            ps, pk = pg.ps[pb], pg.psk[pb]
            for j in range(4):
                kc = half * 4 + j
                k.op('pe', lambda: nc.tensor.transpose(out=ps[:, j * 128:(j + 1) * 128], in_=xn[:, kc * 128:(kc + 1) * 128], identity=pg.ident[:]), [xnk, 'ident'], [pk])
            for j in range(4):
                kc = half * 4 + j
                e = 'dve' if half == 0 else 'act'
                dst = hT[:, kc, c0:c0 + 128] if router is None else h32[:, kc, :]
                dkey = hkeyf(kc, c0) if router is None else (h32k, kc)
                if e == 'dve':
                    k.op('dve', lambda: nc.vector.tensor_scalar(out=dst, in0=ps[:, j * 128:(j + 1) * 128], scalar1=gm[:, kc:kc + 1], scalar2=sh[:, kc:kc + 1], op0=ALU.mult, op1=ALU.add),
                         [pk, gmk, shk], [dkey])
                else:
                    k.op('act', lambda: nc.scalar.activation(out=dst, in_=ps[:, j * 128:(j + 1) * 128], func=AF.Identity, bias=sh[:, kc:kc + 1], scale=gm[:, kc:kc + 1]),
                         [pk, gmk, shk], [dkey])
                if router is not None:
                    k.op('pool', lambda: nc.gpsimd.tensor_copy(out=hT[:, kc, c0:c0 + 128], in_=h32[:, kc, :]), [(h32k, kc)], [hkeyf(kc, c0)])

    def part3(i):
        if router is None:
            del stt[i]
            return
        h32, h32k = stt[i]["h32"]
        ps, pk = pg.ps[4 + (i % 2)], pg.psk[4 + (i % 2)]
        for kc in range(8):
            k.op('pe', lambda: nc.tensor.matmul(ps[:, 0:8], lhsT=h32[:, kc, :], rhs=router["w"][:, kc, :], start=(kc == 0), stop=(kc == 7)), [(h32k, kc), 'rw'], [pk])
        k.op('dve', lambda: nc.vector.tensor_tensor(out=router["logits"][:, i, :], in0=ps[:, 0:8], in1=router["b"][:, :], op=ALU.add), [pk, 'rb'], [('logits', i)])
        del stt[i]

    for s_ in range(n + 2):
        if s_ < n:
            part1(s_)
        if 0 <= s_ - 1 < n:
            part2(s_ - 1)
        if 0 <= s_ - 2 < n:
            part3(s_ - 2)
        yield s_


def mk_norm_rings(nc, es):
    return {"x": Ring(nc, es, "n_x", [128, 1024], F32, 2), "sq": Ring(nc, es, "n_sq", [128, 1024], F32, 1),
            "xn": Ring(nc, es, "n_xn", [128, 1024], F32, 2), "st": Ring(nc, es, "n_st", [128, 4], F32, 3)}


def stage_proj(pg, hT, hkey, d, sc):
    nc, k = pg.nc, pg.k
    wz = d["wz"].rearrange("(kc p) n -> p kc n", p=128)
    with ExitStack() as es:
        wr = Ring(nc, es, "z_w", [128, 8, 512], BF16, 2)
        o32 = Ring(nc, es, "z_o32", [128, 512], F32, 3)
        o16 = Ring(nc, es, "z_o16", [128, 512], BF16, 3)
        t32 = Ring(nc, es, "z_t32", [128, 512], F32, 4)
        rC = es.enter_context(_sbt(nc, "sb_z_rC", [128, S], F32))
        rS = es.enter_context(_sbt(nc, "sb_z_rS", [128, S], F32))
        for hh in range(2):
            k.dma(rC[hh * 64:(hh + 1) * 64, :], pg.din["ropeC"], writes=[('rC', hh)])
            k.dma(rS[hh * 64:(hh + 1) * 64, :], pg.din["ropeS"], writes=[('rS', hh)])
        rCk = [('rC', 0), ('rC', 1)]
        rSk = [('rS', 0), ('rS', 1)]
        psi = [0]

        def mm(wt, wk, c0, m, qc, pb=None):
            if pb is None:
                pb = psi[0] % 6
                psi[0] += 1
            ps, pk = pg.ps[pb], pg.psk[pb]
            for kc in range(8):
                k.op('pe', lambda kc=kc: nc.tensor.matmul(ps[0:m, :], lhsT=wt[:, kc, c0:c0 + m], rhs=hT[:, kc, qc * 512:(qc + 1) * 512], start=(kc == 0), stop=(kc == 7)),
                     [wk] + [hkey(kc, qc * 512 + j * 128) for j in range(4)], [pk])
            return ps, pk

        def load(c0, n):
            wt, wk = wr.next()
            k.dma(wt[:, :, 0:n], wz[:, :, c0:c0 + n], writes=[wk], q='pool')
            return wt, wk

        for (name, c0, n) in (("zu", WZ_U, 256), ("zlru", WZ_LRU, 512), ("zsc", WZ_SC, 384), ("zsc", WZ_SC + 384, 384)):
            wt, wk = load(c0, n)
            r0 = 384 if (name == "zsc" and c0 != WZ_SC) else 0
            for qc in range(8):
                for t in range(n // 128):
                    ps, pk = mm(wt, wk, t * 128, 128, qc)
                    ot, ok = o32.next()
                    pg.copy(pg.evac_engine(), ot[:], ps[:, :], [pk], [ok])
                    k.dma(sc[name][r0 + t * 128:r0 + (t + 1) * 128, qc * 512:(qc + 1) * 512], ot[:], reads=[ok])
        for (name, cm, cs, nh) in (("zq", WZ_Q, WZ_QS, 4), ("zk", WZ_K, WZ_KS, 3)):
            wt, wk = load(cm, 512 if nh == 4 else 448)
            dstv = sc[name].rearrange("h p t -> (h p) t")
            units = [(0, 128), (128, 128)] if nh == 4 else [(0, 128), (128, 64)]
            for qc in range(8):
                for (c0, m) in units:
                    psA, pkA = mm(wt, wk, c0, m, qc)
                    psB, pkB = mm(wt, wk, (cs - cm) + c0, m, qc)
                    ta, tak = t32.next()
                    tb, tbk = t32.next()
                    k.op('dve', lambda: nc.vector.tensor_tensor(out=ta[0:m, :], in0=psA[0:m, :], in1=rC[0:m, qc * 512:(qc + 1) * 512], op=ALU.mult), [pkA] + rCk, [tak])
                    k.op('dve', lambda: nc.vector.tensor_tensor(out=tb[0:m, :], in0=psB[0:m, :], in1=rS[0:m, qc * 512:(qc + 1) * 512], op=ALU.mult), [pkB] + rSk, [tbk])
                    ot, ok = o16.next()
                    k.op('pool', lambda: nc.gpsimd.tensor_tensor(out=ot[0:m, :], in0=ta[0:m, :], in1=tb[0:m, :], op=ALU.add), [tak, tbk], [ok])
                    k.dma(dstv[c0:c0 + m, qc * 512:(qc + 1) * 512], ot[0:m, :], reads=[ok])
                if nh == 3:
                    ps, pk = mm(wt, wk, WZ_VC - WZ_K, 64, qc)
                    ot, ok = o16.next()
                    pg.copy(pg.evac_engine(), ot[0:64, :], ps[0:64, :], [pk], [ok])
                    k.dma(sc["zvc"][:, qc * 512:(qc + 1) * 512], ot[0:64, :], reads=[ok])
        for g in range(8):
            wt, wk = load(WZ_GM + g * 512, 512)
            for qc in range(8):
                for t in range(4):
                    ps, pk = mm(wt, wk, t * 128, 128, qc)
                    ot, ok = o16.next()
                    k.op('act', lambda: nc.scalar.activation(out=ot[:], in_=ps[:, :], func=AF.Sigmoid), [pk], [ok])
                    r = g * 512 + t * 128
                    k.dma(sc["zgm"][r:r + 128, qc * 512:(qc + 1) * 512], ot[:], reads=[ok])
        wt, wk = wr.next()
        k.dma(wt[:, :, 0:204], d["wzt"].rearrange("(kc p) n -> p kc n", p=128), writes=[wk], q='pool')
        for tt in range(NT):
            pb = 6 + tt % 2
            ps, pk = pg.ps[pb], pg.psk[pb]
            for kc in range(8):
                k.op('pe', lambda kc=kc: nc.tensor.matmul(ps[:, 0:204], lhsT=hT[:, kc, tt * 128:(tt + 1) * 128], rhs=wt[:, kc, 0:204], start=(kc == 0), stop=(kc == 7)),
                     [wk, hkey(kc, tt * 128)], [pk])
            ot, ok = o16.next()
            pg.copy('dve', ot[:, 0:192], ps[:, 0:192], [pk], [ok])
            k.dma(sc["zv"][tt * 128:(tt + 1) * 128, :], ot[:, 0:192], reads=[ok])
            og, ogk = o32.next()
            k.op('act', lambda: nc.scalar.activation(out=og[:, 0:12], in_=ps[:, 192:204], func=AF.Sigmoid), [pk], [ogk])
            k.dma(sc["zg"][tt * 128:(tt + 1) * 128, :], og[:, 0:12], reads=[ogk])
        k.barrier()


def build(host_in, stages, dbg=()):
    nc = bass.Bass("TRN2", target_bir_lowering=False)
    pg = Prog(nc, dbg)
    k = pg.k
    for name, arr in host_in.items():
        pg.inp(name, arr)
    L = 2
    lay = [{kk[3:]: v for kk, v in pg.din.items() if kk.startswith("L%d_" % l)} for l in range(L)]
    sc = {}
    for name, shape, dt in (("xa", [S, D], F32), ("xb", [S, D], F32), ("zu", [256, S], F32), ("zq", [4, 64, S], BF16),
                            ("zk", [3, 64, S], BF16), ("zvc", [64, S], BF16), ("zv", [S, 192], BF16), ("zg", [S, 12], F32),
                            ("zlru", [512, S], F32), ("zsc", [768, S], F32), ("zgm", [4096, S], BF16), ("yT", [4, 256, S], BF16)):
        sc[name] = pg.scratch(name, shape, dt)
    out_d = nc.dram_tensor("out", [S, D], F32, kind="ExternalOutput").ap()
    with ExitStack() as es:
        pg.ident = es.enter_context(_sbt(nc, "sb_ident", [128, 128], F32))
        pg.identb = es.enter_context(_sbt(nc, "sb_identb", [128, 128], BF16))
        k.dma(pg.ident[:], pg.din["ident"], writes=['ident'])
        k.dma(pg.identb[:], pg.din["identb"], writes=['identb'])
        vcol, grow = stage_mod(pg, es, L, pg.din["ccol"], lay)
        xsrc = pg.din["x"]
        if "vcol" in pg.dbg:
            dv = nc.dram_tensor("vcol", [128, L, 4, 8], F32, kind="ExternalOutput").ap()
            dg = nc.dram_tensor("grow", [128, L, 2, 1024], F32, kind="ExternalOutput").ap()
            k.dma(dv, vcol[:], reads=[('vcol', l_, i_) for l_ in range(L) for i_ in range(4)], is_output=True)
            k.dma(dg, grow[:], reads=[('grow', l_, g_, h_) for l_ in range(L) for g_ in range(2) for h_ in range(2)], is_output=True)
        for l in range(L):
            if ("stop", l, "mod") in stages:
                break
            d = lay[l]
            with ExitStack() as ls:
                hT = ls.enter_context(_sbt(nc, "sb_hT", [128, 8, S], BF16))
                hkey = lambda kc, c: ('hT', kc, c // 128)
                with ExitStack() as ns:
                    rings = mk_norm_rings(nc, ns)
                    norm_tiles(pg, rings, xsrc, range(NT), hT, 0, vcol[:, l, 0, :], vcol[:, l, 1, :], ('vcol', l, 0), ('vcol', l, 1), hkey)
                    k.barrier()
                if "hT" in pg.dbg and l == 0:
                    dh = nc.dram_tensor("hT", [128, 8, S], BF16, kind="ExternalOutput").ap()
                    k.dma(dh, hT[:], is_output=True)
                    k.barrier()
                if ("stop", l, "norm") in stages:
                    break
                stage_proj(pg, hT, hkey, d, sc)
            if ("stop", l, "proj") in stages:
                break
            xmid = sc["xa"]
            xnext = sc["xb"]
            if "s5" not in stages.get("skip", ()) if isinstance(stages, dict) else True:
                pass
            skip = SKIP
            if "s5" not in skip:
                stage_s5(pg, d, sc)
            if "nsa" not in skip:
                stage_nsa(pg, d, sc)
            if "lru" not in skip:
                stage_lru(pg, d, sc)
            if "sc" not in skip:
                stage_sc(pg, d, sc)
            if ("stop", l, "mix") in stages:
                break
            stage_merge(pg, d, sc, xsrc, xmid, grow[:, l, :, :])
            if ("stop", l, "merge") in stages:
                break
            if l % 2 == 0:
                ex = [(pg.din["ffn_wg"], pg.din["ffn_wu"], pg.din["ffn_wd"])]
                stage_ffn(pg, xmid, xnext, vcol[:, l, 2, :], vcol[:, l, 3, :], ('vcol', l, 2), ('vcol', l, 3), grow[:, l, 1, :], ex, 2816)
            else:
                ex = [(pg.din["moe_wg"][e], pg.din["moe_wu"][e], pg.din["moe_wd"][e]) for e in range(8)]
                stage_ffn(pg, xmid, xnext, vcol[:, l, 2, :], vcol[:, l, 3, :], ('vcol', l, 2), ('vcol', l, 3), grow[:, l, 1, :], ex, 1408,
                          router={"w": pg.din["moe_rw"], "b": pg.din["moe_rb"]}, final=((out_d, pg.din["fin_g"]) if l == L - 1 else None))
            xsrc = xnext
            sc["xa"], sc["xb"] = sc["xb"], sc["xa"]
            sc["xa"], sc["xb"] = sc["xb"], sc["xa"]
            if ("stop", l, "ffn") in stages:
                break
        k.barrier()
    k.finish()
    return nc, pg


SKIP = set()

I32 = mybir.dt.int32
TWO_PI = float(2 * np.pi)


def _wrap_half(pg, f, m, n_reads, key):
    nc, k = pg.nc, pg.k
    k.op('dve', lambda: nc.vector.tensor_scalar(out=m, in0=f, scalar1=0.5, scalar2=None, op0=ALU.is_gt), [key], ['s_MK'])
    k.op('dve', lambda: nc.vector.tensor_tensor(out=f, in0=f, in1=m, op=ALU.subtract), [key, 's_MK'], [key])
    k.op('dve', lambda: nc.vector.tensor_scalar(out=m, in0=f, scalar1=-0.5, scalar2=None, op0=ALU.is_lt), [key], ['s_MK'])
    k.op('dve', lambda: nc.vector.tensor_tensor(out=f, in0=f, in1=m, op=ALU.add), [key, 's_MK'], [key])


S5_ENG = ['dve', 'dve', 'dve', 'dve', 'dve']


def S5_E(nc, i):
    return nc.vector if S5_ENG[i] == 'dve' else nc.gpsimd


def stage_s5(pg, d, sc):
    nc, k = pg.nc, pg.k
    TC = 512
    NTAB = 513
    with ExitStack() as es:
        sbt = lambda name, shape, dt=F32: es.enter_context(_sbt(nc, "sb_s_" + name, shape, dt))
        uT = sbt("uT", [128, 2, S], BF16)
        k.dma(uT[:], sc["zu"].rearrange("(ct p) t -> p ct t", p=128), writes=['s_uT'], q='pool')
        Bw = sbt("Bw", [128, 2, 8, 128], BF16)
        Cw = sbt("Cw", [128, 2, 8, 128], BF16)
        k.dma(Bw[:], d["s5B"].rearrange("r j p n -> p r j n"), writes=['s_Bw'], q='pool')
        k.dma(Cw[:], d["s5C"].rearrange("r j p n -> p r j n"), writes=['s_Cw'], q='pool')
        k.op('pool', lambda: nc.gpsimd.tensor_scalar(out=Cw[:, 1, :, :], in0=Cw[:, 1, :, :], scalar1=-1.0, scalar2=None, op0=ALU.mult), ['s_Cw'], ['s_Cw'])
        NCw = sbt("NCw", [128, 8, 128], BF16)
        k.op('pool', lambda: nc.gpsimd.tensor_scalar(out=NCw[:], in0=Cw[:, 0, :, :], scalar1=-1.0, scalar2=None, op0=ALU.mult), ['s_Cw'], ['s_NCw'])
        nid = sbt("nid", [128, 128], BF16)
        k.op('pool', lambda: nc.gpsimd.tensor_scalar(out=nid[:], in0=pg.identb[:], scalar1=-1.0, scalar2=None, op0=ALU.mult), ['identb'], ['s_nid'])
        glu = sbt("glu", [128, 2, 256], BF16)
        k.dma(glu[:], d["s5glu"].rearrange("(ct p) n -> p ct n", p=128), writes=['s_glu'], q='pool')
        dcol = sbt("dcol", [128, 2])
        k.dma(dcol[:], d["s5d"], writes=['s_d'])
        pr = sbt("pr", [128, 16, 8])
        LR, LI, LS, DT, AR, TH, R_, THN, KR, KI, ER, EI, NEI, T1, T2, T3 = range(16)
        k.dma(pr[:, LR, :], d["s5lr"], writes=[('s_pr', LR)])
        k.dma(pr[:, LI, :], d["s5li"], writes=[('s_pr', LI)])
        k.dma(pr[:, LS, :], d["s5ls"], writes=[('s_pr', LS)])
        MRb = sbt("MRb", [128, 8, TC], BF16)
        MIb = sbt("MIb", [128, 8, TC], BF16)
        COSb = sbt("COSb", [128, 8, TC], BF16)
        SINb = sbt("SINb", [128, 8, TC], BF16)
        RT = sbt("RT", [128, 8, TC])

        def P_(i):
            return pr[:, i, :]

        def sm(op_, o, a, b_):
            k.op('dve', lambda: nc.vector.tensor_tensor(out=P_(o), in0=P_(a), in1=P_(b_), op=op_), [('s_pr', a), ('s_pr', b_)], [('s_pr', o)])
        k.op('act', lambda: nc.scalar.activation(out=P_(DT), in_=P_(LS), func=AF.Exp), [('s_pr', LS)], [('s_pr', DT)])
        sm(ALU.mult, AR, LR, DT)
        sm(ALU.mult, TH, LI, DT)
        k.op('act', lambda: nc.scalar.activation(out=P_(R_), in_=P_(AR), func=AF.Exp), [('s_pr', AR)], [('s_pr', R_)])
        k.op('dve', lambda: nc.vector.tensor_scalar(out=P_(THN), in0=P_(TH), scalar1=1.0 / TWO_PI, scalar2=None, op0=ALU.mult), [('s_pr', TH)], [('s_pr', THN)])
        with ExitStack() as ts:
            tbt = lambda name, shape, dt=F32: ts.enter_context(_sbt(nc, "sb_st_" + name, shape, dt))
            SIN = tbt("SIN", [128, 8, NTAB])
            COS = tbt("COS", [128, 8, NTAB])
            iot = tbt("iot", [128, NTAB])
            k.dma(iot[:], pg.din["iota"], writes=['s_iot'])
            FR = tbt("FR", [128, 8, NTAB])
            FC = tbt("FC", [128, 8, NTAB])
            MK = tbt("MK", [128, 8, NTAB])
            KI32 = tbt("ki", [128, 8, NTAB], I32)
            for j in range(8):
                k.op('dve', lambda: nc.vector.tensor_scalar(out=FR[:, j, :], in0=iot[:], scalar1=pr[:, THN, j:j + 1], scalar2=None, op0=ALU.mult), ['s_iot', ('s_pr', THN)], ['s_FR'])
            k.op('dve', lambda: nc.vector.tensor_copy(out=KI32[:], in_=FR[:]), ['s_FR'], ['s_KI'])
            k.op('dve', lambda: nc.vector.tensor_copy(out=MK[:], in_=KI32[:]), ['s_KI'], ['s_MK'])
            k.op('dve', lambda: nc.vector.tensor_tensor(out=FR[:], in0=FR[:], in1=MK[:], op=ALU.subtract), ['s_FR', 's_MK'], ['s_FR'])
            k.op('dve', lambda: nc.vector.tensor_scalar(out=FC[:], in0=FR[:], scalar1=0.25, scalar2=None, op0=ALU.add), ['s_FR'], ['s_FC'])
            _wrap_half(pg, FR[:], MK[:], None, 's_FR')
            _wrap_half(pg, FC[:], MK[:], None, 's_FC')
            k.op('act', lambda: nc.scalar.activation(out=SIN[:], in_=FR[:], func=AF.Sin, scale=TWO_PI), ['s_FR'], ['s_SIN'])
            k.op('act', lambda: nc.scalar.activation(out=COS[:], in_=FC[:], func=AF.Sin, scale=TWO_PI), ['s_FC'], ['s_COS'])
            k.op('dve', lambda: nc.vector.tensor_tensor(out=P_(T1), in0=P_(R_), in1=COS[:, :, 1], op=ALU.mult), [('s_pr', R_), 's_COS'], [('s_pr', T1)])
            k.op('dve', lambda: nc.vector.tensor_tensor(out=P_(T2), in0=P_(R_), in1=SIN[:, :, 1], op=ALU.mult), [('s_pr', R_), 's_SIN'], [('s_pr', T2)])
            k.op('dve', lambda: nc.vector.tensor_scalar(out=P_(T1), in0=P_(T1), scalar1=-1.0, scalar2=None, op0=ALU.add), [('s_pr', T1)], [('s_pr', T1)])
            sm(ALU.mult, T3, LR, LR)
            sm(ALU.mult, KR, LI, LI)
            sm(ALU.add, T3, T3, KR)
            k.op('dve', lambda: nc.vector.reciprocal(out=P_(T3), in_=P_(T3)), [('s_pr', T3)], [('s_pr', T3)])
            sm(ALU.mult, KR, T1, LR)
            sm(ALU.mult, KI, T2, LI)
            sm(ALU.add, KR, KR, KI)
            sm(ALU.mult, KR, KR, T3)
            sm(ALU.mult, KI, T2, LR)
            sm(ALU.mult, ER, T1, LI)
            sm(ALU.subtract, KI, KI, ER)
            sm(ALU.mult, KI, KI, T3)
            k.op('dve', lambda: nc.vector.tensor_copy(out=P_(ER), in_=COS[:, :, 512]), ['s_COS', ('s_pr', ER)], [('s_pr', ER)])
            k.op('dve', lambda: nc.vector.tensor_copy(out=P_(EI), in_=SIN[:, :, 512]), ['s_SIN'], [('s_pr', EI)])
            k.op('dve', lambda: nc.vector.tensor_scalar(out=P_(NEI), in0=P_(EI), scalar1=-1.0, scalar2=None, op0=ALU.mult), [('s_pr', EI)], [('s_pr', NEI)])
            k.op('dve', lambda: nc.vector.tensor_scalar(out=P_(T1), in0=P_(KR), scalar1=-1.0, scalar2=None, op0=ALU.mult), [('s_pr', KR), ('s_pr', T1)], [('s_pr', T1)])
            for j in range(8):
                mt = FR[:, j, 0:TC]
                k.op('dve', lambda: nc.vector.tensor_scalar(out=mt, in0=COS[:, j, 0:TC], scalar1=pr[:, KR, j:j + 1], scalar2=None, op0=ALU.mult), ['s_COS', ('s_pr', KR), 's_FR'], [('s_mt', j)])
                k.op('dve', lambda: nc.vector.scalar_tensor_tensor(out=MRb[:, j, :], in0=SIN[:, j, 0:TC], scalar=pr[:, KI, j:j + 1], in1=mt, op0=ALU.mult, op1=ALU.add), ['s_SIN', ('s_pr', KI), ('s_mt', j)], [('s_MR', j)])
                mt2 = FC[:, j, 0:TC]
                k.op('dve', lambda: nc.vector.tensor_scalar(out=mt2, in0=COS[:, j, 0:TC], scalar1=pr[:, KI, j:j + 1], scalar2=None, op0=ALU.mult), ['s_COS', ('s_pr', KI), 's_FC'], [('s_mt2', j)])
                k.op('dve', lambda: nc.vector.scalar_tensor_tensor(out=MIb[:, j, :], in0=SIN[:, j, 0:TC], scalar=pr[:, T1, j:j + 1], in1=mt2, op0=ALU.mult, op1=ALU.add), ['s_SIN', ('s_pr', T1), ('s_mt2', j)], [('s_MI', j)])
                k.op('pool', lambda: nc.gpsimd.tensor_copy(out=RT[:, j, :], in_=pr[:, R_, j:j + 1].to_broadcast([128, TC])), [('s_pr', R_)], [('s_RT', j)])
                k.op('act', lambda: nc.scalar.copy(out=COSb[:, j, :], in_=COS[:, j, 0:TC]), ['s_COS'], [('s_COSb', j)])
                k.op('act', lambda: nc.scalar.copy(out=SINb[:, j, :], in_=SIN[:, j, 0:TC]), ['s_SIN'], [('s_SINb', j)])
            k.barrier()
        gend = sbt("gend", [128, 8, 4])
        er = Ring(nc, es, "s_e", [128, TC], BF16, 6)
        ar_ = Ring(nc, es, "s_a", [128, TC], BF16, 14)
        gr_ = Ring(nc, es, "s_g", [128, TC], F32, 8)
        gbr = Ring(nc, es, "s_gb", [128, TC], BF16, 8)
        br4 = Ring(nc, es, "s_b4", [128, TC], BF16, 14)
        ur = Ring(nc, es, "s_u", [128, TC], F32, 2)
        yr = Ring(nc, es, "s_y", [128, TC], F32, 3)
        zr = Ring(nc, es, "s_z", [128, 2, TC], F32, 2)
        zbr = Ring(nc, es, "s_zb", [128, 2, TC], BF16, 2)
        obr = Ring(nc, es, "s_ob", [128, TC], BF16, 2)
        items = [(c, ct, jj) for c in range(S // TC) for ct in range(2) for jj in range(4)]
        st = {}
        zstate = {}

        def phA(w):
            c, ct, jj = w
            j = 4 * ct + jj
            sl = slice(c * TC, (c + 1) * TC)
            ps0, pk0 = pg.ps[0], pg.psk[0]
            ps1, pk1 = pg.ps[1], pg.psk[1]
            k.op('pe', lambda: nc.tensor.matmul(ps0[:, :], lhsT=Bw[:, 0, j, :], rhs=uT[:, ct, sl], start=True, stop=True), ['s_Bw', 's_uT'], [pk0])
            k.op('pe', lambda: nc.tensor.matmul(ps1[:, :], lhsT=Bw[:, 1, j, :], rhs=uT[:, ct, sl], start=True, stop=True), ['s_Bw', 's_uT'], [pk1])
            e0, e0k = er.next(); e1, e1k = er.next()
            k.op('act', lambda: nc.scalar.copy(out=e0[:], in_=ps0[:, :]), [pk0], [e0k])
            k.op('act', lambda: nc.scalar.copy(out=e1[:], in_=ps1[:, :]), [pk1], [e1k])
            a1, a1k = ar_.next(); a2, a2k = ar_.next(); a3, a3k = ar_.next(); a4, a4k = ar_.next()
            k.op('dve', lambda: nc.vector.tensor_tensor(out=a1[:], in0=e0[:], in1=MRb[:, j, :], op=ALU.mult), [e0k, ('s_MR', j)], [a1k])
            k.op('dve', lambda: nc.vector.tensor_tensor(out=a2[:], in0=e1[:], in1=MIb[:, j, :], op=ALU.mult), [e1k, ('s_MI', j)], [a2k])
            k.op('dve', lambda: nc.vector.tensor_tensor(out=a3[:], in0=e1[:], in1=MRb[:, j, :], op=ALU.mult), [e1k, ('s_MR', j)], [a3k])
            k.op('dve', lambda: nc.vector.tensor_tensor(out=a4[:], in0=e0[:], in1=MIb[:, j, :], op=ALU.mult), [e0k, ('s_MI', j)], [a4k])
            st[w] = dict(a=(a1, a1k, a2, a2k, a3, a3k, a4, a4k))

        bcnt = [0]

        def phB(w):
            a1, a1k, a2, a2k, a3, a3k, a4, a4k = st[w]["a"]
            pb = 2 + 2 * (bcnt[0] % 2)
            bcnt[0] += 1
            p1, p1k = pg.ps[pb], pg.psk[pb]
            p2, p2k = pg.ps[pb + 1], pg.psk[pb + 1]
            k.op('pe', lambda: nc.tensor.matmul(p1[:, :], lhsT=pg.identb[:], rhs=a1[:], start=True, stop=False), ['identb', a1k], [p1k])
            k.op('pe', lambda: nc.tensor.matmul(p1[:, :], lhsT=nid[:], rhs=a2[:], start=False, stop=True), ['s_nid', a2k], [p1k])
            k.op('pe', lambda: nc.tensor.matmul(p2[:, :], lhsT=pg.identb[:], rhs=a3[:], start=True, stop=False), ['identb', a3k], [p2k])
            k.op('pe', lambda: nc.tensor.matmul(p2[:, :], lhsT=pg.identb[:], rhs=a4[:], start=False, stop=True), ['identb', a4k], [p2k])
            st[w]["bp"] = (p1, p1k, p2, p2k)

        def phC(w):
            c, ct, jj = w
            j = 4 * ct + jj
            p1, p1k, p2, p2k = st[w]["bp"]
            g1, g1k = gr_.next(); g2, g2k = gr_.next()
            if c == 0:
                ir, ii = 0.0, 0.0
                ikeys = []
            else:
                k.op('dve', lambda: nc.vector.tensor_scalar(out=gend[:, j, 2:3], in0=gend[:, j, 0:1], scalar1=pr[:, ER, j:j + 1], scalar2=None, op0=ALU.mult), [('s_ge', j), ('s_pr', ER)], [('s_gi', j, 0)])
                k.op('dve', lambda: nc.vector.scalar_tensor_tensor(out=gend[:, j, 2:3], in0=gend[:, j, 1:2], scalar=pr[:, NEI, j:j + 1], in1=gend[:, j, 2:3], op0=ALU.mult, op1=ALU.add), [('s_ge', j), ('s_pr', NEI), ('s_gi', j, 0)], [('s_gi', j, 0)])
                k.op('dve', lambda: nc.vector.tensor_scalar(out=gend[:, j, 3:4], in0=gend[:, j, 0:1], scalar1=pr[:, EI, j:j + 1], scalar2=None, op0=ALU.mult), [('s_ge', j), ('s_pr', EI)], [('s_gi', j, 1)])
                k.op('dve', lambda: nc.vector.scalar_tensor_tensor(out=gend[:, j, 3:4], in0=gend[:, j, 1:2], scalar=pr[:, ER, j:j + 1], in1=gend[:, j, 3:4], op0=ALU.mult, op1=ALU.add), [('s_ge', j), ('s_pr', ER), ('s_gi', j, 1)], [('s_gi', j, 1)])
                ir, ii = gend[:, j, 2:3], gend[:, j, 3:4]
                ikeys = [('s_gi', j, 0), ('s_gi', j, 1)]
            k.op('dve', lambda: nc.vector.tensor_tensor_scan(out=g1[:], data0=RT[:, j, :], data1=p1[:, :], initial=ir, op0=ALU.mult, op1=ALU.add), [('s_RT', j), p1k] + ikeys, [g1k])
            k.op('dve', lambda: nc.vector.tensor_tensor_scan(out=g2[:], data0=RT[:, j, :], data1=p2[:, :], initial=ii, op0=ALU.mult, op1=ALU.add), [('s_RT', j), p2k] + ikeys, [g2k])
            k.op('act', lambda: nc.scalar.copy(out=gend[:, j, 0:1], in_=g1[:, TC - 1:TC]), [g1k] + ikeys, [('s_ge', j)])
            k.op('act', lambda: nc.scalar.copy(out=gend[:, j, 1:2], in_=g2[:, TC - 1:TC]), [g2k, ('s_ge', j)] + ikeys, [('s_ge', j)])
            gb1, gb1k = gbr.next(); gb2, gb2k = gbr.next()
            k.op('act', lambda: nc.scalar.copy(out=gb1[:], in_=g1[:]), [g1k], [gb1k])
            k.op('act', lambda: nc.scalar.copy(out=gb2[:], in_=g2[:]), [g2k], [gb2k])
            st[w]["gb"] = (gb1, gb1k, gb2, gb2k)

        def phD(w):
            c, ct, jj = w
            j = 4 * ct + jj
            gb1, gb1k, gb2, gb2k = st[w]["gb"]
            b1, b1k = br4.next(); b2, b2k = br4.next(); b3, b3k = br4.next(); b4, b4k = br4.next()
            k.op('dve', lambda: nc.vector.tensor_tensor(out=b1[:], in0=gb1[:], in1=COSb[:, j, :], op=ALU.mult), [gb1k, ('s_COSb', j)], [b1k])
            k.op('dve', lambda: nc.vector.tensor_tensor(out=b2[:], in0=gb2[:], in1=SINb[:, j, :], op=ALU.mult), [gb2k, ('s_SINb', j)], [b2k])
            k.op(S5_ENG[2], lambda: S5_E(nc, 2).tensor_tensor(out=b3[:], in0=gb1[:], in1=SINb[:, j, :], op=ALU.mult), [gb1k, ('s_SINb', j)], [b3k])
            k.op(S5_ENG[3], lambda: S5_E(nc, 3).tensor_tensor(out=b4[:], in0=gb2[:], in1=COSb[:, j, :], op=ALU.mult), [gb2k, ('s_COSb', j)], [b4k])
            st[w]["h"] = (b1, b1k, b2, b2k, b3, b3k, b4, b4k)

        def phE(w):
            c, ct, jj = w
            j = 4 * ct + jj
            sl = slice(c * TC, (c + 1) * TC)
            b1, b1k, b2, b2k, b3, b3k, b4, b4k = st[w]["h"]
            psy, pky = pg.ps[6], pg.psk[6]
            k.op('pe', lambda: nc.tensor.matmul(psy[:, :], lhsT=Cw[:, 0, j, :], rhs=b1[:], start=(jj == 0), stop=False), ['s_Cw', b1k], [pky])
            k.op('pe', lambda: nc.tensor.matmul(psy[:, :], lhsT=NCw[:, j, :], rhs=b2[:], start=False, stop=False), ['s_NCw', b2k], [pky])
            k.op('pe', lambda: nc.tensor.matmul(psy[:, :], lhsT=Cw[:, 1, j, :], rhs=b3[:], start=False, stop=False), ['s_Cw', b3k], [pky])
            k.op('pe', lambda: nc.tensor.matmul(psy[:, :], lhsT=Cw[:, 1, j, :], rhs=b4[:], start=False, stop=(jj == 3)), ['s_Cw', b4k], [pky])
            del st[w]
            if jj != 3:
                return
            if ct == 0:
                zstate["z"] = zr.next()
                zstate["zb"] = zbr.next()
            z32, z32k = zstate["z"]
            zb, zbk = zstate["zb"]
            u32, u32k = ur.next()
            k.dma(u32[:], sc["zu"][ct * 128:(ct + 1) * 128, sl], writes=[u32k])
            y_, yk = yr.next()
            k.op('dve', lambda: nc.vector.scalar_tensor_tensor(out=y_[:], in0=u32[:], scalar=dcol[:, ct:ct + 1], in1=psy[:, :], op0=ALU.mult, op1=ALU.add), [u32k, 's_d', pky], [yk])
            k.op('act', lambda: nc.scalar.activation(out=z32[:, ct, :], in_=y_[:], func=AF.Gelu_apprx_tanh), [yk], [(z32k, ct)])
            k.op('act', lambda: nc.scalar.copy(out=zb[:, ct, :], in_=z32[:, ct, :]), [(z32k, ct)], [(zbk, ct)])
            if ct != 1:
                return
            for co in range(2):
                psg, pkg = pg.ps[7], pg.psk[7]
                for ci in range(2):
                    k.op('pe', lambda: nc.tensor.matmul(psg[:, :], lhsT=glu[:, ci, co * 128:(co + 1) * 128], rhs=zb[:, ci, :], start=(ci == 0), stop=(ci == 1)), ['s_glu', (zbk, ci)], [pkg])
                sg, sgk = yr.next()
                k.op('act', lambda: nc.scalar.activation(out=sg[:], in_=psg[:, :], func=AF.Sigmoid), [pkg], [sgk])
                ob, obk = obr.next()
                k.op('dve', lambda: nc.vector.tensor_tensor(out=ob[:], in0=z32[:, co, :], in1=sg[:], op=ALU.mult), [(z32k, co), sgk], [obk])
                k.dma(sc["yT"][0, co * 128:(co + 1) * 128, sl], ob[:], reads=[obk])

        phases = (phA, phB, phC, phD, phE)
        n = len(items)
        for s_ in range(n + 4):
            for pi_, ph in enumerate(phases):
                i = s_ - pi_
                if 0 <= i < n:
                    ph(items[i])
        k.barrier()


def stage_sc(pg, d, sc):
    nc, k = pg.nc, pg.k
    with ExitStack() as es:
        cw = es.enter_context(_sbt(nc, "sb_c_w", [128, 2, 3], F32))
        k.dma(cw[:], d["sccw"], writes=['c_w'])
        T = [es.enter_context(_sbt(nc, "sb_c_t%d" % i, [128, S], F32)) for i in range(8)]
        ob = es.enter_context(_sbt(nc, "sb_c_ob", [128, S], BF16))
        pr, ac = T[6], T[7]
        for ct in range(2):
            bt, ctile, xt = T[3 * ct:3 * ct + 3]
            k.dma(ctile[:], sc["zsc"][256 + ct * 128:256 + (ct + 1) * 128, :], writes=[('c_c', ct)])
            k.dma(xt[:], sc["zsc"][512 + ct * 128:512 + (ct + 1) * 128, :], writes=[('c_x', ct)])
            k.dma(bt[:], sc["zsc"][ct * 128:(ct + 1) * 128, :], writes=[('c_b', ct)])
        for ct in range(2):
            bt, ctile, xt = T[3 * ct:3 * ct + 3]
            k.op('dve', lambda: nc.vector.tensor_tensor(out=pr[:], in0=ctile[:], in1=xt[:], op=ALU.mult), [('c_c', ct), ('c_x', ct)], ['c_pr'])
            k.op('dve', lambda: nc.vector.tensor_scalar(out=ac[:], in0=pr[:], scalar1=cw[:, ct, 2:3], scalar2=None, op0=ALU.mult), ['c_pr', 'c_w'], ['c_ac'])
            k.op('dve', lambda: nc.vector.scalar_tensor_tensor(out=ac[:, 1:], in0=pr[:, :S - 1], scalar=cw[:, ct, 1:2], in1=ac[:, 1:], op0=ALU.mult, op1=ALU.add), ['c_pr', 'c_w', 'c_ac'], ['c_ac'])
            k.op('dve', lambda: nc.vector.scalar_tensor_tensor(out=ac[:, 2:], in0=pr[:, :S - 2], scalar=cw[:, ct, 0:1], in1=ac[:, 2:], op0=ALU.mult, op1=ALU.add), ['c_pr', 'c_w', 'c_ac'], ['c_ac'])
            k.op('dve', lambda: nc.vector.tensor_tensor(out=ob[:], in0=ac[:], in1=bt[:], op=ALU.mult), ['c_ac', ('c_b', ct)], ['c_ob'])
            k.dma(sc["yT"][3, ct * 128:(ct + 1) * 128, :], ob[:], reads=['c_ob'])
        k.barrier()


def stage_lru(pg, d, sc):
    nc, k = pg.nc, pg.k
    with ExitStack() as es:
        cw = es.enter_context(_sbt(nc, "sb_l_cw", [128, 2, 4], F32))
        pv = es.enter_context(_sbt(nc, "sb_l_pv", [128, 4, 2], F32))
        sp = es.enter_context(_sbt(nc, "sb_l_sp", [128, 4, 2], F32))
        wa = es.enter_context(_sbt(nc, "sb_l_wa", [128, 2, 128], BF16))
        wx = es.enter_context(_sbt(nc, "sb_l_wx", [128, 2, 128], BF16))
        k.dma(cw[:], d["lrucw"], writes=['l_cw'])
        for i, nm in enumerate(("lrucb", "lruba", "lrubx", "lrulam")):
            k.dma(pv[:, i, :], d[nm], writes=[('l_pv', i)])
        k.dma(wa[:], d["lruwa"].rearrange("c p n -> p c n"), writes=['l_wa'], q='pool')
        k.dma(wx[:], d["lruwx"].rearrange("c p n -> p c n"), writes=['l_wx'], q='pool')
        k.op('act', lambda: nc.scalar.activation(out=sp[:, 0, :], in_=pv[:, 3, :], func=AF.Exp, scale=-1.0), [('l_pv', 3)], [('l_sp', 0)])
        k.op('act', lambda: nc.scalar.activation(out=sp[:, 1, :], in_=sp[:, 0, :], func=AF.Ln, bias=1.0), [('l_sp', 0)], [('l_sp', 1)])
        k.op('dve', lambda: nc.vector.tensor_scalar(out=sp[:, 2, :], in0=sp[:, 1, :], scalar1=-8.0, scalar2=None, op0=ALU.mult), [('l_sp', 1)], [('l_sp', 2)])
        k.op('dve', lambda: nc.vector.tensor_scalar(out=sp[:, 3, :], in0=sp[:, 1, :], scalar1=-16.0, scalar2=None, op0=ALU.mult), [('l_sp', 1)], [('l_sp', 3)])
        T = [es.enter_context(_sbt(nc, "sb_l_t%d" % i, [128, S], F32)) for i in range(9)]
        xcb = es.enter_context(_sbt(nc, "sb_l_xcb", [128, S], BF16))
        ob = es.enter_context(_sbt(nc, "sb_l_ob", [128, S], BF16))
        for ct in range(2):
            k.dma(T[5 + 2 * ct][:], sc["zlru"][ct * 128:(ct + 1) * 128, :], writes=[('l_X', ct)])
        for ct in range(2):
            k.dma(T[6 + 2 * ct][:], sc["zlru"][256 + ct * 128:256 + (ct + 1) * 128, :], writes=[('l_G', ct)])
        for ct in range(2):
            XC, R, I, A, TT = T[0:5]
            X, G = T[5 + 2 * ct], T[6 + 2 * ct]
            kx, kxc, kr, ki, ka, kt_ = ('l_X', ct), 'l_XC', 'l_R', 'l_I', 'l_A', 'l_T'
            k.op('dve', lambda: nc.vector.tensor_scalar(out=XC[:], in0=X[:], scalar1=cw[:, ct, 3:4], scalar2=pv[:, 0, ct:ct + 1], op0=ALU.mult, op1=ALU.add), [kx, 'l_cw', ('l_pv', 0)], [kxc])
            for sh in (1, 2, 3):
                k.op('dve', lambda: nc.vector.scalar_tensor_tensor(out=XC[:, sh:], in0=X[:, :S - sh], scalar=cw[:, ct, 3 - sh:4 - sh], in1=XC[:, sh:], op0=ALU.mult, op1=ALU.add), [kx, 'l_cw', kxc], [kxc])
            k.op('dve', lambda: nc.vector.tensor_copy(out=xcb[:], in_=XC[:]), [kxc], ['l_xcb'])
            for qc in range(8):
                sl = slice(qc * 512, (qc + 1) * 512)
                for (w_, wk_, dst, dk, bi, pb) in ((wa, 'l_wa', R, kr, 1, 0), (wx, 'l_wx', I, ki, 2, 1)):
                    ps, pk = pg.ps[pb + 2 * (qc % 2)], pg.psk[pb + 2 * (qc % 2)]
                    k.op('pe', lambda: nc.tensor.matmul(ps[:, :], lhsT=w_[:, ct, :], rhs=xcb[:, sl], start=True, stop=True), [wk_, 'l_xcb'], [pk])
                    k.op('act', lambda: nc.scalar.activation(out=dst[:, sl], in_=ps[:, :], func=AF.Sigmoid, bias=pv[:, bi, ct:ct + 1]), [pk, ('l_pv', bi)], [(dk, qc)])
            rk = [(kr, qc) for qc in range(8)]
            ik = [(ki, qc) for qc in range(8)]
            k.op('act', lambda: nc.scalar.activation(out=A[:], in_=R[:], func=AF.Exp, scale=sp[:, 2, ct:ct + 1]), rk + [('l_sp', 2)], [ka])
            k.op('act', lambda: nc.scalar.activation(out=TT[:], in_=R[:], func=AF.Exp, scale=sp[:, 3, ct:ct + 1]), rk + [('l_sp', 3)], [kt_])
            k.op('act', lambda: nc.scalar.activation(out=TT[:], in_=TT[:], func=AF.Sqrt, scale=-1.0, bias=1.0), [kt_], [kt_])
            k.op('dve', lambda: nc.vector.tensor_tensor(out=I[:], in0=I[:], in1=XC[:], op=ALU.mult), ik + [kxc], ik + ['l_I2'])
            k.op('dve', lambda: nc.vector.tensor_tensor(out=TT[:], in0=TT[:], in1=I[:], op=ALU.mult), [kt_, 'l_I2'] + ik, [kt_])
            k.op('dve', lambda: nc.vector.tensor_tensor_scan(out=R[:], data0=A[:], data1=TT[:], initial=0.0, op0=ALU.mult, op1=ALU.add), [ka, kt_] + rk, rk + ['l_H'])
            k.op('act', lambda: nc.scalar.activation(out=A[:], in_=G[:], func=AF.Gelu_apprx_tanh), [('l_G', ct)], [ka])
            k.op('dve', lambda: nc.vector.tensor_tensor(out=ob[:], in0=R[:], in1=A[:], op=ALU.mult), ['l_H', ka] + rk, ['l_ob'])
            k.dma(sc["yT"][2, ct * 128:(ct + 1) * 128, :], ob[:], reads=['l_ob'])
        k.barrier()


def stage_merge(pg, d, sc, xsrc, xdst, grow_l):
    nc, k = pg.nc, pg.k
    with ExitStack() as es:
        wb = es.enter_context(_sbt(nc, "sb_g_wb", [128, 4, 2, 1024], BF16))
        wo = es.enter_context(_sbt(nc, "sb_g_wo", [128, 8, 1024], BF16))
        k.dma(wb[:], d["wbr"].rearrange("m (ct p) n -> p m ct n", p=128), writes=['g_wb'], q='pool')
        k.dma(wo[:], d["wout"].rearrange("(kc p) n -> p kc n", p=128), writes=['g_wo'], q='pool')
        ymr = Ring(nc, es, "g_ym", [128, 4, 2, 512], BF16, 2)
        gmr = Ring(nc, es, "g_gm", [128, 4, 512], BF16, 4)
        tbr = Ring(nc, es, "g_tb", [128, 512], BF16, 12)
        tmr = Ring(nc, es, "g_tm", [128, 512], F32, 4)
        mTr = Ring(nc, es, "g_mT", [128, 8, 512], BF16, 2)
        xr = Ring(nc, es, "g_x", [128, 1024], F32, 3)
        gmv = sc["zgm"].rearrange("(m dd r) t -> r m dd t", m=4, dd=8)
        pi = 0
        po = 0

        def emit_acc(pend):
            dt, tms, mT, mTk = pend
            ps, pk = pg.ps[3], pg.psk[3]
            for m, (tb, tbk) in enumerate(tms):
                k.op('pe', lambda: nc.tensor.matmul(ps[:, :], lhsT=pg.identb[:], rhs=tb[:], start=(m == 0), stop=(m == 3)), ['identb', tbk], [pk])
            k.op('act', lambda: nc.scalar.copy(out=mT[:, dt, :], in_=ps[:, :]), [pk], [(mTk, dt)])

        for qc in range(8):
            sl = slice(qc * 512, (qc + 1) * 512)
            ym, ymk = ymr.next()
            k.dma(ym[:], sc["yT"].rearrange("m (ct p) t -> p m ct t", p=128)[:, :, :, sl], writes=[ymk])
            mT, mTk = mTr.next()
            pend = None
            for dt in range(8):
                gm, gmk = gmr.next()
                k.dma(gm[:], gmv[:, :, dt, sl], writes=[gmk])
                tms = []
                for m in range(4):
                    ps, pk = pg.ps[pi % 3], pg.psk[pi % 3]
                    pi += 1
                    for ct in range(2):
                        k.op('pe', lambda: nc.tensor.matmul(ps[:, :], lhsT=wb[:, m, ct, dt * 128:(dt + 1) * 128], rhs=ym[:, m, ct, :], start=(ct == 0), stop=(ct == 1)), ['g_wb', ymk], [pk])
                    tb, tbk = tbr.next()
                    k.op('dve', lambda: nc.vector.tensor_tensor(out=tb[:], in0=ps[:, :], in1=gm[:, m, :], op=ALU.mult), [pk, gmk], [tbk])
                    tms.append((tb, tbk))
                if pend is not None:
                    emit_acc(pend)
                pend = (dt, tms, mT, mTk)
            emit_acc(pend)
            for tt in range(4):
                xt, xk = xr.next()
                T0 = qc * 4 + tt
                k.dma(xt[:], xsrc[T0 * 128:(T0 + 1) * 128, :], writes=[xk])
                for ch in range(2):
                    ps, pk = pg.ps[4 + po % 4], pg.psk[4 + po % 4]
                    po += 1
                    for kc in range(8):
                        k.op('pe', lambda: nc.tensor.matmul(ps[:, :], lhsT=mT[:, kc, tt * 128:(tt + 1) * 128], rhs=wo[:, kc, ch * 512:(ch + 1) * 512], start=(kc == 0), stop=(kc == 7)), [(mTk, kc), 'g_wo'], [pk])
                    tm, tmk = tmr.next()
                    k.op('dve', lambda: nc.vector.tensor_tensor(out=tm[:], in0=ps[:, :], in1=grow_l[:, 0, ch * 512:(ch + 1) * 512], op=ALU.mult), [pk], [tmk])
                    k.op('pool', lambda: nc.gpsimd.tensor_tensor(out=xt[:, ch * 512:(ch + 1) * 512], in0=xt[:, ch * 512:(ch + 1) * 512], in1=tm[:], op=ALU.add), [xk, tmk], [xk])
                k.dma(xdst[T0 * 128:(T0 + 1) * 128, :], xt[:], reads=[xk], q='pool')
        k.barrier()


def stage_ffn(pg, xsrc, xdst, gm, sh, gmk, shk, grow_g, experts, F, router=None, final=None):
    nc, k = pg.nc, pg.k
    NF = F // 128
    TC = 1024
    with ExitStack() as es:
        rings = mk_norm_rings(nc, es)
        nbuf = 2 if router is None else 1
        hTs = [es.enter_context(_sbt(nc, "sb_f_hT%d" % i, [128, 8, TC], BF16)) for i in range(nbuf)]

        def hkeyf(par):
            return lambda kc, c: ('f_hT', par, kc, c // 128)
        gen = None
        aT = es.enter_context(_sbt(nc, "sb_f_aT", [128, NF, TC], BF16))
        nwd = 1 if router is None else 2
        wdr = Ring(nc, es, "f_wd", [128, NF, 1024], BF16, nwd)
        wgr = Ring(nc, es, "f_wg", [128, 8, 512], BF16, 4)
        sr = Ring(nc, es, "f_s", [128, 512], F32, 3)
        rt = None
        if router is None:
            tmr = Ring(nc, es, "f_tm", [128, 512], F32, 3)
        else:
            acc = es.enter_context(_sbt(nc, "sb_f_acc", [128, 8, 1024], F32))
            rt = {"h32": Ring(nc, es, "f_h32", [128, 8, 128], F32, 2),
                  "w": es.enter_context(_sbt(nc, "sb_f_rw", [128, 8, 8], F32)),
                  "b": es.enter_context(_sbt(nc, "sb_f_rb", [128, 8], F32)),
                  "logits": es.enter_context(_sbt(nc, "sb_f_lg", [128, 8, 8], F32))}
            k.dma(rt["w"][:], router["w"].rearrange("(kc p) n -> p kc n", p=128), writes=['rw'])
            k.dma(rt["b"][:], router["b"].to_broadcast([128, 8]), writes=['rb'])
            comb = es.enter_context(_sbt(nc, "sb_f_comb", [128, 8, 8], F32))
            if final is not None:
                fgt = es.enter_context(_sbt(nc, "sb_f_fing", [128, 1024], F32))
                k.dma(fgt[:], final[1].to_broadcast([128, 1024]), writes=['fin_g'])
            rs = es.enter_context(_sbt(nc, "sb_f_rs", [128, 8, 16], F32))
            em = es.enter_context(_sbt(nc, "sb_f_em", [128, 8, 8], F32))
        pi = 0

        def wb_tile(c4w, tl):
            T0 = c4w * 8 + tl
            xt, xk = rings["x"].next()
            k.dma(xt[:], xsrc[T0 * 128:(T0 + 1) * 128, :], writes=[xk])
            ak = [('acc', tl, 0), ('acc', tl, 1)]
            k.op('pool', lambda: nc.gpsimd.tensor_tensor(out=acc[:, tl, :], in0=acc[:, tl, :], in1=grow_g[:, :], op=ALU.mult), ak, ak)
            k.op('dve', lambda: nc.vector.tensor_tensor(out=xt[:], in0=xt[:], in1=acc[:, tl, :], op=ALU.add), [xk] + ak, [xk])
            if final is None:
                k.dma(xdst[T0 * 128:(T0 + 1) * 128, :], xt[:], reads=[xk])
                return
            sq, sqk = rings["sq"].next()
            st, stk = rings["st"].next()
            k.op('act', lambda: nc.scalar.activation(out=sq[:], in_=xt[:], func=AF.Square, accum_out=st[:, 0:1]), [xk], [sqk, (stk, 0)])
            k.op('dve', lambda: nc.vector.tensor_scalar(out=st[:, 1:2], in0=st[:, 0:1], scalar1=1.0 / D, scalar2=1e-6, op0=ALU.mult, op1=ALU.add), [(stk, 0)], [(stk, 1)])
            k.op('act', lambda: nc.scalar.activation(out=st[:, 2:3], in_=st[:, 1:2], func=AF.Sqrt), [(stk, 1)], [(stk, 2)])
            k.op('dve', lambda: nc.vector.reciprocal(out=st[:, 3:4], in_=st[:, 2:3]), [(stk, 2)], [(stk, 3)])
            xn, xnk = rings["xn"].next()
            k.op('dve', lambda: nc.vector.scalar_tensor_tensor(out=xn[:], in0=xt[:], scalar=st[:, 3:4], in1=fgt[:], op0=ALU.mult, op1=ALU.mult), [xk, (stk, 3), 'fin_g'], [xnk])
            k.dma(final[0][T0 * 128:(T0 + 1) * 128, :], xn[:], reads=[xnk], is_output=True)

        nch = S // TC
        for c4 in range(nch):
            tts = list(range(c4 * 8, c4 * 8 + 8))
            hTc = hTs[c4 % nbuf]
            hkey = hkeyf(c4 % nbuf)
            if gen is None:
                norm_tiles(pg, rings, xsrc, tts, hTc, 0, gm, sh, gmk, shk, hkey, router=rt)
            else:
                for _ in gen:
                    pass
                gen = None
            if router is not None:
                lg = rt["logits"]
                for i in range(8):
                    lk = ('logits', i)
                    k.op('dve', lambda: nc.vector.max(out=rs[:, i, 0:8], in_=lg[:, i, :]), [lk], [('rs', i, 0)])
                    k.op('dve', lambda: nc.vector.tensor_scalar(out=rs[:, i, 8:9], in0=rs[:, i, 0:1], scalar1=-1.0, scalar2=None, op0=ALU.mult), [('rs', i, 0)], [('rs', i, 1)])
                    k.op('act', lambda: nc.scalar.activation(out=em[:, i, :], in_=lg[:, i, :], func=AF.Exp, bias=rs[:, i, 8:9]), [lk, ('rs', i, 1)], [('em', i)])
                    k.op('dve', lambda: nc.vector.tensor_scalar(out=comb[:, i, :], in0=lg[:, i, :], scalar1=rs[:, i, 1:2], scalar2=None, op0=ALU.is_ge), [lk, ('rs', i, 0)], [('comb', i)])
                    k.op('dve', lambda: nc.vector.tensor_tensor(out=em[:, i, :], in0=em[:, i, :], in1=comb[:, i, :], op=ALU.mult), [('em', i), ('comb', i)], [('em', i)])
                    k.op('dve', lambda: nc.vector.reduce_sum(out=rs[:, i, 9:10], in_=em[:, i, :], axis=AX.X), [('em', i)], [('rs', i, 2)])
                    k.op('dve', lambda: nc.vector.reciprocal(out=rs[:, i, 10:11], in_=rs[:, i, 9:10]), [('rs', i, 2)], [('rs', i, 3)])
                    k.op('dve', lambda: nc.vector.tensor_scalar(out=comb[:, i, :], in0=em[:, i, :], scalar1=rs[:, i, 10:11], scalar2=None, op0=ALU.mult), [('em', i), ('rs', i, 3)], [('comb', i)])
            for e, (Wg, Wu, Wd) in enumerate(experts):
                wgv = Wg.rearrange("(kc p) n -> p kc n", p=128)
                wuv = Wu.rearrange("(kc p) n -> p kc n", p=128)
                for f0 in range(0, F, 512):
                    n = min(512, F - f0)
                    wg, wgk = wgr.next()
                    wu, wuk = wgr.next()
                    k.dma(wg[:, :, 0:n], wgv[:, :, f0:f0 + n], writes=[wgk], q='pool')
                    k.dma(wu[:, :, 0:n], wuv[:, :, f0:f0 + n], writes=[wuk], q='pool')
                    for ft in range(n // 128):
                        fi = f0 // 128 + ft
                        for hf in range(2):
                            psg, pkg = pg.ps[4 + pi % 2], pg.psk[4 + pi % 2]
                            psu, pku = pg.ps[6 + pi % 2], pg.psk[6 + pi % 2]
                            pi += 1
                            hk = [hkey(kc_, hf * 512 + j * 128) for kc_ in range(8) for j in range(4)]
                            for kc in range(8):
                                k.op('pe', lambda: nc.tensor.matmul(psg[:, :], lhsT=wg[:, kc, ft * 128:(ft + 1) * 128], rhs=hTc[:, kc, hf * 512:(hf + 1) * 512], start=(kc == 0), stop=(kc == 7)), [wgk] + hk, [pkg])
                            for kc in range(8):
                                k.op('pe', lambda: nc.tensor.matmul(psu[:, :], lhsT=wu[:, kc, ft * 128:(ft + 1) * 128], rhs=hTc[:, kc, hf * 512:(hf + 1) * 512], start=(kc == 0), stop=(kc == 7)), [wuk] + hk, [pku])
                            s_, sk = sr.next()
                            k.op('act', lambda: nc.scalar.activation(out=s_[:], in_=psg[:, :], func=AF.Silu), [pkg], [sk])
                            k.op('dve', lambda: nc.vector.tensor_tensor(out=aT[:, fi, hf * 512:(hf + 1) * 512], in0=psu[:, :], in1=s_[:], op=ALU.mult), [pku, sk], [('f_aT', fi, hf)])
                wd, wdk = wdr.next()
                k.dma(wd[:], Wd.rearrange("(fc p) n -> p fc n", p=128), writes=[wdk], q='pool')
                for tl in range(8):
                    T0 = c4 * 8 + tl
                    if router is not None and e == 0 and c4 > 0:
                        wb_tile(c4 - 1, tl)
                    if router is None:
                        xt, xk = rings["x"].next()
                        k.dma(xt[:], xsrc[T0 * 128:(T0 + 1) * 128, :], writes=[xk])
                    for ch in range(2):
                        ps, pk = pg.ps[pi % 4], pg.psk[pi % 4]
                        pi += 1
                        for fc in range(NF):
                            k.op('pe', lambda: nc.tensor.matmul(ps[:, :], lhsT=aT[:, fc, tl * 128:(tl + 1) * 128], rhs=wd[:, fc, ch * 512:(ch + 1) * 512], start=(fc == 0), stop=(fc == NF - 1)), [('f_aT', fc, tl // 4), wdk], [pk])
                        csl = slice(ch * 512, (ch + 1) * 512)
                        if router is None:
                            tm, tmk = tmr.next()
                            k.op('dve', lambda: nc.vector.tensor_tensor(out=tm[:], in0=ps[:, :], in1=grow_g[:, csl], op=ALU.mult), [pk], [tmk])
                            k.op('pool', lambda: nc.gpsimd.tensor_tensor(out=xt[:, csl], in0=xt[:, csl], in1=tm[:], op=ALU.add), [xk, tmk], [xk])
                        elif e == 0:
                            k.op('dve', lambda: nc.vector.tensor_scalar(out=acc[:, tl, csl], in0=ps[:, :], scalar1=comb[:, tl, e:e + 1], scalar2=None, op0=ALU.mult), [pk, ('comb', tl)], [('acc', tl, ch)])
                        else:
                            k.op('dve', lambda: nc.vector.scalar_tensor_tensor(out=acc[:, tl, csl], in0=ps[:, :], scalar=comb[:, tl, e:e + 1], in1=acc[:, tl, csl], op0=ALU.mult, op1=ALU.add), [pk, ('comb', tl), ('acc', tl, ch)], [('acc', tl, ch)])
                    if router is None:
                        k.dma(xdst[T0 * 128:(T0 + 1) * 128, :], xt[:], reads=[xk])
                        if c4 + 1 < nch:
                            if gen is None:
                                ntts = list(range((c4 + 1) * 8, (c4 + 1) * 8 + 8))
                                gen = norm_tiles_gen(pg, rings, xsrc, ntts, hTs[(c4 + 1) % nbuf], 0, gm, sh, gmk, shk, hkeyf((c4 + 1) % nbuf), pbase=4)
                            next(gen, None)
        if router is not None:
            for tl in range(8):
                wb_tile(S // TC - 1, tl)
        k.barrier()


def stage_final(pg, xsrc, out_d, g_row):
    nc, k = pg.nc, pg.k
    with ExitStack() as es:
        rings = mk_norm_rings(nc, es)
        gt = es.enter_context(_sbt(nc, "sb_fin_g", [128, 1024], F32))
        k.dma(gt[:], g_row.to_broadcast([128, 1024]), writes=['fin_g'])
        for tt in range(NT):
            xt, xk = rings["x"].next()
            k.dma(xt[:], xsrc[tt * 128:(tt + 1) * 128, :], writes=[xk])
            sq, sqk = rings["sq"].next()
            st, stk = rings["st"].next()
            k.op('act', lambda: nc.scalar.activation(out=sq[:], in_=xt[:], func=AF.Square, accum_out=st[:, 0:1]), [xk], [sqk, (stk, 0)])
            k.op('dve', lambda: nc.vector.tensor_scalar(out=st[:, 1:2], in0=st[:, 0:1], scalar1=1.0 / D, scalar2=1e-6, op0=ALU.mult, op1=ALU.add), [(stk, 0)], [(stk, 1)])
            k.op('act', lambda: nc.scalar.activation(out=st[:, 2:3], in_=st[:, 1:2], func=AF.Sqrt), [(stk, 1)], [(stk, 2)])
            k.op('dve', lambda: nc.vector.reciprocal(out=st[:, 3:4], in_=st[:, 2:3]), [(stk, 2)], [(stk, 3)])
            xn, xnk = rings["xn"].next()
            k.op('dve', lambda: nc.vector.scalar_tensor_tensor(out=xn[:], in0=xt[:], scalar=st[:, 3:4], in1=gt[:], op0=ALU.mult, op1=ALU.mult), [xk, (stk, 3), 'fin_g'], [xnk])
            k.dma(out_d[tt * 128:(tt + 1) * 128, :], xn[:], reads=[xnk], is_output=True)
        k.barrier()


def stage_nsa(pg, d, sc):
    nc, k = pg.nc, pg.k
    SCALE = 0.125
    with ExitStack() as es:
        sbt = lambda name, shape, dt=F32: es.enter_context(_sbt(nc, "sb_a_" + name, shape, dt))
        kT = sbt("kT", [64, 3, S], BF16)
        k.dma(kT[:, 0, :], sc["zk"][0], writes=[('a_kT', 0)])
        k.dma(kT[:, 2, :], sc["zk"][2], writes=[('a_kT', 2)])
        kx = sbt("kx", [128, S], BF16)
        k.dma(kx[0:64, :], sc["zk"][1], writes=['a_kx0'])
        k.dma(kx[64:128, :], pg.din["xpf"], writes=['a_kx1'])
        V1 = [sbt("V1_%d" % i, [128, 32, 65], BF16) for i in range(2)]
        zvv = sc["zv"].rearrange("(kt p) c -> p kt c", p=128)
        for i in range(2):
            k.dma(V1[i][:, :, 0:64], zvv[:, :, 64 * (i + 1):64 * (i + 2)], writes=[('a_V1', i)])
            k.op('pool', lambda: nc.gpsimd.memset(V1[i][:, :, 64:65], 1.0), [], [('a_V1o', i)])
        caus = sbt("caus", [128, 4, 512], BF16)
        wlow = sbt("wlow", [128, 4, 512], BF16)
        k.dma(caus[:], pg.din["caus"], writes=['a_caus'])
        k.dma(wlow[:], pg.din["wlow"], writes=['a_wlow'])
        gts = sbt("gts", [128, 32, 12])
        k.dma(gts[:], sc["zg"].rearrange("(tt p) c -> p tt c", p=128), writes=['a_g'])
        kcmpT = sbt("kcmpT", [64, 256], BF16)
        VC1 = sbt("VC1", [128, 2, 129], BF16)
        with ExitStack() as cs:
            cbt = lambda name, shape, dt=F32: cs.enter_context(_sbt(nc, "sb_ac_" + name, shape, dt))
            vcT = cbt("vcT", [64, S], BF16)
            k.dma(vcT[:], sc["zvc"], writes=['ac_vcT'])
            ovl = cbt("ovl", [128, 2, 64])
            k.dma(ovl[:], pg.din["ovl"], writes=['ac_ovl'])
            for nt in range(2):
                k.op('pool', lambda: nc.gpsimd.memset(VC1[:, nt, 64:65], 1.0), [], [('a_VC1o', nt)])
                k.op('pool', lambda: nc.gpsimd.tensor_copy(out=VC1[:, nt, 65:129], in_=ovl[:, nt, :]), ['ac_ovl'], [('a_VC1v', nt)])
            k.op('pool', lambda: nc.gpsimd.memset(kcmpT[:], 0.0), [], ['a_kcmpT'])
            for which, (w1n, w2n, pen) in enumerate((("ckw1", "ckw2", "pekT"), ("cvw1", "cvw2", "pevT"))):
                w1 = cbt("w1_%d" % which, [64, 32, 128], BF16)
                w2 = cbt("w2_%d" % which, [128, 64], BF16)
                pe = cbt("pe_%d" % which, [64, 32], BF16)
                gT = cbt("gT_%d" % which, [128, 256], BF16)
                bc = cbt("bc_%d" % which, [128, 1])
                kk = 'ac%d_' % which
                k.dma(w1[:], d[w1n].rearrange("(l dd) j -> dd l j", dd=64), writes=[kk + 'w1'], q='pool')
                k.dma(w2[:], d[w2n], writes=[kk + 'w2'], q='pool')
                k.dma(pe[:], d[pen], writes=[kk + 'pe'], q='pool')
                k.op('pool', lambda: nc.gpsimd.memset(gT[:], 0.0), [], [kk + 'gT'])
                psb, pkb = pg.ps[6], pg.psk[6]
                for l in range(32):
                    k.op('pe', lambda: nc.tensor.matmul(psb[:, 0:1], lhsT=w1[:, l, :], rhs=pe[:, l:l + 1], start=(l == 0), stop=(l == 31)), [kk + 'w1', kk + 'pe'], [pkb])
                k.op('dve', lambda: nc.vector.tensor_copy(out=bc[:], in_=psb[:, 0:1]), [pkb], [kk + 'bc'])
                psh, pkh = pg.ps[7], pg.psk[7]
                for l in range(32):
                    src = kT[:, 0, l:l + 16 * 254 + 1:16] if which == 0 else vcT[:, l:l + 16 * 254 + 1:16]
                    k.op('pe', lambda: nc.tensor.matmul(psh[:, 0:255], lhsT=w1[:, l, :], rhs=src, start=(l == 0), stop=(l == 31)), [kk + 'w1', ('a_kT', 0) if which == 0 else 'ac_vcT'], [pkh])
                k.op('act', lambda: nc.scalar.activation(out=gT[:, 0:255], in_=psh[:, 0:255], func=AF.Gelu_apprx_tanh, bias=bc[:, 0:1]), [pkh, kk + 'bc', kk + 'gT'], [kk + 'gT'])
                if which == 0:
                    ps, pk = pg.ps[5], pg.psk[5]
                    k.op('pe', lambda: nc.tensor.matmul(ps[0:64, 0:255], lhsT=w2[:, :], rhs=gT[:, 0:255], start=True, stop=True), [kk + 'w2', kk + 'gT'], [pk])
                    k.op('dve', lambda: nc.vector.tensor_copy(out=kcmpT[:, 0:255], in_=ps[0:64, 0:255]), [pk, 'a_kcmpT'], ['a_kcmpT'])
                else:
                    for nt in range(2):
                        ps, pk = pg.ps[4 + nt], pg.psk[4 + nt]
                        k.op('pe', lambda: nc.tensor.matmul(ps[:, 0:64], lhsT=gT[:, nt * 128:(nt + 1) * 128], rhs=w2[:, :], start=True, stop=True), [kk + 'w2', kk + 'gT'], [pk])
                        k.op('dve', lambda: nc.vector.tensor_copy(out=VC1[:, nt, 0:64], in_=ps[:, 0:64]), [pk], [('a_VC1', nt)])
            k.barrier()
        ETn = 36
        ET = Ring(nc, es, "a_ET", [128, 512], BF16, ETn)
        OBs = [sbt("OB%d" % i, [128, 4, 2, 4, 65]) for i in range(2)]
        OCs = [sbt("OC%d" % i, [128, 4, 4, 129]) for i in range(2)]
        cmpb_r = Ring(nc, es, "a_cmpb", [128, 2, 512], BF16, 2)
        impA_r = Ring(nc, es, "a_impA", [128, 4, 64], F32, 2)
        impB_r = Ring(nc, es, "a_impB", [128, 4, 64], F32, 2)
        sm = Ring(nc, es, "a_sm", [128, 64], F32, 6)
        sbr = Ring(nc, es, "a_sb", [128, 128], F32, 8)
        for i_, t_ in enumerate(sbr.tiles):
            k.op('pool', lambda: nc.gpsimd.memset(t_[:, 0:64], 0.0), [], [sbr.keys[i_]])
        qsr = Ring(nc, es, "a_qs", [128, 4, 512], BF16, 2)
        sm8 = Ring(nc, es, "a_sm8", [128, 16], F32, 4)
        rdn = Ring(nc, es, "a_rd", [128, 12], F32, 3)
        cf = Ring(nc, es, "a_cf", [128, 12], F32, 3)
        otok = Ring(nc, es, "a_ot", [128, 256], F32, 8)
        yb = Ring(nc, es, "a_yb", [128, 2, 512], BF16, 2)
        cnt = {"s": 0, "o": 0}

        def attend(qc, h, tiles, vis, out_fn, rq, rqk):
            ets = []
            for (kl, biases, rv, rvk, kkeys) in tiles:
                pb = cnt["s"] % 3
                cnt["s"] += 1
                ps, pk = pg.ps[pb], pg.psk[pb]
                nb = len(biases)
                k.op('pe', lambda: nc.tensor.matmul(ps[:, :], lhsT=kl, rhs=rq, start=True, stop=(nb == 0)), rqk + kkeys, [pk])
                for bi, bias in enumerate(biases):
                    bl, br_, bk = bias[0:3]
                    if len(bias) > 3:
                        c0 = bias[3]
                        k.op('pe', lambda: nc.tensor.matmul(ps[:, c0:c0 + 128], lhsT=bl, rhs=br_[:, c0:c0 + 128], start=False, stop=(bi == nb - 1)), bk, [pk])
                    else:
                        k.op('pe', lambda: nc.tensor.matmul(ps[:, :], lhsT=bl, rhs=br_, start=False, stop=(bi == nb - 1)), bk, [pk])
                et, etk = ET.next()
                k.op('act', lambda: nc.scalar.activation(out=et[:], in_=ps[:, :], func=AF.Exp, scale=SCALE), [pk], [etk])
                ets.append((et, etk, rv, rvk))
            for tt in range(4):
                idx = [i for i in range(len(ets)) if vis(i, tt)]
                pb = 3 + cnt["o"] % 2
                cnt["o"] += 1
                ps, pk = pg.ps[pb], pg.psk[pb]
                for n_, i in enumerate(idx):
                    et, etk, rv, rvk = ets[i]
                    ncol = rv.shape[-1]
                    k.op('pe', lambda: nc.tensor.matmul(ps[:, 0:ncol], lhsT=et[:, tt * 128:(tt + 1) * 128], rhs=rv, start=(n_ == 0), stop=(n_ == len(idx) - 1)), [etk] + rvk, [pk])
                out_fn(tt, ps, pk)

        stq = {}

        def ph_cmp(qc):
            par = qc % 2
            OC = OCs[par]
            qsl = slice(qc * 512, (qc + 1) * 512)
            cb, cbk = cmpb_r.next()
            k.dma(cb[:], pg.din["cmpb"][:, :, qsl], writes=[cbk])
            iA, iAk = impA_r.next()
            iB, iBk = impB_r.next()
            k.dma(iA[:], pg.din["impA"][:, qc * 4:(qc + 1) * 4, :], writes=[iAk])
            k.dma(iB[:], pg.din["impB"][:, qc * 4:(qc + 1) * 4, :], writes=[iBk])
            qs, qsk = qsr.next()
            k.dma(qs[0:64, :, :], sc["zq"].rearrange("h p t -> p h t")[:, :, qsl], writes=[(qsk, 'q')])
            stq[qc] = dict(iA=(iA, iAk), iB=(iB, iBk), qs=(qs, qsk))
            nts = [0] if qc < 4 else [0, 1]
            for h in range(4):
                tiles = [(kcmpT[:, nt * 128:(nt + 1) * 128], [(pg.identb[:], cb[:, nt, :], ['identb', cbk])], VC1[:, nt, :],
                          [('a_VC1', nt), ('a_VC1o', nt), ('a_VC1v', nt)], ['a_kcmpT']) for nt in nts]

                def out_c(tt, ps, pk, h=h):
                    pg.copy('act', OC[:, tt, h, :], ps[:, 0:129], [pk], [('a_OC', par, tt, h)])
                attend(qc, h, tiles, lambda i, tt: True, out_c, qs[0:64, h, :], [(qsk, 'q')])

        def ph_select_dve(qc):
            par = qc % 2
            OC = OCs[par]
            iA, iAk = stq[qc]["iA"]
            iB, iBk = stq[qc]["iB"]
            sbs = []
            for tt in range(4):
                ock = [('a_OC', par, tt, h) for h in range(4)]
                rd, rdk = rdn.next()
                k.op('dve', lambda: nc.vector.tensor_scalar(out=rd[:, 0:4], in0=OC[:, tt, :, 64], scalar1=1e-30, scalar2=None, op0=ALU.max), ock, [(rdk, 0)])
                k.op('dve', lambda: nc.vector.reciprocal(out=rd[:, 0:4], in_=rd[:, 0:4]), [(rdk, 0)], [(rdk, 0)])
                im, imk = sm.next()
                k.op('dve', lambda: nc.vector.tensor_scalar(out=im[:], in0=OC[:, tt, 0, 65:129], scalar1=rd[:, 0:1], scalar2=None, op0=ALU.mult), ock + [(rdk, 0)], [imk])
                for h in range(1, 4):
                    k.op('dve', lambda: nc.vector.scalar_tensor_tensor(out=im[:], in0=OC[:, tt, h, 65:129], scalar=rd[:, h:h + 1], in1=im[:], op0=ALU.mult, op1=ALU.add), ock + [(rdk, 0), imk], [imk])
                k.op('dve', lambda: nc.vector.tensor_tensor(out=im[:], in0=im[:], in1=iA[:, tt, :], op=ALU.mult), [imk, iAk], [imk])
                k.op('dve', lambda: nc.vector.tensor_tensor(out=im[:], in0=im[:], in1=iB[:, tt, :], op=ALU.add), [imk, iBk], [imk])
                m8, m8k = sm8.next()
                rp, rpk = sm.next()
                k.op('dve', lambda: nc.vector.max(out=m8[:, 0:8], in_=im[:]), [imk], [(m8k, 0)])
                k.op('dve', lambda: nc.vector.match_replace(out=rp[:], in_to_replace=m8[:, 0:8], in_values=im[:], imm_value=-1e30), [imk, (m8k, 0)], [rpk])
                k.op('dve', lambda: nc.vector.max(out=m8[:, 8:16], in_=rp[:]), [rpk], [(m8k, 1)])
                sb_, sbk = sbr.next()
                k.op('dve', lambda: nc.vector.tensor_scalar(out=sb_[:, 64:128], in0=im[:], scalar1=m8[:, 15:16], scalar2=None, op0=ALU.is_ge), [imk, (m8k, 1), sbk], [sbk])
                k.op('dve', lambda: nc.vector.tensor_scalar(out=sb_[:, 64:128], in0=sb_[:, 64:128], scalar1=-NEG, scalar2=NEG, op0=ALU.mult, op1=ALU.add), [sbk], [sbk])
                sbs.append((sb_, sbk))
            stq[qc]["sbs"] = sbs

        def ph_select_T(qc):
            qs, qsk = stq[qc]["qs"]
            for tt, (sb_, sbk) in enumerate(stq[qc]["sbs"]):
                ps, pk = pg.ps[5], pg.psk[5]
                k.op('pe', lambda: nc.tensor.transpose(out=ps[:, 0:128], in_=sb_[:, :], identity=pg.ident[:]), [sbk, 'ident'], [pk])
                for h in range(4):
                    k.op('act', lambda: nc.scalar.copy(out=qs[64:128, h, tt * 128:(tt + 1) * 128], in_=ps[64:128, 0:128]), [pk], [(qsk, 's', tt, h)])

        def ph_win(qc):
            par = qc % 2
            OB = OBs[par]
            qs, qsk = stq[qc]["qs"]
            for h in range(4):
                tiles = []
                k0 = max(0, 4 * qc - 4)
                for kt in range(k0, 4 * qc + 4):
                    if kt >= 4 * qc:
                        b = [(pg.identb[:], caus[:, kt - 4 * qc, :], ['identb', 'a_caus'], 128 * (kt - 4 * qc))]
                    else:
                        b = [(pg.identb[:], wlow[:, kt - (4 * qc - 4), :], ['identb', 'a_wlow'], 128 * (kt - (4 * qc - 4)))]
                    tiles.append((kT[:, 2, kt * 128:(kt + 1) * 128], b, V1[1][:, kt, :], [('a_V1', 1), ('a_V1o', 1)], [('a_kT', 2)]))

                def out_w(tt, ps, pk, h=h):
                    pg.copy('act', OB[:, tt, 1, h, :], ps[:, 0:65], [pk], [('a_OB', par, tt, 1, h)])
                attend(qc, h, tiles, lambda i, tt, k0=k0: (4 * qc + tt - 4) <= (k0 + i) <= (4 * qc + tt), out_w, qs[0:64, h, :], [(qsk, 'q')])

        def ph_sel(qc):
            par = qc % 2
            OB = OBs[par]
            qs, qsk = stq[qc]["qs"]
            for h in range(4):
                tiles = []
                for kt in range(4 * qc + 4):
                    b = []
                    if kt >= 4 * qc:
                        b.append((pg.identb[:], caus[:, kt - 4 * qc, :], ['identb', 'a_caus'], 128 * (kt - 4 * qc)))
                    tiles.append((kx[:, kt * 128:(kt + 1) * 128], b, V1[0][:, kt, :], [('a_V1', 0), ('a_V1o', 0)], ['a_kx0', 'a_kx1']))

                def out_s(tt, ps, pk, h=h):
                    pg.copy('act', OB[:, tt, 0, h, :], ps[:, 0:65], [pk], [('a_OB', par, tt, 0, h)])
                attend(qc, h, tiles, lambda i, tt: i <= 4 * qc + tt, out_s, qs[:, h, :], [(qsk, 'q')] + [(qsk, 's', tt, h) for tt in range(4)])

        def ph_combine_dve(qc):
            par = qc % 2
            OC, OB = OCs[par], OBs[par]
            ots = []
            for tt in range(4):
                T0 = 4 * qc + tt
                rd, rdk = rdn.next()
                allk = [('a_OC', par, tt, h) for h in range(4)] + [('a_OB', par, tt, b_, h) for b_ in range(2) for h in range(4)]
                k.op('dve', lambda: nc.vector.tensor_scalar(out=rd[:, 0:4], in0=OC[:, tt, :, 64], scalar1=1e-30, scalar2=None, op0=ALU.max), allk, [(rdk, 0)])
                k.op('dve', lambda: nc.vector.tensor_copy(out=rd[:, 4:8], in_=OB[:, tt, 0, :, 64]), allk, [(rdk, 1)])
                k.op('dve', lambda: nc.vector.tensor_copy(out=rd[:, 8:12], in_=OB[:, tt, 1, :, 64]), allk, [(rdk, 2)])
                k.op('dve', lambda: nc.vector.reciprocal(out=rd[:, :], in_=rd[:, :]), [(rdk, 0), (rdk, 1), (rdk, 2)], [(rdk, 3)])
                c_, ck = cf.next()
                k.op('dve', lambda: nc.vector.tensor_tensor(out=c_[:, :].rearrange("p (b h) -> p b h", b=3), in0=rd[:, :].rearrange("p (b h) -> p b h", b=3),
                                                            in1=gts[:, T0, :].rearrange("p (h b) -> p b h", b=3), op=ALU.mult), [(rdk, 3), 'a_g'], [ck])
                ot, otk = otok.next()
                for h in range(4):
                    hs = slice(h * 64, (h + 1) * 64)
                    k.op('dve', lambda: nc.vector.tensor_scalar(out=ot[:, hs], in0=OC[:, tt, h, 0:64], scalar1=c_[:, h:h + 1], scalar2=None, op0=ALU.mult), allk + [ck], [(otk, h)])
                    for b_ in range(2):
                        k.op('dve', lambda: nc.vector.scalar_tensor_tensor(out=ot[:, hs], in0=OB[:, tt, b_, h, 0:64], scalar=c_[:, 4 * (b_ + 1) + h:4 * (b_ + 1) + h + 1], in1=ot[:, hs], op0=ALU.mult, op1=ALU.add), allk + [ck, (otk, h)], [(otk, h)])
                ots.append((ot, otk))
            stq[qc]["ots"] = ots

        def ph_combine_T(qc):
            qsl = slice(qc * 512, (qc + 1) * 512)
            ybt, ybk = yb.next()
            for tt, (ot, otk) in enumerate(stq[qc]["ots"]):
                for ct in range(2):
                    ps, pk = pg.ps[6 + ct], pg.psk[6 + ct]
                    k.op('pe', lambda: nc.tensor.transpose(out=ps[:, 0:128], in_=ot[:, ct * 128:(ct + 1) * 128], identity=pg.ident[:]), [(otk, 2 * ct), (otk, 2 * ct + 1), 'ident'], [pk])
                    pg.copy('act', ybt[:, ct, tt * 128:(tt + 1) * 128], ps[:, 0:128], [pk], [(ybk, ct, tt)])
            k.dma(sc["yT"].rearrange("m (ct p) t -> p m ct t", p=128)[:, 1, :, qsl], ybt[:], reads=[(ybk, ct, tt) for ct in range(2) for tt in range(4)])
            del stq[qc]

        for qc in range(8):
            ph_cmp(qc)
            ph_select_dve(qc)
            if qc > 0:
                ph_combine_dve(qc - 1)
            ph_win(qc)
            ph_select_T(qc)
            ph_sel(qc)
            if qc > 0:
                ph_combine_T(qc - 1)
        ph_combine_dve(7)
        ph_combine_T(7)
        k.barrier()


def make_host_inputs(inputs, b, consts=None, layers=None):
    hi = dict(consts if consts is not None else host_consts())
    hi["x"] = np.ascontiguousarray(inputs["x"][b], dtype=np.float32)
    hi["ccol"] = _col(inputs["c"][b], 8)
    layers = layers if layers is not None else [host_layer(inputs, l) for l in range(2)]
    for l in range(2):
        for kk, v in layers[l].items():
            hi["L%d_%s" % (l, kk)] = np.ascontiguousarray(v, dtype=np.float32)
    hi["ffn_wg"] = np.ascontiguousarray(inputs["ffn_w_gate"][0])
    hi["ffn_wu"] = np.ascontiguousarray(inputs["ffn_w_up"][0])
    hi["ffn_wd"] = np.ascontiguousarray(inputs["ffn_w_down"][0])
    hi["moe_wg"] = np.ascontiguousarray(inputs["moe_w_gate"][0])
    hi["moe_wu"] = np.ascontiguousarray(inputs["moe_w_up"][0])
    hi["moe_wd"] = np.ascontiguousarray(inputs["moe_w_down"][0])
    hi["moe_rw"] = np.ascontiguousarray(inputs["moe_router_w"][0])
    hi["moe_rb"] = np.ascontiguousarray(inputs["moe_router_b"][0].reshape(1, 8))
    hi["fin_g"] = np.ascontiguousarray(inputs["final_norm_g"].reshape(1, 1024))
    return hi


def kernel(**inputs):
    inputs = {k_: np.asarray(v) for k_, v in inputs.items()}
    consts = host_consts()
    layers = [host_layer(inputs, l) for l in range(2)]
    in_maps = [make_host_inputs(inputs, b, consts, layers) for b in range(8)]
    nc, pg = build(in_maps[0], set())
    res = run_bass_kernel_spmd(nc, in_maps, core_ids=list(range(8)))
    out = np.stack([np.asarray(r["out"], dtype=np.float32) for r in res.results], 0)
    return out
```

```python
import numpy as np
import concourse.bass as bass
import concourse.mybir as mybir
from concourse.bass_utils import run_bass_kernel_spmd

F32 = mybir.dt.float32
BF16 = mybir.dt.bfloat16
U32 = mybir.dt.uint32
AF = mybir.ActivationFunctionType
ALU = mybir.AluOpType
AX = mybir.AxisListType

EPOCH = 30000
NSLOT = 12


class K:
    def __init__(self, nc):
        self.nc = nc
        self.eng = {'pe': nc.tensor, 'act': nc.scalar, 'dve': nc.vector,
                    'pool': nc.gpsimd, 'sp': nc.sync}
        self.sem = {}
        self.cnt = {}
        self.known = {e: {} for e in self.eng}
        self.last_w = {}
        self.readers = {}
        self.vcs = {}
        self.epoch = {e: 0 for e in ('pe', 'act', 'dve', 'pool')}
        self.dma_i = {e: 0 for e in ('sp', 'pool', 'act')}
        self.nsem = 0
        self.ninst = 0
        self.out_events = []

    def _clock(self, name):
        if name not in self.sem:
            self.sem[name] = self.nc.alloc_semaphore("s_%s_%s" % (name[0], name[1]))
            self.cnt[name] = 0
            self.nsem += 1
        return name

    def _eclock(self, e):
        c = (e, self.epoch[e])
        self._clock(c)
        if self.cnt[c] >= EPOCH:
            self.epoch[e] += 1
            c = (e, self.epoch[e])
            self._clock(c)
        return c

    def _wait(self, e, deps):
        need = {}
        for (c, v) in deps:
            if v > need.get(c, 0):
                need[c] = v
        kn = self.known[e]
        for c, v in need.items():
            if e == 'pe' and c[0] == 'pe':
                continue
            if kn.get(c, 0) >= v:
                continue
            self.eng[e].wait_ge(self.sem[c], v)
            self.ninst += 1
            vc = self.vcs.get((c, v))
            if vc is not None:
                for c2, v2 in vc.items():
                    if kn.get(c2, 0) < v2:
                        kn[c2] = v2
            if kn.get(c, 0) < v:
                kn[c] = v

    def _deps(self, reads, writes):
        deps = set()
        for k in reads:
            w = self.last_w.get(k)
            if w is not None:
                deps.add(w)
        for k in writes:
            w = self.last_w.get(k)
            if w is not None:
                deps.add(w)
            r = self.readers.get(k)
            if r:
                for c, v in r.items():
                    deps.add((c, v))
        return deps

    def _record(self, ev, e, reads, writes):
        vc = dict(self.known[e])
        vc[ev[0]] = ev[1]
        self.vcs[ev] = vc
        for k in writes:
            self.last_w[k] = ev
            self.readers[k] = {}
        for k in reads:
            r = self.readers.setdefault(k, {})
            if r.get(ev[0], 0) < ev[1]:
                r[ev[0]] = ev[1]

    def op(self, e, fn, reads=(), writes=()):
        pr = [x for x in reads if isinstance(x, tuple) and x[0] == 'ps']
        if pr:
            writes = list(writes) + pr
        self._wait(e, self._deps(reads, writes))
        c = self._eclock(e)
        ins = fn()
        ins.then_inc(self.sem[c], 1)
        self.cnt[c] += 1
        self.ninst += 1
        self._record((c, self.cnt[c]), e, reads, writes)

    def dma(self, out, in_, reads=(), writes=(), q='sp', is_output=False, **kw):
        i = self.dma_i[q]
        self.dma_i[q] = i + 1
        slot = self._clock(('d' + q, i % NSLOT))
        deps = self._deps(reads, writes)
        if self.cnt[slot] > 0:
            deps.add((slot, self.cnt[slot]))
        self._wait(q, deps)
        ins = self.eng[q].dma_start(out=out, in_=in_, **kw)
        ins.then_inc(self.sem[slot], 16)
        self.cnt[slot] += 16
        self.ninst += 1
        ev = (slot, self.cnt[slot])
        self._record(ev, q, reads, writes)
        if is_output:
            self.out_events.append(ev)

    def finish(self):
        deps = set(self.out_events)
        for k, w in self.last_w.items():
            deps.add(w)
        self._wait('sp', deps)

    def barrier(self):
        allc = [(c, v) for c, v in self.cnt.items() if v > 0]
        for e in self.eng:
            self._wait(e, allc)


S = 4096
D = 1024
NT = 32
NEG = -30000.0
import ml_dtypes
from contextlib import ExitStack

WZ_U, WZ_Q, WZ_QS, WZ_K, WZ_KS, WZ_VC, WZ_LRU, WZ_SC, WZ_GM, WZ_END = 0, 256, 512, 768, 960, 1152, 1216, 1728, 2496, 6592


def _col(v, nt):
    return np.ascontiguousarray(np.asarray(v, np.float32).reshape(nt, 128).T)


def host_consts():
    c = {}
    c["ident"] = np.eye(128, dtype=np.float32)
    c["identb"] = np.eye(128, dtype=np.float32).astype(ml_dtypes.bfloat16)
    inv = (10000.0 ** (-np.arange(0, 64, 2, dtype=np.float32) / np.float32(64))).astype(np.float32)
    ang = (np.arange(S, dtype=np.float32)[:, None] * inv[None, :]).astype(np.float32)
    cs, sn = np.cos(ang).astype(np.float32).T, np.sin(ang).astype(np.float32).T
    c["ropeC"] = np.ascontiguousarray(np.concatenate([cs, cs], 0))
    c["ropeS"] = np.ascontiguousarray(np.concatenate([-sn, sn], 0))
    p = np.arange(128)[:, None, None]
    r = np.arange(4)[None, :, None]
    f = np.arange(512)[None, None, :]
    c["caus"] = np.where(128 * r + p <= f, 0.0, NEG).astype(ml_dtypes.bfloat16)
    c["wlow"] = np.where(128 * r + p > f, 0.0, NEG).astype(ml_dtypes.bfloat16)
    n = 128 * np.arange(2)[None, :, None] + p
    t = np.arange(S)[None, None, :]
    c["cmpb"] = np.where((n <= 254) & (16 * n + 31 <= t), 0.0, NEG).astype(ml_dtypes.bfloat16)
    j = np.arange(64)[:, None, None]
    kt = np.arange(32)[None, :, None]
    s_ = np.arange(128)[None, None, :]
    c["xpf"] = np.ascontiguousarray((j == 2 * kt + s_ // 64).astype(np.float32).astype(ml_dtypes.bfloat16).reshape(64, S))
    nn = np.arange(256)
    jj = np.arange(64)
    ov = np.clip(np.minimum(nn[:, None] * 16 + 32, jj[None, :] * 64 + 64) - np.maximum(nn[:, None] * 16, jj[None, :] * 64), 0, None) / 32.0
    ov[255] = 0
    c["ovl"] = np.ascontiguousarray(ov.reshape(2, 128, 64).transpose(1, 0, 2)).astype(np.float32)
    tt = np.arange(S)
    cur = tt // 64
    blk = np.arange(64)[None, :]
    forced = (blk == 0) | (blk == cur[:, None]) | (blk == cur[:, None] - 1)
    future = (blk * 64) > tt[:, None]
    A = (~forced & ~future).astype(np.float32)
    Bc = np.where(future, -1.0, np.where(forced, 1.0e4, 0.0)).astype(np.float32)
    c["impA"] = np.ascontiguousarray(A.reshape(32, 128, 64).transpose(1, 0, 2))
    c["impB"] = np.ascontiguousarray(Bc.reshape(32, 128, 64).transpose(1, 0, 2))
    c["iota"] = np.ascontiguousarray(np.broadcast_to(np.arange(513, dtype=np.float32), (128, 513)))
    return c


def host_layer(P, l):
    o = {}
    f32 = np.float32
    o["modw"] = P["mod_w"][l]
    o["modbc"] = _col(P["mod_b"][l], 48)
    o["modbr"] = np.ascontiguousarray(P["mod_b"][l].reshape(1, 6144))
    o["nmg"] = _col(P["norm_mix_g"][l], 8)
    o["nfg"] = _col(P["norm_ffn_g"][l], 8)
    w = P["w_in"][l]
    pts = np.cumsum((256, 256, 64, 64, 64, 64, 64, 64, 12, 256, 256, 256, 256, 256, 4096))[:-1]
    u, q, kc, vc, ks, vs, kw, vw, gn, xl, gl, bs, cs_, xs, gm = np.split(w, pts, axis=1)

    def sw(m):
        m4 = m.reshape(1024, -1, 2, 32)
        return m4[:, :, ::-1, :].reshape(1024, -1)
    k3 = np.concatenate([kc, ks, kw], 1)
    o["wz"] = np.ascontiguousarray(np.concatenate([u, q, sw(q), k3, sw(k3), vc, xl, gl, bs, cs_, xs, gm], 1))
    o["wzt"] = np.ascontiguousarray(np.concatenate([vc, vs, vw, gn], 1))
    def stcol(a):
        return np.ascontiguousarray(a.reshape(8, 128).T)
    o["s5lr"] = stcol(P["s5_lambda_re"][l])
    o["s5li"] = stcol(P["s5_lambda_im"][l])
    o["s5ls"] = stcol(np.repeat(P["s5_log_step"][l][:, None], 64, 1))
    Bp = np.zeros((2, 8, 128, 128), f32)
    Cp = np.zeros((2, 8, 128, 128), f32)
    for g in range(16):
        j, gl_ = g // 2, g % 2
        c0 = 32 * (j % 4) + gl_ * 16
        for ri, (bb, cc) in enumerate(((P["s5_b_re"][l], P["s5_c_re"][l]), (P["s5_b_im"][l], P["s5_c_im"][l]))):
            Bp[ri, j, c0:c0 + 16, gl_ * 64:gl_ * 64 + 64] = bb[g].T
            Cp[ri, j, gl_ * 64:gl_ * 64 + 64, c0:c0 + 16] = cc[g].T
    o["s5B"] = Bp
    o["s5C"] = Cp
    o["s5d"] = _col(P["s5_d"][l], 2)
    o["s5glu"] = P["s5_w_glu"][l]
    o["pekT"] = np.ascontiguousarray(P["nsa_pe_k"][l].T)
    o["pevT"] = np.ascontiguousarray(P["nsa_pe_v"][l].T)
    o["ckw1"] = P["nsa_cmp_k_w1"][l]
    o["ckw2"] = P["nsa_cmp_k_w2"][l]
    o["cvw1"] = P["nsa_cmp_v_w1"][l]
    o["cvw2"] = P["nsa_cmp_v_w2"][l]
    o["lrucw"] = np.ascontiguousarray(P["lru_conv_w"][l].reshape(4, 2, 128).transpose(2, 1, 0))
    o["lrucb"] = _col(P["lru_conv_b"][l], 2)
    for nm, src in (("lruwa", P["lru_w_a"][l]), ("lruwx", P["lru_w_x"][l])):
        m = np.zeros((2, 128, 128), f32)
        for nb in range(8):
            ct, bl = nb // 4, nb % 4
            m[ct, bl * 32:bl * 32 + 32, bl * 32:bl * 32 + 32] = src[nb]
        o[nm] = m
    o["lruba"] = _col(P["lru_b_a"][l], 2)
    o["lrubx"] = _col(P["lru_b_x"][l], 2)
    o["lrulam"] = _col(P["lru_lambda"][l], 2)
    o["sccw"] = np.ascontiguousarray(P["sc_conv_w"][l].reshape(3, 2, 128).transpose(2, 1, 0))
    o["wbr"] = P["w_branch"][l]
    o["wout"] = P["w_out"][l]
    return o


_UID = [0]


def _sbt(nc, name, shape, dtype):
    _UID[0] += 1
    return nc.sbuf_tensor("%s_u%d" % (name, _UID[0]), shape, dtype)


class Ring:
    def __init__(self, nc, es, name, shape, dtype, n):
        self.tiles = [es.enter_context(_sbt(nc, "sb_%s%d" % (name, i), shape, dtype)) for i in range(n)]
        self.keys = [(name, i) for i in range(n)]
        self.i = 0

    def next(self):
        j = self.i % len(self.tiles)
        self.i += 1
        return self.tiles[j], self.keys[j]


class Prog:
    def __init__(self, nc, dbg=()):
        self.nc = nc
        self.k = K(nc)
        self.dbg = set(dbg)
        self.din = {}
        self.dram = {}
        self.ps = [nc.alloc_psum_tensor("psb%d" % i, [128, 512], F32) for i in range(8)]
        self.psk = [('ps', i) for i in range(8)]
        self.alt = 0

    def inp(self, name, arr):
        shape = list(arr.shape)
        dt = BF16 if arr.dtype == ml_dtypes.bfloat16 else F32
        self.din[name] = nc_ap = self.nc.dram_tensor(name, shape, dt, kind="ExternalInput").ap()
        return nc_ap

    def scratch(self, name, shape, dt):
        kind = "ExternalOutput" if name in self.dbg else "Internal"
        t = self.nc.dram_tensor(name, shape, dt, kind=kind).ap()
        self.dram[name] = t
        return t

    def evac_engine(self):
        self.alt += 1
        return 'dve' if self.alt % 2 else 'act'

    def copy(self, e, out, in_, reads, writes):
        nc = self.nc
        if e == 'act':
            self.k.op('act', lambda: nc.scalar.copy(out=out, in_=in_), reads, writes)
        elif e == 'dve':
            self.k.op('dve', lambda: nc.vector.tensor_copy(out=out, in_=in_), reads, writes)
        else:
            self.k.op('pool', lambda: nc.gpsimd.tensor_copy(out=out, in_=in_), reads, writes)


def stage_mod(pg, es, L, ccol_d, lay):
    nc, k = pg.nc, pg.k
    vcol = es.enter_context(_sbt(nc, "sb_vcol", [128, L, 4, 8], F32))
    grow = es.enter_context(_sbt(nc, "sb_grow", [128, L, 2, 1024], F32))
    with ExitStack() as st:
        cc = st.enter_context(_sbt(nc, "sb_m_cc", [128, 8], F32))
        sc = st.enter_context(_sbt(nc, "sb_m_sc", [128, 8], F32))
        cb = st.enter_context(_sbt(nc, "sb_m_cb", [128, 8, 128], F32))
        mw = Ring(nc, st, "m_mw", [128, 8, 1024], F32, 2)
        mbc = st.enter_context(_sbt(nc, "sb_m_mbc", [128, 48], F32))
        mbr = st.enter_context(_sbt(nc, "sb_m_mbr", [128, 1024], F32))
        ng = st.enter_context(_sbt(nc, "sb_m_ng", [128, 8], F32))
        raw = st.enter_context(_sbt(nc, "sb_m_raw", [128, 6, 8], F32))
        rowt = st.enter_context(_sbt(nc, "sb_m_rowt", [128, 1024], F32))
        k.dma(cc[:], ccol_d, writes=['m_cc'])
        k.op('act', lambda: nc.scalar.activation(out=sc[:], in_=cc[:], func=AF.Silu), ['m_cc'], ['m_sc'])
        for kc in range(8):
            k.op('dve', lambda kc=kc: nc.vector.tensor_copy(out=cb[:, kc, :], in_=sc[:, kc:kc + 1].to_broadcast([128, 128])), ['m_sc'], [('m_cb', kc)])
        for l in range(L):
            d = lay[l]
            k.dma(mbc[:], d["modbc"], writes=['m_mbc'])
            for v in range(6):
                wt, wk = mw.next()
                k.dma(wt[:], d["modw"].rearrange("(kc p) n -> p kc n", p=128)[:, :, v * 1024:(v + 1) * 1024], writes=[wk])
                if v in (2, 5):
                    gi = 0 if v == 2 else 1
                    k.dma(mbr[:], d["modbr"][:, v * 1024:(v + 1) * 1024].to_broadcast([128, 1024]), writes=['m_mbr'])
                    for hf in range(2):
                        ps, pk = pg.ps[hf], pg.psk[hf]
                        for kc in range(8):
                            k.op('pe', lambda kc=kc, ps=ps, hf=hf, wt=wt: nc.tensor.matmul(ps[:, :], lhsT=cb[:, kc, :], rhs=wt[:, kc, hf * 512:(hf + 1) * 512], start=(kc == 0), stop=(kc == 7)),
                                 [wk, ('m_cb', kc)], [pk])
                        k.op('dve', lambda ps=ps, hf=hf, gi=gi, l=l: nc.vector.tensor_tensor(out=grow[:, l, gi, hf * 512:(hf + 1) * 512], in0=ps[:, :], in1=mbr[:, hf * 512:(hf + 1) * 512], op=ALU.add),
                             [pk, 'm_mbr'], [('grow', l, gi, hf)])
                else:
                    for hf in range(2):
                        ps, pk = pg.ps[2 + hf], pg.psk[2 + hf]
                        for kc in range(8):
                            k.op('pe', lambda kc=kc, ps=ps, hf=hf, wt=wt: nc.tensor.matmul(ps[:, :], lhsT=cb[:, kc, :], rhs=wt[:, kc, hf * 512:(hf + 1) * 512], start=(kc == 0), stop=(kc == 7)),
                                 [wk, ('m_cb', kc)], [pk])
                        if hf == 0:
                            k.op('dve', lambda ps=ps: nc.vector.tensor_copy(out=rowt[:, 0:512], in_=ps[:, :]), [pk], [('m_rowt', 0)])
                        else:
                            k.op('act', lambda ps=ps: nc.scalar.copy(out=rowt[:, 512:1024], in_=ps[:, :]), [pk], [('m_rowt', 1)])
                    for b_ in range(2):
                        ps, pk = pg.ps[6 + b_], pg.psk[6 + b_]
                        for j in range(4):
                            kc = 4 * b_ + j
                            k.op('pe', lambda kc=kc, j=j, ps=ps: nc.tensor.transpose(out=ps[:, j * 128:(j + 1) * 128], in_=rowt[:, kc * 128:(kc + 1) * 128], identity=pg.ident[:]),
                                 [('m_rowt', b_), 'ident'], [pk])
                        k.op('dve', lambda ps=ps, v=v, b_=b_: nc.vector.tensor_tensor(out=raw[:, v, 4 * b_:4 * b_ + 4], in0=ps[:, 0:512:128], in1=mbc[:, v * 8 + 4 * b_:v * 8 + 4 * b_ + 4], op=ALU.add),
                             [pk, 'm_mbc'], [('m_raw', v, b_)])
            for half, (vs, vsh, gname) in enumerate(((1, 0, "nmg"), (4, 3, "nfg"))):
                k.dma(ng[:], d[gname], writes=['m_ng'])
                rk_s = [('m_raw', vs, 0), ('m_raw', vs, 1)]
                rk_h = [('m_raw', vsh, 0), ('m_raw', vsh, 1)]
                k.op('dve', lambda vs=vs: nc.vector.tensor_scalar(out=raw[:, vs, :], in0=raw[:, vs, :], scalar1=1.0, scalar2=None, op0=ALU.add), rk_s, rk_s)
                k.op('dve', lambda vs=vs, half=half, l=l: nc.vector.tensor_tensor(out=vcol[:, l, 2 * half, :], in0=raw[:, vs, :], in1=ng[:], op=ALU.mult), rk_s + ['m_ng'], [('vcol', l, 2 * half)])
                k.op('dve', lambda vsh=vsh, half=half, l=l: nc.vector.tensor_copy(out=vcol[:, l, 2 * half + 1, :], in_=raw[:, vsh, :]), rk_h, [('vcol', l, 2 * half + 1)])
        k.barrier()
    return vcol, grow


def norm_tiles(pg, rings, xsrc, tts, hT, hcol0, gm, sh, gmk, shk, hkeyf, router=None):
    for _ in norm_tiles_gen(pg, rings, xsrc, tts, hT, hcol0, gm, sh, gmk, shk, hkeyf, router=router):
        pass


def norm_tiles_gen(pg, rings, xsrc, tts, hT, hcol0, gm, sh, gmk, shk, hkeyf, router=None, pbase=0):
    nc, k = pg.nc, pg.k
    xr, sqr, xnr, str_ = rings["x"], rings["sq"], rings["xn"], rings["st"]
    tts = list(tts)
    n = len(tts)
    stt = {}

    def part1(i):
        tt = tts[i]
        xt, xk = xr.next()
        k.dma(xt[:], xsrc[tt * 128:(tt + 1) * 128, :], writes=[xk])
        sq, sqk = sqr.next()
        st, stk = str_.next()
        k.op('act', lambda: nc.scalar.activation(out=sq[:], in_=xt[:], func=AF.Square, accum_out=st[:, 0:1]), [xk], [sqk, (stk, 0)])
        k.op('dve', lambda: nc.vector.tensor_scalar(out=st[:, 1:2], in0=st[:, 0:1], scalar1=1.0 / D, scalar2=1e-6, op0=ALU.mult, op1=ALU.add), [(stk, 0)], [(stk, 1)])
        k.op('act', lambda: nc.scalar.activation(out=st[:, 2:3], in_=st[:, 1:2], func=AF.Sqrt), [(stk, 1)], [(stk, 2)])
        k.op('dve', lambda: nc.vector.reciprocal(out=st[:, 3:4], in_=st[:, 2:3]), [(stk, 2)], [(stk, 3)])
        xn, xnk = xnr.next()
        k.op('dve', lambda: nc.vector.tensor_scalar(out=xn[:], in0=xt[:], scalar1=st[:, 3:4], scalar2=None, op0=ALU.mult), [xk, (stk, 3)], [xnk])
        stt[i] = dict(xn=(xn, xnk))

    def part2(i):
        xn, xnk = stt[i]["xn"]
        c0 = hcol0 + i * 128
        h32 = None
        if router is not None:
            h32, h32k = router["h32"].next()
            stt[i]["h32"] = (h32, h32k)
        for half in range(2):
            pb = pbase + (2 * i + half) % 4
            ps, pk = pg.ps[pb], pg.psk[pb]
            for j in range(4):
                kc = half * 4 + j
                k.op('pe', lambda: nc.tensor.transpose(out=ps[:, j * 128:(j + 1) * 128], in_=xn[:, kc * 128:(kc + 1) * 128], identity=pg.ident[:]), [xnk, 'ident'], [pk])
            for j in range(4):
                kc = half * 4 + j
                e = 'dve' if half == 0 else 'act'
                dst = hT[:, kc, c0:c0 + 128] if router is None else h32[:, kc, :]
                dkey = hkeyf(kc, c0) if router is None else (h32k, kc)
                if e == 'dve':
                    k.op('dve', lambda: nc.vector.tensor_scalar(out=dst, in0=ps[:, j * 128:(j + 1) * 128], scalar1=gm[:, kc:kc + 1], scalar2=sh[:, kc:kc + 1], op0=ALU.mult, op1=ALU.add),
                         [pk, gmk, shk], [dkey])
                else:
                    k.op('act', lambda: nc.scalar.activation(out=dst, in_=ps[:, j * 128:(j + 1) * 128], func=AF.Identity, bias=sh[:, kc:kc + 1], scale=gm[:, kc:kc + 1]),
                         [pk, gmk, shk], [dkey])
                if router is not None:
                    k.op('pool', lambda: nc.gpsimd.tensor_copy(out=hT[:, kc, c0:c0 + 128], in_=h32[:, kc, :]), [(h32k, kc)], [hkeyf(kc, c0)])

    def part3(i):
        if router is None:
            del stt[i]
            return
        h32, h32k = stt[i]["h32"]
        ps, pk = pg.ps[4 + (i % 2)], pg.psk[4 + (i % 2)]
        for kc in range(8):
            k.op('pe', lambda: nc.tensor.matmul(ps[:, 0:8], lhsT=h32[:, kc, :], rhs=router["w"][:, kc, :], start=(kc == 0), stop=(kc == 7)), [(h32k, kc), 'rw'], [pk])
        k.op('dve', lambda: nc.vector.tensor_tensor(out=router["logits"][:, i, :], in0=ps[:, 0:8], in1=router["b"][:, :], op=ALU.add), [pk, 'rb'], [('logits', i)])
        del stt[i]

    for s_ in range(n + 2):
        if s_ < n:
            part1(s_)
        if 0 <= s_ - 1 < n:
            part2(s_ - 1)
        if 0 <= s_ - 2 < n:
            part3(s_ - 2)
        yield s_


def mk_norm_rings(nc, es):
    return {"x": Ring(nc, es, "n_x", [128, 1024], F32, 2), "sq": Ring(nc, es, "n_sq", [128, 1024], F32, 1),
            "xn": Ring(nc, es, "n_xn", [128, 1024], F32, 2), "st": Ring(nc, es, "n_st", [128, 4], F32, 3)}


def stage_proj(pg, hT, hkey, d, sc):
    nc, k = pg.nc, pg.k
    wz = d["wz"].rearrange("(kc p) n -> p kc n", p=128)
    with ExitStack() as es:
        wr = Ring(nc, es, "z_w", [128, 8, 512], BF16, 2)
        o32 = Ring(nc, es, "z_o32", [128, 512], F32, 3)
        o16 = Ring(nc, es, "z_o16", [128, 512], BF16, 3)
        t32 = Ring(nc, es, "z_t32", [128, 512], F32, 4)
        rC = es.enter_context(_sbt(nc, "sb_z_rC", [128, S], F32))
        rS = es.enter_context(_sbt(nc, "sb_z_rS", [128, S], F32))
        for hh in range(2):
            k.dma(rC[hh * 64:(hh + 1) * 64, :], pg.din["ropeC"], writes=[('rC', hh)])
            k.dma(rS[hh * 64:(hh + 1) * 64, :], pg.din["ropeS"], writes=[('rS', hh)])
        rCk = [('rC', 0), ('rC', 1)]
        rSk = [('rS', 0), ('rS', 1)]
        psi = [0]

        def mm(wt, wk, c0, m, qc, pb=None):
            if pb is None:
                pb = psi[0] % 6
                psi[0] += 1
            ps, pk = pg.ps[pb], pg.psk[pb]
            for kc in range(8):
                k.op('pe', lambda kc=kc: nc.tensor.matmul(ps[0:m, :], lhsT=wt[:, kc, c0:c0 + m], rhs=hT[:, kc, qc * 512:(qc + 1) * 512], start=(kc == 0), stop=(kc == 7)),
                     [wk] + [hkey(kc, qc * 512 + j * 128) for j in range(4)], [pk])
            return ps, pk

        def load(c0, n):
            wt, wk = wr.next()
            k.dma(wt[:, :, 0:n], wz[:, :, c0:c0 + n], writes=[wk], q='pool')
            return wt, wk

        for (name, c0, n) in (("zu", WZ_U, 256), ("zlru", WZ_LRU, 512), ("zsc", WZ_SC, 384), ("zsc", WZ_SC + 384, 384)):
            wt, wk = load(c0, n)
            r0 = 384 if (name == "zsc" and c0 != WZ_SC) else 0
            for qc in range(8):
                for t in range(n // 128):
                    ps, pk = mm(wt, wk, t * 128, 128, qc)
                    ot, ok = o32.next()
                    pg.copy(pg.evac_engine(), ot[:], ps[:, :], [pk], [ok])
                    k.dma(sc[name][r0 + t * 128:r0 + (t + 1) * 128, qc * 512:(qc + 1) * 512], ot[:], reads=[ok])
        for (name, cm, cs, nh) in (("zq", WZ_Q, WZ_QS, 4), ("zk", WZ_K, WZ_KS, 3)):
            wt, wk = load(cm, 512 if nh == 4 else 448)
            dstv = sc[name].rearrange("h p t -> (h p) t")
            units = [(0, 128), (128, 128)] if nh == 4 else [(0, 128), (128, 64)]
            for qc in range(8):
                for (c0, m) in units:
                    psA, pkA = mm(wt, wk, c0, m, qc)
                    psB, pkB = mm(wt, wk, (cs - cm) + c0, m, qc)
                    ta, tak = t32.next()
                    tb, tbk = t32.next()
                    k.op('dve', lambda: nc.vector.tensor_tensor(out=ta[0:m, :], in0=psA[0:m, :], in1=rC[0:m, qc * 512:(qc + 1) * 512], op=ALU.mult), [pkA] + rCk, [tak])
                    k.op('dve', lambda: nc.vector.tensor_tensor(out=tb[0:m, :], in0=psB[0:m, :], in1=rS[0:m, qc * 512:(qc + 1) * 512], op=ALU.mult), [pkB] + rSk, [tbk])
                    ot, ok = o16.next()
                    k.op('pool', lambda: nc.gpsimd.tensor_tensor(out=ot[0:m, :], in0=ta[0:m, :], in1=tb[0:m, :], op=ALU.add), [tak, tbk], [ok])
                    k.dma(dstv[c0:c0 + m, qc * 512:(qc + 1) * 512], ot[0:m, :], reads=[ok])
                if nh == 3:
                    ps, pk = mm(wt, wk, WZ_VC - WZ_K, 64, qc)
                    ot, ok = o16.next()
                    pg.copy(pg.evac_engine(), ot[0:64, :], ps[0:64, :], [pk], [ok])
                    k.dma(sc["zvc"][:, qc * 512:(qc + 1) * 512], ot[0:64, :], reads=[ok])
        for g in range(8):
            wt, wk = load(WZ_GM + g * 512, 512)
            for qc in range(8):
                for t in range(4):
                    ps, pk = mm(wt, wk, t * 128, 128, qc)
                    ot, ok = o16.next()
                    k.op('act', lambda: nc.scalar.activation(out=ot[:], in_=ps[:, :], func=AF.Sigmoid), [pk], [ok])
                    r = g * 512 + t * 128
                    k.dma(sc["zgm"][r:r + 128, qc * 512:(qc + 1) * 512], ot[:], reads=[ok])
        wt, wk = wr.next()
        k.dma(wt[:, :, 0:204], d["wzt"].rearrange("(kc p) n -> p kc n", p=128), writes=[wk], q='pool')
        for tt in range(NT):
            pb = 6 + tt % 2
            ps, pk = pg.ps[pb], pg.psk[pb]
            for kc in range(8):
                k.op('pe', lambda kc=kc: nc.tensor.matmul(ps[:, 0:204], lhsT=hT[:, kc, tt * 128:(tt + 1) * 128], rhs=wt[:, kc, 0:204], start=(kc == 0), stop=(kc == 7)),
                     [wk, hkey(kc, tt * 128)], [pk])
            ot, ok = o16.next()
            pg.copy('dve', ot[:, 0:192], ps[:, 0:192], [pk], [ok])
            k.dma(sc["zv"][tt * 128:(tt + 1) * 128, :], ot[:, 0:192], reads=[ok])
            og, ogk = o32.next()
            k.op('act', lambda: nc.scalar.activation(out=og[:, 0:12], in_=ps[:, 192:204], func=AF.Sigmoid), [pk], [ogk])
            k.dma(sc["zg"][tt * 128:(tt + 1) * 128, :], og[:, 0:12], reads=[ogk])
        k.barrier()


def build(host_in, stages, dbg=()):
    nc = bass.Bass("TRN2", target_bir_lowering=False)
    pg = Prog(nc, dbg)
    k = pg.k
    for name, arr in host_in.items():
        pg.inp(name, arr)
    L = 2
    lay = [{kk[3:]: v for kk, v in pg.din.items() if kk.startswith("L%d_" % l)} for l in range(L)]
    sc = {}
    for name, shape, dt in (("xa", [S, D], F32), ("xb", [S, D], F32), ("zu", [256, S], F32), ("zq", [4, 64, S], BF16),
                            ("zk", [3, 64, S], BF16), ("zvc", [64, S], BF16), ("zv", [S, 192], BF16), ("zg", [S, 12], F32),
                            ("zlru", [512, S], F32), ("zsc", [768, S], F32), ("zgm", [4096, S], BF16), ("yT", [4, 256, S], BF16)):
        sc[name] = pg.scratch(name, shape, dt)
    out_d = nc.dram_tensor("out", [S, D], F32, kind="ExternalOutput").ap()
    with ExitStack() as es:
        pg.ident = es.enter_context(_sbt(nc, "sb_ident", [128, 128], F32))
        pg.identb = es.enter_context(_sbt(nc, "sb_identb", [128, 128], BF16))
        k.dma(pg.ident[:], pg.din["ident"], writes=['ident'])
        k.dma(pg.identb[:], pg.din["identb"], writes=['identb'])
        vcol, grow = stage_mod(pg, es, L, pg.din["ccol"], lay)
        xsrc = pg.din["x"]
        if "vcol" in pg.dbg:
            dv = nc.dram_tensor("vcol", [128, L, 4, 8], F32, kind="ExternalOutput").ap()
            dg = nc.dram_tensor("grow", [128, L, 2, 1024], F32, kind="ExternalOutput").ap()
            k.dma(dv, vcol[:], reads=[('vcol', l_, i_) for l_ in range(L) for i_ in range(4)], is_output=True)
            k.dma(dg, grow[:], reads=[('grow', l_, g_, h_) for l_ in range(L) for g_ in range(2) for h_ in range(2)], is_output=True)
        for l in range(L):
            if ("stop", l, "mod") in stages:
                break
            d = lay[l]
            with ExitStack() as ls:
                hT = ls.enter_context(_sbt(nc, "sb_hT", [128, 8, S], BF16))
                hkey = lambda kc, c: ('hT', kc, c // 128)
                with ExitStack() as ns:
                    rings = mk_norm_rings(nc, ns)
                    norm_tiles(pg, rings, xsrc, range(NT), hT, 0, vcol[:, l, 0, :], vcol[:, l, 1, :], ('vcol', l, 0), ('vcol', l, 1), hkey)
                    k.barrier()
                if "hT" in pg.dbg and l == 0:
                    dh = nc.dram_tensor("hT", [128, 8, S], BF16, kind="ExternalOutput").ap()
                    k.dma(dh, hT[:], is_output=True)
                    k.barrier()
                if ("stop", l, "norm") in stages:
                    break
                stage_proj(pg, hT, hkey, d, sc)
            if ("stop", l, "proj") in stages:
                break
            xmid = sc["xa"]
            xnext = sc["xb"]
            if "s5" not in stages.get("skip", ()) if isinstance(stages, dict) else True:
                pass
            skip = SKIP
            if "s5" not in skip:
                stage_s5(pg, d, sc)
            if "nsa" not in skip:
                stage_nsa(pg, d, sc)
            if "lru" not in skip:
                stage_lru(pg, d, sc)
            if "sc" not in skip:
                stage_sc(pg, d, sc)
            if ("stop", l, "mix") in stages:
                break
            stage_merge(pg, d, sc, xsrc, xmid, grow[:, l, :, :])
            if ("stop", l, "merge") in stages:
                break
            if l % 2 == 0:
                ex = [(pg.din["ffn_wg"], pg.din["ffn_wu"], pg.din["ffn_wd"])]
                stage_ffn(pg, xmid, xnext, vcol[:, l, 2, :], vcol[:, l, 3, :], ('vcol', l, 2), ('vcol', l, 3), grow[:, l, 1, :], ex, 2816)
            else:
                ex = [(pg.din["moe_wg"][e], pg.din["moe_wu"][e], pg.din["moe_wd"][e]) for e in range(8)]
                stage_ffn(pg, xmid, xnext, vcol[:, l, 2, :], vcol[:, l, 3, :], ('vcol', l, 2), ('vcol', l, 3), grow[:, l, 1, :], ex, 1408,
                          router={"w": pg.din["moe_rw"], "b": pg.din["moe_rb"]}, final=((out_d, pg.din["fin_g"]) if l == L - 1 else None))
            xsrc = xnext
            sc["xa"], sc["xb"] = sc["xb"], sc["xa"]
            sc["xa"], sc["xb"] = sc["xb"], sc["xa"]
            if ("stop", l, "ffn") in stages:
                break
        k.barrier()
    k.finish()
    return nc, pg


SKIP = set()

I32 = mybir.dt.int32
TWO_PI = float(2 * np.pi)


def _wrap_half(pg, f, m, n_reads, key):
    nc, k = pg.nc, pg.k
    k.op('dve', lambda: nc.vector.tensor_scalar(out=m, in0=f, scalar1=0.5, scalar2=None, op0=ALU.is_gt), [key], ['s_MK'])
    k.op('dve', lambda: nc.vector.tensor_tensor(out=f, in0=f, in1=m, op=ALU.subtract), [key, 's_MK'], [key])
    k.op('dve', lambda: nc.vector.tensor_scalar(out=m, in0=f, scalar1=-0.5, scalar2=None, op0=ALU.is_lt), [key], ['s_MK'])
    k.op('dve', lambda: nc.vector.tensor_tensor(out=f, in0=f, in1=m, op=ALU.add), [key, 's_MK'], [key])


S5_ENG = ['dve', 'dve', 'dve', 'dve', 'dve']


def S5_E(nc, i):
    return nc.vector if S5_ENG[i] == 'dve' else nc.gpsimd


def stage_s5(pg, d, sc):
    nc, k = pg.nc, pg.k
    TC = 512
    NTAB = 513
    with ExitStack() as es:
        sbt = lambda name, shape, dt=F32: es.enter_context(_sbt(nc, "sb_s_" + name, shape, dt))
        uT = sbt("uT", [128, 2, S], BF16)
        k.dma(uT[:], sc["zu"].rearrange("(ct p) t -> p ct t", p=128), writes=['s_uT'], q='pool')
        Bw = sbt("Bw", [128, 2, 8, 128], BF16)
        Cw = sbt("Cw", [128, 2, 8, 128], BF16)
        k.dma(Bw[:], d["s5B"].rearrange("r j p n -> p r j n"), writes=['s_Bw'], q='pool')
        k.dma(Cw[:], d["s5C"].rearrange("r j p n -> p r j n"), writes=['s_Cw'], q='pool')
        k.op('pool', lambda: nc.gpsimd.tensor_scalar(out=Cw[:, 1, :, :], in0=Cw[:, 1, :, :], scalar1=-1.0, scalar2=None, op0=ALU.mult), ['s_Cw'], ['s_Cw'])
        NCw = sbt("NCw", [128, 8, 128], BF16)
        k.op('pool', lambda: nc.gpsimd.tensor_scalar(out=NCw[:], in0=Cw[:, 0, :, :], scalar1=-1.0, scalar2=None, op0=ALU.mult), ['s_Cw'], ['s_NCw'])
        nid = sbt("nid", [128, 128], BF16)
        k.op('pool', lambda: nc.gpsimd.tensor_scalar(out=nid[:], in0=pg.identb[:], scalar1=-1.0, scalar2=None, op0=ALU.mult), ['identb'], ['s_nid'])
        glu = sbt("glu", [128, 2, 256], BF16)
        k.dma(glu[:], d["s5glu"].rearrange("(ct p) n -> p ct n", p=128), writes=['s_glu'], q='pool')
        dcol = sbt("dcol", [128, 2])
        k.dma(dcol[:], d["s5d"], writes=['s_d'])
        pr = sbt("pr", [128, 16, 8])
        LR, LI, LS, DT, AR, TH, R_, THN, KR, KI, ER, EI, NEI, T1, T2, T3 = range(16)
        k.dma(pr[:, LR, :], d["s5lr"], writes=[('s_pr', LR)])
        k.dma(pr[:, LI, :], d["s5li"], writes=[('s_pr', LI)])
        k.dma(pr[:, LS, :], d["s5ls"], writes=[('s_pr', LS)])
        MRb = sbt("MRb", [128, 8, TC], BF16)
        MIb = sbt("MIb", [128, 8, TC], BF16)
        COSb = sbt("COSb", [128, 8, TC], BF16)
        SINb = sbt("SINb", [128, 8, TC], BF16)
        RT = sbt("RT", [128, 8, TC])

        def P_(i):
            return pr[:, i, :]

        def sm(op_, o, a, b_):
            k.op('dve', lambda: nc.vector.tensor_tensor(out=P_(o), in0=P_(a), in1=P_(b_), op=op_), [('s_pr', a), ('s_pr', b_)], [('s_pr', o)])
        k.op('act', lambda: nc.scalar.activation(out=P_(DT), in_=P_(LS), func=AF.Exp), [('s_pr', LS)], [('s_pr', DT)])
        sm(ALU.mult, AR, LR, DT)
        sm(ALU.mult, TH, LI, DT)
        k.op('act', lambda: nc.scalar.activation(out=P_(R_), in_=P_(AR), func=AF.Exp), [('s_pr', AR)], [('s_pr', R_)])
        k.op('dve', lambda: nc.vector.tensor_scalar(out=P_(THN), in0=P_(TH), scalar1=1.0 / TWO_PI, scalar2=None, op0=ALU.mult), [('s_pr', TH)], [('s_pr', THN)])
        with ExitStack() as ts:
            tbt = lambda name, shape, dt=F32: ts.enter_context(_sbt(nc, "sb_st_" + name, shape, dt))
            SIN = tbt("SIN", [128, 8, NTAB])
            COS = tbt("COS", [128, 8, NTAB])
            iot = tbt("iot", [128, NTAB])
            k.dma(iot[:], pg.din["iota"], writes=['s_iot'])
            FR = tbt("FR", [128, 8, NTAB])
            FC = tbt("FC", [128, 8, NTAB])
            MK = tbt("MK", [128, 8, NTAB])
            KI32 = tbt("ki", [128, 8, NTAB], I32)
            for j in range(8):
                k.op('dve', lambda: nc.vector.tensor_scalar(out=FR[:, j, :], in0=iot[:], scalar1=pr[:, THN, j:j + 1], scalar2=None, op0=ALU.mult), ['s_iot', ('s_pr', THN)], ['s_FR'])
            k.op('dve', lambda: nc.vector.tensor_copy(out=KI32[:], in_=FR[:]), ['s_FR'], ['s_KI'])
            k.op('dve', lambda: nc.vector.tensor_copy(out=MK[:], in_=KI32[:]), ['s_KI'], ['s_MK'])
            k.op('dve', lambda: nc.vector.tensor_tensor(out=FR[:], in0=FR[:], in1=MK[:], op=ALU.subtract), ['s_FR', 's_MK'], ['s_FR'])
            k.op('dve', lambda: nc.vector.tensor_scalar(out=FC[:], in0=FR[:], scalar1=0.25, scalar2=None, op0=ALU.add), ['s_FR'], ['s_FC'])
            _wrap_half(pg, FR[:], MK[:], None, 's_FR')
            _wrap_half(pg, FC[:], MK[:], None, 's_FC')
            k.op('act', lambda: nc.scalar.activation(out=SIN[:], in_=FR[:], func=AF.Sin, scale=TWO_PI), ['s_FR'], ['s_SIN'])
            k.op('act', lambda: nc.scalar.activation(out=COS[:], in_=FC[:], func=AF.Sin, scale=TWO_PI), ['s_FC'], ['s_COS'])
            k.op('dve', lambda: nc.vector.tensor_tensor(out=P_(T1), in0=P_(R_), in1=COS[:, :, 1], op=ALU.mult), [('s_pr', R_), 's_COS'], [('s_pr', T1)])
            k.op('dve', lambda: nc.vector.tensor_tensor(out=P_(T2), in0=P_(R_), in1=SIN[:, :, 1], op=ALU.mult), [('s_pr', R_), 's_SIN'], [('s_pr', T2)])
            k.op('dve', lambda: nc.vector.tensor_scalar(out=P_(T1), in0=P_(T1), scalar1=-1.0, scalar2=None, op0=ALU.add), [('s_pr', T1)], [('s_pr', T1)])
            sm(ALU.mult, T3, LR, LR)
            sm(ALU.mult, KR, LI, LI)
            sm(ALU.add, T3, T3, KR)
            k.op('dve', lambda: nc.vector.reciprocal(out=P_(T3), in_=P_(T3)), [('s_pr', T3)], [('s_pr', T3)])
            sm(ALU.mult, KR, T1, LR)
            sm(ALU.mult, KI, T2, LI)
            sm(ALU.add, KR, KR, KI)
            sm(ALU.mult, KR, KR, T3)
            sm(ALU.mult, KI, T2, LR)
            sm(ALU.mult, ER, T1, LI)
            sm(ALU.subtract, KI, KI, ER)
            sm(ALU.mult, KI, KI, T3)
            k.op('dve', lambda: nc.vector.tensor_copy(out=P_(ER), in_=COS[:, :, 512]), ['s_COS', ('s_pr', ER)], [('s_pr', ER)])
            k.op('dve', lambda: nc.vector.tensor_copy(out=P_(EI), in_=SIN[:, :, 512]), ['s_SIN'], [('s_pr', EI)])
            k.op('dve', lambda: nc.vector.tensor_scalar(out=P_(NEI), in0=P_(EI), scalar1=-1.0, scalar2=None, op0=ALU.mult), [('s_pr', EI)], [('s_pr', NEI)])
            k.op('dve', lambda: nc.vector.tensor_scalar(out=P_(T1), in0=P_(KR), scalar1=-1.0, scalar2=None, op0=ALU.mult), [('s_pr', KR), ('s_pr', T1)], [('s_pr', T1)])
            for j in range(8):
                mt = FR[:, j, 0:TC]
                k.op('dve', lambda: nc.vector.tensor_scalar(out=mt, in0=COS[:, j, 0:TC], scalar1=pr[:, KR, j:j + 1], scalar2=None, op0=ALU.mult), ['s_COS', ('s_pr', KR), 's_FR'], [('s_mt', j)])
                k.op('dve', lambda: nc.vector.scalar_tensor_tensor(out=MRb[:, j, :], in0=SIN[:, j, 0:TC], scalar=pr[:, KI, j:j + 1], in1=mt, op0=ALU.mult, op1=ALU.add), ['s_SIN', ('s_pr', KI), ('s_mt', j)], [('s_MR', j)])
                mt2 = FC[:, j, 0:TC]
                k.op('dve', lambda: nc.vector.tensor_scalar(out=mt2, in0=COS[:, j, 0:TC], scalar1=pr[:, KI, j:j + 1], scalar2=None, op0=ALU.mult), ['s_COS', ('s_pr', KI), 's_FC'], [('s_mt2', j)])
                k.op('dve', lambda: nc.vector.scalar_tensor_tensor(out=MIb[:, j, :], in0=SIN[:, j, 0:TC], scalar=pr[:, T1, j:j + 1], in1=mt2, op0=ALU.mult, op1=ALU.add), ['s_SIN', ('s_pr', T1), ('s_mt2', j)], [('s_MI', j)])
                k.op('pool', lambda: nc.gpsimd.tensor_copy(out=RT[:, j, :], in_=pr[:, R_, j:j + 1].to_broadcast([128, TC])), [('s_pr', R_)], [('s_RT', j)])
                k.op('act', lambda: nc.scalar.copy(out=COSb[:, j, :], in_=COS[:, j, 0:TC]), ['s_COS'], [('s_COSb', j)])
                k.op('act', lambda: nc.scalar.copy(out=SINb[:, j, :], in_=SIN[:, j, 0:TC]), ['s_SIN'], [('s_SINb', j)])
            k.barrier()
        gend = sbt("gend", [128, 8, 4])
        er = Ring(nc, es, "s_e", [128, TC], BF16, 6)
        ar_ = Ring(nc, es, "s_a", [128, TC], BF16, 14)
        gr_ = Ring(nc, es, "s_g", [128, TC], F32, 8)
        gbr = Ring(nc, es, "s_gb", [128, TC], BF16, 8)
        br4 = Ring(nc, es, "s_b4", [128, TC], BF16, 14)
        ur = Ring(nc, es, "s_u", [128, TC], F32, 2)
        yr = Ring(nc, es, "s_y", [128, TC], F32, 3)
        zr = Ring(nc, es, "s_z", [128, 2, TC], F32, 2)
        zbr = Ring(nc, es, "s_zb", [128, 2, TC], BF16, 2)
        obr = Ring(nc, es, "s_ob", [128, TC], BF16, 2)
        items = [(c, ct, jj) for c in range(S // TC) for ct in range(2) for jj in range(4)]
        st = {}
        zstate = {}

        def phA(w):
            c, ct, jj = w
            j = 4 * ct + jj
            sl = slice(c * TC, (c + 1) * TC)
            ps0, pk0 = pg.ps[0], pg.psk[0]
            ps1, pk1 = pg.ps[1], pg.psk[1]
            k.op('pe', lambda: nc.tensor.matmul(ps0[:, :], lhsT=Bw[:, 0, j, :], rhs=uT[:, ct, sl], start=True, stop=True), ['s_Bw', 's_uT'], [pk0])
            k.op('pe', lambda: nc.tensor.matmul(ps1[:, :], lhsT=Bw[:, 1, j, :], rhs=uT[:, ct, sl], start=True, stop=True), ['s_Bw', 's_uT'], [pk1])
            e0, e0k = er.next(); e1, e1k = er.next()
            k.op('act', lambda: nc.scalar.copy(out=e0[:], in_=ps0[:, :]), [pk0], [e0k])
            k.op('act', lambda: nc.scalar.copy(out=e1[:], in_=ps1[:, :]), [pk1], [e1k])
            a1, a1k = ar_.next(); a2, a2k = ar_.next(); a3, a3k = ar_.next(); a4, a4k = ar_.next()
            k.op('dve', lambda: nc.vector.tensor_tensor(out=a1[:], in0=e0[:], in1=MRb[:, j, :], op=ALU.mult), [e0k, ('s_MR', j)], [a1k])
            k.op('dve', lambda: nc.vector.tensor_tensor(out=a2[:], in0=e1[:], in1=MIb[:, j, :], op=ALU.mult), [e1k, ('s_MI', j)], [a2k])
            k.op('dve', lambda: nc.vector.tensor_tensor(out=a3[:], in0=e1[:], in1=MRb[:, j, :], op=ALU.mult), [e1k, ('s_MR', j)], [a3k])
            k.op('dve', lambda: nc.vector.tensor_tensor(out=a4[:], in0=e0[:], in1=MIb[:, j, :], op=ALU.mult), [e0k, ('s_MI', j)], [a4k])
            st[w] = dict(a=(a1, a1k, a2, a2k, a3, a3k, a4, a4k))

        bcnt = [0]

        def phB(w):
            a1, a1k, a2, a2k, a3, a3k, a4, a4k = st[w]["a"]
            pb = 2 + 2 * (bcnt[0] % 2)
            bcnt[0] += 1
            p1, p1k = pg.ps[pb], pg.psk[pb]
            p2, p2k = pg.ps[pb + 1], pg.psk[pb + 1]
            k.op('pe', lambda: nc.tensor.matmul(p1[:, :], lhsT=pg.identb[:], rhs=a1[:], start=True, stop=False), ['identb', a1k], [p1k])
            k.op('pe', lambda: nc.tensor.matmul(p1[:, :], lhsT=nid[:], rhs=a2[:], start=False, stop=True), ['s_nid', a2k], [p1k])
            k.op('pe', lambda: nc.tensor.matmul(p2[:, :], lhsT=pg.identb[:], rhs=a3[:], start=True, stop=False), ['identb', a3k], [p2k])
            k.op('pe', lambda: nc.tensor.matmul(p2[:, :], lhsT=pg.identb[:], rhs=a4[:], start=False, stop=True), ['identb', a4k], [p2k])
            st[w]["bp"] = (p1, p1k, p2, p2k)

        def phC(w):
            c, ct, jj = w
            j = 4 * ct + jj
            p1, p1k, p2, p2k = st[w]["bp"]
            g1, g1k = gr_.next(); g2, g2k = gr_.next()
            if c == 0:
                ir, ii = 0.0, 0.0
                ikeys = []
            else:
                k.op('dve', lambda: nc.vector.tensor_scalar(out=gend[:, j, 2:3], in0=gend[:, j, 0:1], scalar1=pr[:, ER, j:j + 1], scalar2=None, op0=ALU.mult), [('s_ge', j), ('s_pr', ER)], [('s_gi', j, 0)])
                k.op('dve', lambda: nc.vector.scalar_tensor_tensor(out=gend[:, j, 2:3], in0=gend[:, j, 1:2], scalar=pr[:, NEI, j:j + 1], in1=gend[:, j, 2:3], op0=ALU.mult, op1=ALU.add), [('s_ge', j), ('s_pr', NEI), ('s_gi', j, 0)], [('s_gi', j, 0)])
                k.op('dve', lambda: nc.vector.tensor_scalar(out=gend[:, j, 3:4], in0=gend[:, j, 0:1], scalar1=pr[:, EI, j:j + 1], scalar2=None, op0=ALU.mult), [('s_ge', j), ('s_pr', EI)], [('s_gi', j, 1)])
                k.op('dve', lambda: nc.vector.scalar_tensor_tensor(out=gend[:, j, 3:4], in0=gend[:, j, 1:2], scalar=pr[:, ER, j:j + 1], in1=gend[:, j, 3:4], op0=ALU.mult, op1=ALU.add), [('s_ge', j), ('s_pr', ER), ('s_gi', j, 1)], [('s_gi', j, 1)])
                ir, ii = gend[:, j, 2:3], gend[:, j, 3:4]
                ikeys = [('s_gi', j, 0), ('s_gi', j, 1)]
            k.op('dve', lambda: nc.vector.tensor_tensor_scan(out=g1[:], data0=RT[:, j, :], data1=p1[:, :], initial=ir, op0=ALU.mult, op1=ALU.add), [('s_RT', j), p1k] + ikeys, [g1k])
            k.op('dve', lambda: nc.vector.tensor_tensor_scan(out=g2[:], data0=RT[:, j, :], data1=p2[:, :], initial=ii, op0=ALU.mult, op1=ALU.add), [('s_RT', j), p2k] + ikeys, [g2k])
            k.op('act', lambda: nc.scalar.copy(out=gend[:, j, 0:1], in_=g1[:, TC - 1:TC]), [g1k] + ikeys, [('s_ge', j)])
            k.op('act', lambda: nc.scalar.copy(out=gend[:, j, 1:2], in_=g2[:, TC - 1:TC]), [g2k, ('s_ge', j)] + ikeys, [('s_ge', j)])
            gb1, gb1k = gbr.next(); gb2, gb2k = gbr.next()
            k.op('act', lambda: nc.scalar.copy(out=gb1[:], in_=g1[:]), [g1k], [gb1k])
            k.op('act', lambda: nc.scalar.copy(out=gb2[:], in_=g2[:]), [g2k], [gb2k])
            st[w]["gb"] = (gb1, gb1k, gb2, gb2k)

        def phD(w):
            c, ct, jj = w
            j = 4 * ct + jj
            gb1, gb1k, gb2, gb2k = st[w]["gb"]
            b1, b1k = br4.next(); b2, b2k = br4.next(); b3, b3k = br4.next(); b4, b4k = br4.next()
            k.op('dve', lambda: nc.vector.tensor_tensor(out=b1[:], in0=gb1[:], in1=COSb[:, j, :], op=ALU.mult), [gb1k, ('s_COSb', j)], [b1k])
            k.op('dve', lambda: nc.vector.tensor_tensor(out=b2[:], in0=gb2[:], in1=SINb[:, j, :], op=ALU.mult), [gb2k, ('s_SINb', j)], [b2k])
            k.op(S5_ENG[2], lambda: S5_E(nc, 2).tensor_tensor(out=b3[:], in0=gb1[:], in1=SINb[:, j, :], op=ALU.mult), [gb1k, ('s_SINb', j)], [b3k])
            k.op(S5_ENG[3], lambda: S5_E(nc, 3).tensor_tensor(out=b4[:], in0=gb2[:], in1=COSb[:, j, :], op=ALU.mult), [gb2k, ('s_COSb', j)], [b4k])
            st[w]["h"] = (b1, b1k, b2, b2k, b3, b3k, b4, b4k)

        def phE(w):
            c, ct, jj = w
            j = 4 * ct + jj
            sl = slice(c * TC, (c + 1) * TC)
            b1, b1k, b2, b2k, b3, b3k, b4, b4k = st[w]["h"]
            psy, pky = pg.ps[6], pg.psk[6]
            k.op('pe', lambda: nc.tensor.matmul(psy[:, :], lhsT=Cw[:, 0, j, :], rhs=b1[:], start=(jj == 0), stop=False), ['s_Cw', b1k], [pky])
            k.op('pe', lambda: nc.tensor.matmul(psy[:, :], lhsT=NCw[:, j, :], rhs=b2[:], start=False, stop=False), ['s_NCw', b2k], [pky])
            k.op('pe', lambda: nc.tensor.matmul(psy[:, :], lhsT=Cw[:, 1, j, :], rhs=b3[:], start=False, stop=False), ['s_Cw', b3k], [pky])
            k.op('pe', lambda: nc.tensor.matmul(psy[:, :], lhsT=Cw[:, 1, j, :], rhs=b4[:], start=False, stop=(jj == 3)), ['s_Cw', b4k], [pky])
            del st[w]
            if jj != 3:
                return
            if ct == 0:
                zstate["z"] = zr.next()
                zstate["zb"] = zbr.next()
            z32, z32k = zstate["z"]
            zb, zbk = zstate["zb"]
            u32, u32k = ur.next()
            k.dma(u32[:], sc["zu"][ct * 128:(ct + 1) * 128, sl], writes=[u32k])
            y_, yk = yr.next()
            k.op('dve', lambda: nc.vector.scalar_tensor_tensor(out=y_[:], in0=u32[:], scalar=dcol[:, ct:ct + 1], in1=psy[:, :], op0=ALU.mult, op1=ALU.add), [u32k, 's_d', pky], [yk])
            k.op('act', lambda: nc.scalar.activation(out=z32[:, ct, :], in_=y_[:], func=AF.Gelu_apprx_tanh), [yk], [(z32k, ct)])
            k.op('act', lambda: nc.scalar.copy(out=zb[:, ct, :], in_=z32[:, ct, :]), [(z32k, ct)], [(zbk, ct)])
            if ct != 1:
                return
            for co in range(2):
                psg, pkg = pg.ps[7], pg.psk[7]
                for ci in range(2):
                    k.op('pe', lambda: nc.tensor.matmul(psg[:, :], lhsT=glu[:, ci, co * 128:(co + 1) * 128], rhs=zb[:, ci, :], start=(ci == 0), stop=(ci == 1)), ['s_glu', (zbk, ci)], [pkg])
                sg, sgk = yr.next()
                k.op('act', lambda: nc.scalar.activation(out=sg[:], in_=psg[:, :], func=AF.Sigmoid), [pkg], [sgk])
                ob, obk = obr.next()
                k.op('dve', lambda: nc.vector.tensor_tensor(out=ob[:], in0=z32[:, co, :], in1=sg[:], op=ALU.mult), [(z32k, co), sgk], [obk])
                k.dma(sc["yT"][0, co * 128:(co + 1) * 128, sl], ob[:], reads=[obk])

        phases = (phA, phB, phC, phD, phE)
        n = len(items)
        for s_ in range(n + 4):
            for pi_, ph in enumerate(phases):
                i = s_ - pi_
                if 0 <= i < n:
                    ph(items[i])
        k.barrier()


def stage_sc(pg, d, sc):
    nc, k = pg.nc, pg.k
    with ExitStack() as es:
        cw = es.enter_context(_sbt(nc, "sb_c_w", [128, 2, 3], F32))
        k.dma(cw[:], d["sccw"], writes=['c_w'])
        T = [es.enter_context(_sbt(nc, "sb_c_t%d" % i, [128, S], F32)) for i in range(8)]
        ob = es.enter_context(_sbt(nc, "sb_c_ob", [128, S], BF16))
        pr, ac = T[6], T[7]
        for ct in range(2):
            bt, ctile, xt = T[3 * ct:3 * ct + 3]
            k.dma(ctile[:], sc["zsc"][256 + ct * 128:256 + (ct + 1) * 128, :], writes=[('c_c', ct)])
            k.dma(xt[:], sc["zsc"][512 + ct * 128:512 + (ct + 1) * 128, :], writes=[('c_x', ct)])
            k.dma(bt[:], sc["zsc"][ct * 128:(ct + 1) * 128, :], writes=[('c_b', ct)])
        for ct in range(2):
            bt, ctile, xt = T[3 * ct:3 * ct + 3]
            k.op('dve', lambda: nc.vector.tensor_tensor(out=pr[:], in0=ctile[:], in1=xt[:], op=ALU.mult), [('c_c', ct), ('c_x', ct)], ['c_pr'])
            k.op('dve', lambda: nc.vector.tensor_scalar(out=ac[:], in0=pr[:], scalar1=cw[:, ct, 2:3], scalar2=None, op0=ALU.mult), ['c_pr', 'c_w'], ['c_ac'])
            k.op('dve', lambda: nc.vector.scalar_tensor_tensor(out=ac[:, 1:], in0=pr[:, :S - 1], scalar=cw[:, ct, 1:2], in1=ac[:, 1:], op0=ALU.mult, op1=ALU.add), ['c_pr', 'c_w', 'c_ac'], ['c_ac'])
            k.op('dve', lambda: nc.vector.scalar_tensor_tensor(out=ac[:, 2:], in0=pr[:, :S - 2], scalar=cw[:, ct, 0:1], in1=ac[:, 2:], op0=ALU.mult, op1=ALU.add), ['c_pr', 'c_w', 'c_ac'], ['c_ac'])
            k.op('dve', lambda: nc.vector.tensor_tensor(out=ob[:], in0=ac[:], in1=bt[:], op=ALU.mult), ['c_ac', ('c_b', ct)], ['c_ob'])
            k.dma(sc["yT"][3, ct * 128:(ct + 1) * 128, :], ob[:], reads=['c_ob'])
        k.barrier()


def stage_lru(pg, d, sc):
    nc, k = pg.nc, pg.k
    with ExitStack() as es:
        cw = es.enter_context(_sbt(nc, "sb_l_cw", [128, 2, 4], F32))
        pv = es.enter_context(_sbt(nc, "sb_l_pv", [128, 4, 2], F32))
        sp = es.enter_context(_sbt(nc, "sb_l_sp", [128, 4, 2], F32))
        wa = es.enter_context(_sbt(nc, "sb_l_wa", [128, 2, 128], BF16))
        wx = es.enter_context(_sbt(nc, "sb_l_wx", [128, 2, 128], BF16))
        k.dma(cw[:], d["lrucw"], writes=['l_cw'])
        for i, nm in enumerate(("lrucb", "lruba", "lrubx", "lrulam")):
            k.dma(pv[:, i, :], d[nm], writes=[('l_pv', i)])
        k.dma(wa[:], d["lruwa"].rearrange("c p n -> p c n"), writes=['l_wa'], q='pool')
        k.dma(wx[:], d["lruwx"].rearrange("c p n -> p c n"), writes=['l_wx'], q='pool')
        k.op('act', lambda: nc.scalar.activation(out=sp[:, 0, :], in_=pv[:, 3, :], func=AF.Exp, scale=-1.0), [('l_pv', 3)], [('l_sp', 0)])
        k.op('act', lambda: nc.scalar.activation(out=sp[:, 1, :], in_=sp[:, 0, :], func=AF.Ln, bias=1.0), [('l_sp', 0)], [('l_sp', 1)])
        k.op('dve', lambda: nc.vector.tensor_scalar(out=sp[:, 2, :], in0=sp[:, 1, :], scalar1=-8.0, scalar2=None, op0=ALU.mult), [('l_sp', 1)], [('l_sp', 2)])
        k.op('dve', lambda: nc.vector.tensor_scalar(out=sp[:, 3, :], in0=sp[:, 1, :], scalar1=-16.0, scalar2=None, op0=ALU.mult), [('l_sp', 1)], [('l_sp', 3)])
        T = [es.enter_context(_sbt(nc, "sb_l_t%d" % i, [128, S], F32)) for i in range(9)]
        xcb = es.enter_context(_sbt(nc, "sb_l_xcb", [128, S], BF16))
        ob = es.enter_context(_sbt(nc, "sb_l_ob", [128, S], BF16))
        for ct in range(2):
            k.dma(T[5 + 2 * ct][:], sc["zlru"][ct * 128:(ct + 1) * 128, :], writes=[('l_X', ct)])
        for ct in range(2):
            k.dma(T[6 + 2 * ct][:], sc["zlru"][256 + ct * 128:256 + (ct + 1) * 128, :], writes=[('l_G', ct)])
        for ct in range(2):
            XC, R, I, A, TT = T[0:5]
            X, G = T[5 + 2 * ct], T[6 + 2 * ct]
            kx, kxc, kr, ki, ka, kt_ = ('l_X', ct), 'l_XC', 'l_R', 'l_I', 'l_A', 'l_T'
            k.op('dve', lambda: nc.vector.tensor_scalar(out=XC[:], in0=X[:], scalar1=cw[:, ct, 3:4], scalar2=pv[:, 0, ct:ct + 1], op0=ALU.mult, op1=ALU.add), [kx, 'l_cw', ('l_pv', 0)], [kxc])
            for sh in (1, 2, 3):
                k.op('dve', lambda: nc.vector.scalar_tensor_tensor(out=XC[:, sh:], in0=X[:, :S - sh], scalar=cw[:, ct, 3 - sh:4 - sh], in1=XC[:, sh:], op0=ALU.mult, op1=ALU.add), [kx, 'l_cw', kxc], [kxc])
            k.op('dve', lambda: nc.vector.tensor_copy(out=xcb[:], in_=XC[:]), [kxc], ['l_xcb'])
            for qc in range(8):
                sl = slice(qc * 512, (qc + 1) * 512)
                for (w_, wk_, dst, dk, bi, pb) in ((wa, 'l_wa', R, kr, 1, 0), (wx, 'l_wx', I, ki, 2, 1)):
                    ps, pk = pg.ps[pb + 2 * (qc % 2)], pg.psk[pb + 2 * (qc % 2)]
                    k.op('pe', lambda: nc.tensor.matmul(ps[:, :], lhsT=w_[:, ct, :], rhs=xcb[:, sl], start=True, stop=True), [wk_, 'l_xcb'], [pk])
                    k.op('act', lambda: nc.scalar.activation(out=dst[:, sl], in_=ps[:, :], func=AF.Sigmoid, bias=pv[:, bi, ct:ct + 1]), [pk, ('l_pv', bi)], [(dk, qc)])
            rk = [(kr, qc) for qc in range(8)]
            ik = [(ki, qc) for qc in range(8)]
            k.op('act', lambda: nc.scalar.activation(out=A[:], in_=R[:], func=AF.Exp, scale=sp[:, 2, ct:ct + 1]), rk + [('l_sp', 2)], [ka])
            k.op('act', lambda: nc.scalar.activation(out=TT[:], in_=R[:], func=AF.Exp, scale=sp[:, 3, ct:ct + 1]), rk + [('l_sp', 3)], [kt_])
            k.op('act', lambda: nc.scalar.activation(out=TT[:], in_=TT[:], func=AF.Sqrt, scale=-1.0, bias=1.0), [kt_], [kt_])
            k.op('dve', lambda: nc.vector.tensor_tensor(out=I[:], in0=I[:], in1=XC[:], op=ALU.mult), ik + [kxc], ik + ['l_I2'])
            k.op('dve', lambda: nc.vector.tensor_tensor(out=TT[:], in0=TT[:], in1=I[:], op=ALU.mult), [kt_, 'l_I2'] + ik, [kt_])
            k.op('dve', lambda: nc.vector.tensor_tensor_scan(out=R[:], data0=A[:], data1=TT[:], initial=0.0, op0=ALU.mult, op1=ALU.add), [ka, kt_] + rk, rk + ['l_H'])
            k.op('act', lambda: nc.scalar.activation(out=A[:], in_=G[:], func=AF.Gelu_apprx_tanh), [('l_G', ct)], [ka])
            k.op('dve', lambda: nc.vector.tensor_tensor(out=ob[:], in0=R[:], in1=A[:], op=ALU.mult), ['l_H', ka] + rk, ['l_ob'])
            k.dma(sc["yT"][2, ct * 128:(ct + 1) * 128, :], ob[:], reads=['l_ob'])
        k.barrier()


def stage_merge(pg, d, sc, xsrc, xdst, grow_l):
    nc, k = pg.nc, pg.k
    with ExitStack() as es:
        wb = es.enter_context(_sbt(nc, "sb_g_wb", [128, 4, 2, 1024], BF16))
        wo = es.enter_context(_sbt(nc, "sb_g_wo", [128, 8, 1024], BF16))
        k.dma(wb[:], d["wbr"].rearrange("m (ct p) n -> p m ct n", p=128), writes=['g_wb'], q='pool')
        k.dma(wo[:], d["wout"].rearrange("(kc p) n -> p kc n", p=128), writes=['g_wo'], q='pool')
        ymr = Ring(nc, es, "g_ym", [128, 4, 2, 512], BF16, 2)
        gmr = Ring(nc, es, "g_gm", [128, 4, 512], BF16, 4)
        tbr = Ring(nc, es, "g_tb", [128, 512], BF16, 12)
        tmr = Ring(nc, es, "g_tm", [128, 512], F32, 4)
        mTr = Ring(nc, es, "g_mT", [128, 8, 512], BF16, 2)
        xr = Ring(nc, es, "g_x", [128, 1024], F32, 3)
        gmv = sc["zgm"].rearrange("(m dd r) t -> r m dd t", m=4, dd=8)
        pi = 0
        po = 0

        def emit_acc(pend):
            dt, tms, mT, mTk = pend
            ps, pk = pg.ps[3], pg.psk[3]
            for m, (tb, tbk) in enumerate(tms):
                k.op('pe', lambda: nc.tensor.matmul(ps[:, :], lhsT=pg.identb[:], rhs=tb[:], start=(m == 0), stop=(m == 3)), ['identb', tbk], [pk])
            k.op('act', lambda: nc.scalar.copy(out=mT[:, dt, :], in_=ps[:, :]), [pk], [(mTk, dt)])

        for qc in range(8):
            sl = slice(qc * 512, (qc + 1) * 512)
            ym, ymk = ymr.next()
            k.dma(ym[:], sc["yT"].rearrange("m (ct p) t -> p m ct t", p=128)[:, :, :, sl], writes=[ymk])
            mT, mTk = mTr.next()
            pend = None
            for dt in range(8):
                gm, gmk = gmr.next()
                k.dma(gm[:], gmv[:, :, dt, sl], writes=[gmk])
                tms = []
                for m in range(4):
                    ps, pk = pg.ps[pi % 3], pg.psk[pi % 3]
                    pi += 1
                    for ct in range(2):
                        k.op('pe', lambda: nc.tensor.matmul(ps[:, :], lhsT=wb[:, m, ct, dt * 128:(dt + 1) * 128], rhs=ym[:, m, ct, :], start=(ct == 0), stop=(ct == 1)), ['g_wb', ymk], [pk])
                    tb, tbk = tbr.next()
                    k.op('dve', lambda: nc.vector.tensor_tensor(out=tb[:], in0=ps[:, :], in1=gm[:, m, :], op=ALU.mult), [pk, gmk], [tbk])
                    tms.append((tb, tbk))
                if pend is not None:
                    emit_acc(pend)
                pend = (dt, tms, mT, mTk)
            emit_acc(pend)
            for tt in range(4):
                xt, xk = xr.next()
                T0 = qc * 4 + tt
                k.dma(xt[:], xsrc[T0 * 128:(T0 + 1) * 128, :], writes=[xk])
                for ch in range(2):
                    ps, pk = pg.ps[4 + po % 4], pg.psk[4 + po % 4]
                    po += 1
                    for kc in range(8):
                        k.op('pe', lambda: nc.tensor.matmul(ps[:, :], lhsT=mT[:, kc, tt * 128:(tt + 1) * 128], rhs=wo[:, kc, ch * 512:(ch + 1) * 512], start=(kc == 0), stop=(kc == 7)), [(mTk, kc), 'g_wo'], [pk])
                    tm, tmk = tmr.next()
                    k.op('dve', lambda: nc.vector.tensor_tensor(out=tm[:], in0=ps[:, :], in1=grow_l[:, 0, ch * 512:(ch + 1) * 512], op=ALU.mult), [pk], [tmk])
                    k.op('pool', lambda: nc.gpsimd.tensor_tensor(out=xt[:, ch * 512:(ch + 1) * 512], in0=xt[:, ch * 512:(ch + 1) * 512], in1=tm[:], op=ALU.add), [xk, tmk], [xk])
                k.dma(xdst[T0 * 128:(T0 + 1) * 128, :], xt[:], reads=[xk], q='pool')
        k.barrier()


def stage_ffn(pg, xsrc, xdst, gm, sh, gmk, shk, grow_g, experts, F, router=None, final=None):
    nc, k = pg.nc, pg.k
    NF = F // 128
    TC = 1024
    with ExitStack() as es:
        rings = mk_norm_rings(nc, es)
        nbuf = 2 if router is None else 1
        hTs = [es.enter_context(_sbt(nc, "sb_f_hT%d" % i, [128, 8, TC], BF16)) for i in range(nbuf)]

        def hkeyf(par):
            return lambda kc, c: ('f_hT', par, kc, c // 128)
        gen = None
        aT = es.enter_context(_sbt(nc, "sb_f_aT", [128, NF, TC], BF16))
        nwd = 1 if router is None else 2
        wdr = Ring(nc, es, "f_wd", [128, NF, 1024], BF16, nwd)
        wgr = Ring(nc, es, "f_wg", [128, 8, 512], BF16, 4)
        sr = Ring(nc, es, "f_s", [128, 512], F32, 3)
        rt = None
        if router is None:
            tmr = Ring(nc, es, "f_tm", [128, 512], F32, 3)
        else:
            acc = es.enter_context(_sbt(nc, "sb_f_acc", [128, 8, 1024], F32))
            rt = {"h32": Ring(nc, es, "f_h32", [128, 8, 128], F32, 2),
                  "w": es.enter_context(_sbt(nc, "sb_f_rw", [128, 8, 8], F32)),
                  "b": es.enter_context(_sbt(nc, "sb_f_rb", [128, 8], F32)),
                  "logits": es.enter_context(_sbt(nc, "sb_f_lg", [128, 8, 8], F32))}
            k.dma(rt["w"][:], router["w"].rearrange("(kc p) n -> p kc n", p=128), writes=['rw'])
            k.dma(rt["b"][:], router["b"].to_broadcast([128, 8]), writes=['rb'])
            comb = es.enter_context(_sbt(nc, "sb_f_comb", [128, 8, 8], F32))
            if final is not None:
                fgt = es.enter_context(_sbt(nc, "sb_f_fing", [128, 1024], F32))
                k.dma(fgt[:], final[1].to_broadcast([128, 1024]), writes=['fin_g'])
            rs = es.enter_context(_sbt(nc, "sb_f_rs", [128, 8, 16], F32))
            em = es.enter_context(_sbt(nc, "sb_f_em", [128, 8, 8], F32))
        pi = 0

        def wb_tile(c4w, tl):
            T0 = c4w * 8 + tl
            xt, xk = rings["x"].next()
            k.dma(xt[:], xsrc[T0 * 128:(T0 + 1) * 128, :], writes=[xk])
            ak = [('acc', tl, 0), ('acc', tl, 1)]
            k.op('pool', lambda: nc.gpsimd.tensor_tensor(out=acc[:, tl, :], in0=acc[:, tl, :], in1=grow_g[:, :], op=ALU.mult), ak, ak)
            k.op('dve', lambda: nc.vector.tensor_tensor(out=xt[:], in0=xt[:], in1=acc[:, tl, :], op=ALU.add), [xk] + ak, [xk])
            if final is None:
                k.dma(xdst[T0 * 128:(T0 + 1) * 128, :], xt[:], reads=[xk])
                return
            sq, sqk = rings["sq"].next()
            st, stk = rings["st"].next()
            k.op('act', lambda: nc.scalar.activation(out=sq[:], in_=xt[:], func=AF.Square, accum_out=st[:, 0:1]), [xk], [sqk, (stk, 0)])
            k.op('dve', lambda: nc.vector.tensor_scalar(out=st[:, 1:2], in0=st[:, 0:1], scalar1=1.0 / D, scalar2=1e-6, op0=ALU.mult, op1=ALU.add), [(stk, 0)], [(stk, 1)])
            k.op('act', lambda: nc.scalar.activation(out=st[:, 2:3], in_=st[:, 1:2], func=AF.Sqrt), [(stk, 1)], [(stk, 2)])
            k.op('dve', lambda: nc.vector.reciprocal(out=st[:, 3:4], in_=st[:, 2:3]), [(stk, 2)], [(stk, 3)])
            xn, xnk = rings["xn"].next()
            k.op('dve', lambda: nc.vector.scalar_tensor_tensor(out=xn[:], in0=xt[:], scalar=st[:, 3:4], in1=fgt[:], op0=ALU.mult, op1=ALU.mult), [xk, (stk, 3), 'fin_g'], [xnk])
            k.dma(final[0][T0 * 128:(T0 + 1) * 128, :], xn[:], reads=[xnk], is_output=True)

        nch = S // TC
        for c4 in range(nch):
            tts = list(range(c4 * 8, c4 * 8 + 8))
            hTc = hTs[c4 % nbuf]
            hkey = hkeyf(c4 % nbuf)
            if gen is None:
                norm_tiles(pg, rings, xsrc, tts, hTc, 0, gm, sh, gmk, shk, hkey, router=rt)
            else:
                for _ in gen:
                    pass
                gen = None
            if router is not None:
                lg = rt["logits"]
                for i in range(8):
                    lk = ('logits', i)
                    k.op('dve', lambda: nc.vector.max(out=rs[:, i, 0:8], in_=lg[:, i, :]), [lk], [('rs', i, 0)])
                    k.op('dve', lambda: nc.vector.tensor_scalar(out=rs[:, i, 8:9], in0=rs[:, i, 0:1], scalar1=-1.0, scalar2=None, op0=ALU.mult), [('rs', i, 0)], [('rs', i, 1)])
                    k.op('act', lambda: nc.scalar.activation(out=em[:, i, :], in_=lg[:, i, :], func=AF.Exp, bias=rs[:, i, 8:9]), [lk, ('rs', i, 1)], [('em', i)])
                    k.op('dve', lambda: nc.vector.tensor_scalar(out=comb[:, i, :], in0=lg[:, i, :], scalar1=rs[:, i, 1:2], scalar2=None, op0=ALU.is_ge), [lk, ('rs', i, 0)], [('comb', i)])
                    k.op('dve', lambda: nc.vector.tensor_tensor(out=em[:, i, :], in0=em[:, i, :], in1=comb[:, i, :], op=ALU.mult), [('em', i), ('comb', i)], [('em', i)])
                    k.op('dve', lambda: nc.vector.reduce_sum(out=rs[:, i, 9:10], in_=em[:, i, :], axis=AX.X), [('em', i)], [('rs', i, 2)])
                    k.op('dve', lambda: nc.vector.reciprocal(out=rs[:, i, 10:11], in_=rs[:, i, 9:10]), [('rs', i, 2)], [('rs', i, 3)])
                    k.op('dve', lambda: nc.vector.tensor_scalar(out=comb[:, i, :], in0=em[:, i, :], scalar1=rs[:, i, 10:11], scalar2=None, op0=ALU.mult), [('em', i), ('rs', i, 3)], [('comb', i)])
            for e, (Wg, Wu, Wd) in enumerate(experts):
                wgv = Wg.rearrange("(kc p) n -> p kc n", p=128)
                wuv = Wu.rearrange("(kc p) n -> p kc n", p=128)
                for f0 in range(0, F, 512):
                    n = min(512, F - f0)
                    wg, wgk = wgr.next()
                    wu, wuk = wgr.next()
                    k.dma(wg[:, :, 0:n], wgv[:, :, f0:f0 + n], writes=[wgk], q='pool')
                    k.dma(wu[:, :, 0:n], wuv[:, :, f0:f0 + n], writes=[wuk], q='pool')
                    for ft in range(n // 128):
                        fi = f0 // 128 + ft
                        for hf in range(2):
                            psg, pkg = pg.ps[4 + pi % 2], pg.psk[4 + pi % 2]
                            psu, pku = pg.ps[6 + pi % 2], pg.psk[6 + pi % 2]
                            pi += 1
                            hk = [hkey(kc_, hf * 512 + j * 128) for kc_ in range(8) for j in range(4)]
                            for kc in range(8):
                                k.op('pe', lambda: nc.tensor.matmul(psg[:, :], lhsT=wg[:, kc, ft * 128:(ft + 1) * 128], rhs=hTc[:, kc, hf * 512:(hf + 1) * 512], start=(kc == 0), stop=(kc == 7)), [wgk] + hk, [pkg])
                            for kc in range(8):
                                k.op('pe', lambda: nc.tensor.matmul(psu[:, :], lhsT=wu[:, kc, ft * 128:(ft + 1) * 128], rhs=hTc[:, kc, hf * 512:(hf + 1) * 512], start=(kc == 0), stop=(kc == 7)), [wuk] + hk, [pku])
                            s_, sk = sr.next()
                            k.op('act', lambda: nc.scalar.activation(out=s_[:], in_=psg[:, :], func=AF.Silu), [pkg], [sk])
                            k.op('dve', lambda: nc.vector.tensor_tensor(out=aT[:, fi, hf * 512:(hf + 1) * 512], in0=psu[:, :], in1=s_[:], op=ALU.mult), [pku, sk], [('f_aT', fi, hf)])
                wd, wdk = wdr.next()
                k.dma(wd[:], Wd.rearrange("(fc p) n -> p fc n", p=128), writes=[wdk], q='pool')
                for tl in range(8):
                    T0 = c4 * 8 + tl
                    if router is not None and e == 0 and c4 > 0:
                        wb_tile(c4 - 1, tl)
                    if router is None:
                        xt, xk = rings["x"].next()
                        k.dma(xt[:], xsrc[T0 * 128:(T0 + 1) * 128, :], writes=[xk])
                    for ch in range(2):
                        ps, pk = pg.ps[pi % 4], pg.psk[pi % 4]
                        pi += 1
                        for fc in range(NF):
                            k.op('pe', lambda: nc.tensor.matmul(ps[:, :], lhsT=aT[:, fc, tl * 128:(tl + 1) * 128], rhs=wd[:, fc, ch * 512:(ch + 1) * 512], start=(fc == 0), stop=(fc == NF - 1)), [('f_aT', fc, tl // 4), wdk], [pk])
                        csl = slice(ch * 512, (ch + 1) * 512)
                        if router is None:
                            tm, tmk = tmr.next()
                            k.op('dve', lambda: nc.vector.tensor_tensor(out=tm[:], in0=ps[:, :], in1=grow_g[:, csl], op=ALU.mult), [pk], [tmk])
                            k.op('pool', lambda: nc.gpsimd.tensor_tensor(out=xt[:, csl], in0=xt[:, csl], in1=tm[:], op=ALU.add), [xk, tmk], [xk])
                        elif e == 0:
                            k.op('dve', lambda: nc.vector.tensor_scalar(out=acc[:, tl, csl], in0=ps[:, :], scalar1=comb[:, tl, e:e + 1], scalar2=None, op0=ALU.mult), [pk, ('comb', tl)], [('acc', tl, ch)])
                        else:
                            k.op('dve', lambda: nc.vector.scalar_tensor_tensor(out=acc[:, tl, csl], in0=ps[:, :], scalar=comb[:, tl, e:e + 1], in1=acc[:, tl, csl], op0=ALU.mult, op1=ALU.add), [pk, ('comb', tl), ('acc', tl, ch)], [('acc', tl, ch)])
                    if router is None:
                        k.dma(xdst[T0 * 128:(T0 + 1) * 128, :], xt[:], reads=[xk], q='pool')
                        if c4 + 1 < nch:
                            if gen is None:
                                ntts = list(range((c4 + 1) * 8, (c4 + 1) * 8 + 8))
                                gen = norm_tiles_gen(pg, rings, xsrc, ntts, hTs[(c4 + 1) % nbuf], 0, gm, sh, gmk, shk, hkeyf((c4 + 1) % nbuf), pbase=4)
                            next(gen, None)
        if router is not None:
            for tl in range(8):
                wb_tile(S // TC - 1, tl)
        k.barrier()


def stage_final(pg, xsrc, out_d, g_row):
    nc, k = pg.nc, pg.k
    with ExitStack() as es:
        rings = mk_norm_rings(nc, es)
        gt = es.enter_context(_sbt(nc, "sb_fin_g", [128, 1024], F32))
        k.dma(gt[:], g_row.to_broadcast([128, 1024]), writes=['fin_g'])
        for tt in range(NT):
            xt, xk = rings["x"].next()
            k.dma(xt[:], xsrc[tt * 128:(tt + 1) * 128, :], writes=[xk])
            sq, sqk = rings["sq"].next()
            st, stk = rings["st"].next()
            k.op('act', lambda: nc.scalar.activation(out=sq[:], in_=xt[:], func=AF.Square, accum_out=st[:, 0:1]), [xk], [sqk, (stk, 0)])
            k.op('dve', lambda: nc.vector.tensor_scalar(out=st[:, 1:2], in0=st[:, 0:1], scalar1=1.0 / D, scalar2=1e-6, op0=ALU.mult, op1=ALU.add), [(stk, 0)], [(stk, 1)])
            k.op('act', lambda: nc.scalar.activation(out=st[:, 2:3], in_=st[:, 1:2], func=AF.Sqrt), [(stk, 1)], [(stk, 2)])
            k.op('dve', lambda: nc.vector.reciprocal(out=st[:, 3:4], in_=st[:, 2:3]), [(stk, 2)], [(stk, 3)])
            xn, xnk = rings["xn"].next()
            k.op('dve', lambda: nc.vector.scalar_tensor_tensor(out=xn[:], in0=xt[:], scalar=st[:, 3:4], in1=gt[:], op0=ALU.mult, op1=ALU.mult), [xk, (stk, 3), 'fin_g'], [xnk])
            k.dma(out_d[tt * 128:(tt + 1) * 128, :], xn[:], reads=[xnk], is_output=True)
        k.barrier()


def stage_nsa(pg, d, sc):
    nc, k = pg.nc, pg.k
    SCALE = 0.125
    with ExitStack() as es:
        sbt = lambda name, shape, dt=F32: es.enter_context(_sbt(nc, "sb_a_" + name, shape, dt))
        kT = sbt("kT", [64, 3, S], BF16)
        k.dma(kT[:, 0, :], sc["zk"][0], writes=[('a_kT', 0)])
        k.dma(kT[:, 2, :], sc["zk"][2], writes=[('a_kT', 2)])
        kx = sbt("kx", [128, S], BF16)
        k.dma(kx[0:64, :], sc["zk"][1], writes=['a_kx0'])
        k.dma(kx[64:128, :], pg.din["xpf"], writes=['a_kx1'])
        V1 = [sbt("V1_%d" % i, [128, 32, 65], BF16) for i in range(2)]
        zvv = sc["zv"].rearrange("(kt p) c -> p kt c", p=128)
        for i in range(2):
            k.dma(V1[i][:, :, 0:64], zvv[:, :, 64 * (i + 1):64 * (i + 2)], writes=[('a_V1', i)])
            k.op('pool', lambda: nc.gpsimd.memset(V1[i][:, :, 64:65], 1.0), [], [('a_V1o', i)])
        caus = sbt("caus", [128, 4, 512], BF16)
        wlow = sbt("wlow", [128, 4, 512], BF16)
        k.dma(caus[:], pg.din["caus"], writes=['a_caus'])
        k.dma(wlow[:], pg.din["wlow"], writes=['a_wlow'])
        gts = sbt("gts", [128, 32, 12])
        k.dma(gts[:], sc["zg"].rearrange("(tt p) c -> p tt c", p=128), writes=['a_g'])
        kcmpT = sbt("kcmpT", [64, 256], BF16)
        VC1 = sbt("VC1", [128, 2, 129], BF16)
        with ExitStack() as cs:
            cbt = lambda name, shape, dt=F32: cs.enter_context(_sbt(nc, "sb_ac_" + name, shape, dt))
            vcT = cbt("vcT", [64, S], BF16)
            k.dma(vcT[:], sc["zvc"], writes=['ac_vcT'])
            ovl = cbt("ovl", [128, 2, 64])
            k.dma(ovl[:], pg.din["ovl"], writes=['ac_ovl'])
            for nt in range(2):
                k.op('pool', lambda: nc.gpsimd.memset(VC1[:, nt, 64:65], 1.0), [], [('a_VC1o', nt)])
                k.op('pool', lambda: nc.gpsimd.tensor_copy(out=VC1[:, nt, 65:129], in_=ovl[:, nt, :]), ['ac_ovl'], [('a_VC1v', nt)])
            k.op('pool', lambda: nc.gpsimd.memset(kcmpT[:], 0.0), [], ['a_kcmpT'])
            for which, (w1n, w2n, pen) in enumerate((("ckw1", "ckw2", "pekT"), ("cvw1", "cvw2", "pevT"))):
                w1 = cbt("w1_%d" % which, [64, 32, 128], BF16)
                w2 = cbt("w2_%d" % which, [128, 64], BF16)
                pe = cbt("pe_%d" % which, [64, 32], BF16)
                gT = cbt("gT_%d" % which, [128, 256], BF16)
                bc = cbt("bc_%d" % which, [128, 1])
                kk = 'ac%d_' % which
                k.dma(w1[:], d[w1n].rearrange("(l dd) j -> dd l j", dd=64), writes=[kk + 'w1'], q='pool')
                k.dma(w2[:], d[w2n], writes=[kk + 'w2'], q='pool')
                k.dma(pe[:], d[pen], writes=[kk + 'pe'], q='pool')
                k.op('pool', lambda: nc.gpsimd.memset(gT[:], 0.0), [], [kk + 'gT'])
                psb, pkb = pg.ps[6], pg.psk[6]
                for l in range(32):
                    k.op('pe', lambda: nc.tensor.matmul(psb[:, 0:1], lhsT=w1[:, l, :], rhs=pe[:, l:l + 1], start=(l == 0), stop=(l == 31)), [kk + 'w1', kk + 'pe'], [pkb])
                k.op('dve', lambda: nc.vector.tensor_copy(out=bc[:], in_=psb[:, 0:1]), [pkb], [kk + 'bc'])
                psh, pkh = pg.ps[7], pg.psk[7]
                for l in range(32):
                    src = kT[:, 0, l:l + 16 * 254 + 1:16] if which == 0 else vcT[:, l:l + 16 * 254 + 1:16]
                    k.op('pe', lambda: nc.tensor.matmul(psh[:, 0:255], lhsT=w1[:, l, :], rhs=src, start=(l == 0), stop=(l == 31)), [kk + 'w1', ('a_kT', 0) if which == 0 else 'ac_vcT'], [pkh])
                k.op('act', lambda: nc.scalar.activation(out=gT[:, 0:255], in_=psh[:, 0:255], func=AF.Gelu_apprx_tanh, bias=bc[:, 0:1]), [pkh, kk + 'bc', kk + 'gT'], [kk + 'gT'])
                if which == 0:
                    ps, pk = pg.ps[5], pg.psk[5]
                    k.op('pe', lambda: nc.tensor.matmul(ps[0:64, 0:255], lhsT=w2[:, :], rhs=gT[:, 0:255], start=True, stop=True), [kk + 'w2', kk + 'gT'], [pk])
                    k.op('dve', lambda: nc.vector.tensor_copy(out=kcmpT[:, 0:255], in_=ps[0:64, 0:255]), [pk, 'a_kcmpT'], ['a_kcmpT'])
                else:
                    for nt in range(2):
                        ps, pk = pg.ps[4 + nt], pg.psk[4 + nt]
                        k.op('pe', lambda: nc.tensor.matmul(ps[:, 0:64], lhsT=gT[:, nt * 128:(nt + 1) * 128], rhs=w2[:, :], start=True, stop=True), [kk + 'w2', kk + 'gT'], [pk])
                        k.op('dve', lambda: nc.vector.tensor_copy(out=VC1[:, nt, 0:64], in_=ps[:, 0:64]), [pk], [('a_VC1', nt)])
            k.barrier()
        ETn = 36
        ET = Ring(nc, es, "a_ET", [128, 512], BF16, ETn)
        OBs = [sbt("OB%d" % i, [128, 4, 2, 4, 65]) for i in range(2)]
        OCs = [sbt("OC%d" % i, [128, 4, 4, 129]) for i in range(2)]
        cmpb_r = Ring(nc, es, "a_cmpb", [128, 2, 512], BF16, 2)
        impA_r = Ring(nc, es, "a_impA", [128, 4, 64], F32, 2)
        impB_r = Ring(nc, es, "a_impB", [128, 4, 64], F32, 2)
        sm = Ring(nc, es, "a_sm", [128, 64], F32, 6)
        sbr = Ring(nc, es, "a_sb", [128, 128], F32, 8)
        for i_, t_ in enumerate(sbr.tiles):
            k.op('pool', lambda: nc.gpsimd.memset(t_[:, 0:64], 0.0), [], [sbr.keys[i_]])
        qsr = Ring(nc, es, "a_qs", [128, 4, 512], BF16, 2)
        sm8 = Ring(nc, es, "a_sm8", [128, 16], F32, 4)
        rdn = Ring(nc, es, "a_rd", [128, 12], F32, 3)
        cf = Ring(nc, es, "a_cf", [128, 12], F32, 3)
        otok = Ring(nc, es, "a_ot", [128, 256], F32, 8)
        yb = Ring(nc, es, "a_yb", [128, 2, 512], BF16, 2)
        cnt = {"s": 0, "o": 0}

        def attend(qc, h, tiles, vis, out_fn, rq, rqk):
            ets = []
            for (kl, biases, rv, rvk, kkeys) in tiles:
                pb = cnt["s"] % 3
                cnt["s"] += 1
                ps, pk = pg.ps[pb], pg.psk[pb]
                nb = len(biases)
                k.op('pe', lambda: nc.tensor.matmul(ps[:, :], lhsT=kl, rhs=rq, start=True, stop=(nb == 0)), rqk + kkeys, [pk])
                for bi, bias in enumerate(biases):
                    bl, br_, bk = bias[0:3]
                    if len(bias) > 3:
                        c0 = bias[3]
                        k.op('pe', lambda: nc.tensor.matmul(ps[:, c0:c0 + 128], lhsT=bl, rhs=br_[:, c0:c0 + 128], start=False, stop=(bi == nb - 1)), bk, [pk])
                    else:
                        k.op('pe', lambda: nc.tensor.matmul(ps[:, :], lhsT=bl, rhs=br_, start=False, stop=(bi == nb - 1)), bk, [pk])
                et, etk = ET.next()
                k.op('act', lambda: nc.scalar.activation(out=et[:], in_=ps[:, :], func=AF.Exp, scale=SCALE), [pk], [etk])
                ets.append((et, etk, rv, rvk))
            for tt in range(4):
                idx = [i for i in range(len(ets)) if vis(i, tt)]
                pb = 3 + cnt["o"] % 2
                cnt["o"] += 1
                ps, pk = pg.ps[pb], pg.psk[pb]
                for n_, i in enumerate(idx):
                    et, etk, rv, rvk = ets[i]
                    ncol = rv.shape[-1]
                    k.op('pe', lambda: nc.tensor.matmul(ps[:, 0:ncol], lhsT=et[:, tt * 128:(tt + 1) * 128], rhs=rv, start=(n_ == 0), stop=(n_ == len(idx) - 1)), [etk] + rvk, [pk])
                out_fn(tt, ps, pk)

        stq = {}

        def ph_cmp(qc):
            par = qc % 2
            OC = OCs[par]
            qsl = slice(qc * 512, (qc + 1) * 512)
            cb, cbk = cmpb_r.next()
            k.dma(cb[:], pg.din["cmpb"][:, :, qsl], writes=[cbk])
            iA, iAk = impA_r.next()
            iB, iBk = impB_r.next()
            k.dma(iA[:], pg.din["impA"][:, qc * 4:(qc + 1) * 4, :], writes=[iAk])
            k.dma(iB[:], pg.din["impB"][:, qc * 4:(qc + 1) * 4, :], writes=[iBk])
            qs, qsk = qsr.next()
            k.dma(qs[0:64, :, :], sc["zq"].rearrange("h p t -> p h t")[:, :, qsl], writes=[(qsk, 'q')])
            stq[qc] = dict(iA=(iA, iAk), iB=(iB, iBk), qs=(qs, qsk))
            nts = [0] if qc < 4 else [0, 1]
            for h in range(4):
                tiles = [(kcmpT[:, nt * 128:(nt + 1) * 128], [(pg.identb[:], cb[:, nt, :], ['identb', cbk])], VC1[:, nt, :],
                          [('a_VC1', nt), ('a_VC1o', nt), ('a_VC1v', nt)], ['a_kcmpT']) for nt in nts]

                def out_c(tt, ps, pk, h=h):
                    pg.copy('act', OC[:, tt, h, :], ps[:, 0:129], [pk], [('a_OC', par, tt, h)])
                attend(qc, h, tiles, lambda i, tt: True, out_c, qs[0:64, h, :], [(qsk, 'q')])

        def ph_select_dve(qc):
            par = qc % 2
            OC = OCs[par]
            iA, iAk = stq[qc]["iA"]
            iB, iBk = stq[qc]["iB"]
            sbs = []
            for tt in range(4):
                ock = [('a_OC', par, tt, h) for h in range(4)]
                rd, rdk = rdn.next()
                k.op('dve', lambda: nc.vector.tensor_scalar(out=rd[:, 0:4], in0=OC[:, tt, :, 64], scalar1=1e-30, scalar2=None, op0=ALU.max), ock, [(rdk, 0)])
                k.op('dve', lambda: nc.vector.reciprocal(out=rd[:, 0:4], in_=rd[:, 0:4]), [(rdk, 0)], [(rdk, 0)])
                im, imk = sm.next()
                k.op('dve', lambda: nc.vector.tensor_scalar(out=im[:], in0=OC[:, tt, 0, 65:129], scalar1=rd[:, 0:1], scalar2=None, op0=ALU.mult), ock + [(rdk, 0)], [imk])
                for h in range(1, 4):
                    k.op('dve', lambda: nc.vector.scalar_tensor_tensor(out=im[:], in0=OC[:, tt, h, 65:129], scalar=rd[:, h:h + 1], in1=im[:], op0=ALU.mult, op1=ALU.add), ock + [(rdk, 0), imk], [imk])
                k.op('dve', lambda: nc.vector.tensor_tensor(out=im[:], in0=im[:], in1=iA[:, tt, :], op=ALU.mult), [imk, iAk], [imk])
                k.op('dve', lambda: nc.vector.tensor_tensor(out=im[:], in0=im[:], in1=iB[:, tt, :], op=ALU.add), [imk, iBk], [imk])
                m8, m8k = sm8.next()
                rp, rpk = sm.next()
                k.op('dve', lambda: nc.vector.max(out=m8[:, 0:8], in_=im[:]), [imk], [(m8k, 0)])
                k.op('dve', lambda: nc.vector.match_replace(out=rp[:], in_to_replace=m8[:, 0:8], in_values=im[:], imm_value=-1e30), [imk, (m8k, 0)], [rpk])
                k.op('dve', lambda: nc.vector.max(out=m8[:, 8:16], in_=rp[:]), [rpk], [(m8k, 1)])
                sb_, sbk = sbr.next()
                k.op('dve', lambda: nc.vector.tensor_scalar(out=sb_[:, 64:128], in0=im[:], scalar1=m8[:, 15:16], scalar2=None, op0=ALU.is_ge), [imk, (m8k, 1), sbk], [sbk])
                k.op('dve', lambda: nc.vector.tensor_scalar(out=sb_[:, 64:128], in0=sb_[:, 64:128], scalar1=-NEG, scalar2=NEG, op0=ALU.mult, op1=ALU.add), [sbk], [sbk])
                sbs.append((sb_, sbk))
            stq[qc]["sbs"] = sbs

        def ph_select_T(qc):
            qs, qsk = stq[qc]["qs"]
            for tt, (sb_, sbk) in enumerate(stq[qc]["sbs"]):
                ps, pk = pg.ps[5], pg.psk[5]
                k.op('pe', lambda: nc.tensor.transpose(out=ps[:, 0:128], in_=sb_[:, :], identity=pg.ident[:]), [sbk, 'ident'], [pk])
                for h in range(4):
                    k.op('act', lambda: nc.scalar.copy(out=qs[64:128, h, tt * 128:(tt + 1) * 128], in_=ps[64:128, 0:128]), [pk], [(qsk, 's', tt, h)])

        def ph_win(qc):
            par = qc % 2
            OB = OBs[par]
            qs, qsk = stq[qc]["qs"]
            for h in range(4):
                tiles = []
                k0 = max(0, 4 * qc - 4)
                for kt in range(k0, 4 * qc + 4):
                    if kt >= 4 * qc:
                        b = [(pg.identb[:], caus[:, kt - 4 * qc, :], ['identb', 'a_caus'], 128 * (kt - 4 * qc))]
                    else:
                        b = [(pg.identb[:], wlow[:, kt - (4 * qc - 4), :], ['identb', 'a_wlow'], 128 * (kt - (4 * qc - 4)))]
                    tiles.append((kT[:, 2, kt * 128:(kt + 1) * 128], b, V1[1][:, kt, :], [('a_V1', 1), ('a_V1o', 1)], [('a_kT', 2)]))

                def out_w(tt, ps, pk, h=h):
                    pg.copy('act', OB[:, tt, 1, h, :], ps[:, 0:65], [pk], [('a_OB', par, tt, 1, h)])
                attend(qc, h, tiles, lambda i, tt, k0=k0: (4 * qc + tt - 4) <= (k0 + i) <= (4 * qc + tt), out_w, qs[0:64, h, :], [(qsk, 'q')])

        def ph_sel(qc):
            par = qc % 2
            OB = OBs[par]
            qs, qsk = stq[qc]["qs"]
            for h in range(4):
                tiles = []
                for kt in range(4 * qc + 4):
                    b = []
                    if kt >= 4 * qc:
                        b.append((pg.identb[:], caus[:, kt - 4 * qc, :], ['identb', 'a_caus'], 128 * (kt - 4 * qc)))
                    tiles.append((kx[:, kt * 128:(kt + 1) * 128], b, V1[0][:, kt, :], [('a_V1', 0), ('a_V1o', 0)], ['a_kx0', 'a_kx1']))

                def out_s(tt, ps, pk, h=h):
                    pg.copy('act', OB[:, tt, 0, h, :], ps[:, 0:65], [pk], [('a_OB', par, tt, 0, h)])
                attend(qc, h, tiles, lambda i, tt: i <= 4 * qc + tt, out_s, qs[:, h, :], [(qsk, 'q')] + [(qsk, 's', tt, h) for tt in range(4)])

        def ph_combine_dve(qc):
            par = qc % 2
            OC, OB = OCs[par], OBs[par]
            ots = []
            for tt in range(4):
                T0 = 4 * qc + tt
                rd, rdk = rdn.next()
                allk = [('a_OC', par, tt, h) for h in range(4)] + [('a_OB', par, tt, b_, h) for b_ in range(2) for h in range(4)]
                k.op('dve', lambda: nc.vector.tensor_scalar(out=rd[:, 0:4], in0=OC[:, tt, :, 64], scalar1=1e-30, scalar2=None, op0=ALU.max), allk, [(rdk, 0)])
                k.op('dve', lambda: nc.vector.tensor_copy(out=rd[:, 4:8], in_=OB[:, tt, 0, :, 64]), allk, [(rdk, 1)])
                k.op('dve', lambda: nc.vector.tensor_copy(out=rd[:, 8:12], in_=OB[:, tt, 1, :, 64]), allk, [(rdk, 2)])
                k.op('dve', lambda: nc.vector.reciprocal(out=rd[:, :], in_=rd[:, :]), [(rdk, 0), (rdk, 1), (rdk, 2)], [(rdk, 3)])
                c_, ck = cf.next()
                k.op('dve', lambda: nc.vector.tensor_tensor(out=c_[:, :].rearrange("p (b h) -> p b h", b=3), in0=rd[:, :].rearrange("p (b h) -> p b h", b=3),
                                                            in1=gts[:, T0, :].rearrange("p (h b) -> p b h", b=3), op=ALU.mult), [(rdk, 3), 'a_g'], [ck])
                ot, otk = otok.next()
                for h in range(4):
                    hs = slice(h * 64, (h + 1) * 64)
                    k.op('dve', lambda: nc.vector.tensor_scalar(out=ot[:, hs], in0=OC[:, tt, h, 0:64], scalar1=c_[:, h:h + 1], scalar2=None, op0=ALU.mult), allk + [ck], [(otk, h)])
                    for b_ in range(2):
                        k.op('dve', lambda: nc.vector.scalar_tensor_tensor(out=ot[:, hs], in0=OB[:, tt, b_, h, 0:64], scalar=c_[:, 4 * (b_ + 1) + h:4 * (b_ + 1) + h + 1], in1=ot[:, hs], op0=ALU.mult, op1=ALU.add), allk + [ck, (otk, h)], [(otk, h)])
                ots.append((ot, otk))
            stq[qc]["ots"] = ots

        def ph_combine_T(qc):
            qsl = slice(qc * 512, (qc + 1) * 512)
            ybt, ybk = yb.next()
            for tt, (ot, otk) in enumerate(stq[qc]["ots"]):
                for ct in range(2):
                    ps, pk = pg.ps[6 + ct], pg.psk[6 + ct]
                    k.op('pe', lambda: nc.tensor.transpose(out=ps[:, 0:128], in_=ot[:, ct * 128:(ct + 1) * 128], identity=pg.ident[:]), [(otk, 2 * ct), (otk, 2 * ct + 1), 'ident'], [pk])
                    pg.copy('act', ybt[:, ct, tt * 128:(tt + 1) * 128], ps[:, 0:128], [pk], [(ybk, ct, tt)])
            k.dma(sc["yT"].rearrange("m (ct p) t -> p m ct t", p=128)[:, 1, :, qsl], ybt[:], reads=[(ybk, ct, tt) for ct in range(2) for tt in range(4)])
            del stq[qc]

        for qc in range(8):
            ph_cmp(qc)
            ph_select_dve(qc)
            if qc > 0:
                ph_combine_dve(qc - 1)
            ph_win(qc)
            ph_select_T(qc)
            ph_sel(qc)
            if qc > 0:
                ph_combine_T(qc - 1)
        ph_combine_dve(7)
        ph_combine_T(7)
        k.barrier()


def make_host_inputs(inputs, b, consts=None, layers=None):
    hi = dict(consts if consts is not None else host_consts())
    hi["x"] = np.ascontiguousarray(inputs["x"][b], dtype=np.float32)
    hi["ccol"] = _col(inputs["c"][b], 8)
    layers = layers if layers is not None else [host_layer(inputs, l) for l in range(2)]
    for l in range(2):
        for kk, v in layers[l].items():
            hi["L%d_%s" % (l, kk)] = np.ascontiguousarray(v, dtype=np.float32)
    hi["ffn_wg"] = np.ascontiguousarray(inputs["ffn_w_gate"][0])
    hi["ffn_wu"] = np.ascontiguousarray(inputs["ffn_w_up"][0])
    hi["ffn_wd"] = np.ascontiguousarray(inputs["ffn_w_down"][0])
    hi["moe_wg"] = np.ascontiguousarray(inputs["moe_w_gate"][0])
    hi["moe_wu"] = np.ascontiguousarray(inputs["moe_w_up"][0])
    hi["moe_wd"] = np.ascontiguousarray(inputs["moe_w_down"][0])
    hi["moe_rw"] = np.ascontiguousarray(inputs["moe_router_w"][0])
    hi["moe_rb"] = np.ascontiguousarray(inputs["moe_router_b"][0].reshape(1, 8))
    hi["fin_g"] = np.ascontiguousarray(inputs["final_norm_g"].reshape(1, 1024))
    return hi


def kernel(**inputs):
    inputs = {k_: np.asarray(v) for k_, v in inputs.items()}
    consts = host_consts()
    layers = [host_layer(inputs, l) for l in range(2)]
    in_maps = [make_host_inputs(inputs, b, consts, layers) for b in range(8)]
    nc, pg = build(in_maps[0], set())
    res = run_bass_kernel_spmd(nc, in_maps, core_ids=list(range(8)))
    out = np.stack([np.asarray(r["out"], dtype=np.float32) for r in res.results], 0)
    return out
```
